# Optimizing a Trainium2 kernel written in Bass

```python
import math
import jax, jax.numpy as jnp
from jax import lax
import numpy as np

D_MODEL = 1024
BATCH = 8
SEQ = 4096
DEPTH = 4

N_MIXERS = 3
HEAD_DIM = 64
MIX_WIDTH = D_MODEL
MEM_LEN = 256
MEM_HEADS = 4
MEM_WIDTH = MEM_HEADS * HEAD_DIM
TOK_WIDTH = MIX_WIDTH - MEM_WIDTH

NSA_HEADS = TOK_WIDTH // HEAD_DIM
NSA_GROUPS = 4
NSA_REP = NSA_HEADS // NSA_GROUPS
NSA_KV = NSA_GROUPS * HEAD_DIM
CMP_BLOCK = 32
CMP_STRIDE = 16
CMP_HIDDEN = 256
SEL_BLOCK = 64
SEL_TOPK = 16
WINDOW = 512
Q_BLOCK = 128
FORCE_SCORE = 1.0e4
NSA_SIZES = (TOK_WIDTH, NSA_KV, NSA_KV, NSA_KV, NSA_KV, NSA_KV, NSA_KV, 3 * NSA_HEADS)
NSA_COLS = sum(NSA_SIZES) + MEM_WIDTH

REL_BUCKETS = 32
REL_MAX_DIST = 128

GLA_HEADS = 4
GLA_DV = TOK_WIDTH // GLA_HEADS
GLA_DK = GLA_DV // 2
GLA_GATE_RANK = 16
GLA_GATE_NORM = 16.0
GLA_CHUNK = 64
GLA_SIZES = (GLA_HEADS * GLA_DK, GLA_HEADS * GLA_DK, TOK_WIDTH, GLA_GATE_RANK, TOK_WIDTH)
GLA_COLS = sum(GLA_SIZES) + MEM_WIDTH

RWKV_HEADS = TOK_WIDTH // HEAD_DIM
RWKV_DECAY_LORA = 64
RWKV_AAA_LORA = 64
RWKV_GATE_LORA = 128
RWKV_SIZES = (TOK_WIDTH, RWKV_DECAY_LORA, TOK_WIDTH, TOK_WIDTH, RWKV_AAA_LORA, RWKV_GATE_LORA)
RWKV_TOK_COLS = sum(RWKV_SIZES)
RWKV_COLS = RWKV_TOK_COLS + MEM_WIDTH
RWKV_GN_EPS = 64e-5

N_EXPERTS = 32
TOP_K = 4
D_FF = D_MODEL
SWIGLU_ALPHA = 1.702
SWIGLU_LIMIT = 7.0
MOE_BLOCK = 256

DEEP_ALPHA = (2 * DEPTH) ** 0.25
DEEP_BETA = (8 * DEPTH) ** -0.25
N_NSA = (DEPTH + 2) // 3
N_GLA = (DEPTH + 1) // 3
N_RWKV = DEPTH // 3
LN_EPS = 1e-5
NEG_INF = -1e30

kernel_name = 'hybrid_nsa_gla_rwkv7_moe_deepnorm'


def _split(h, sizes):
    out, start = [], 0
    for s in sizes:
        out.append(h[..., start:start + s])
        start += s
    return out


def _layer_norm(x, g, b):
    xf = x.astype(jnp.float32)
    mu = xf.mean(-1, keepdims=True)
    var = jnp.square(xf - mu).mean(-1, keepdims=True)
    return ((xf - mu) * lax.rsqrt(var + LN_EPS) * g + b).astype(x.dtype)


def _masked_softmax(s, mask):
    s = jnp.where(mask, s.astype(jnp.float32), NEG_INF)
    e = jnp.where(mask, jnp.exp(s - s.max(-1, keepdims=True)), 0.0)
    return e / jnp.maximum(e.sum(-1, keepdims=True), 1e-30)


def _t5_bucket(dist):
    exact = REL_BUCKETS // 2
    d = jnp.maximum(dist, 0)
    scaled = jnp.log(jnp.maximum(d, 1).astype(jnp.float32) / exact) / math.log(REL_MAX_DIST / exact)
    large = jnp.minimum(exact + (scaled * (REL_BUCKETS - exact)).astype(jnp.int32), REL_BUCKETS - 1)
    return jnp.where(d < exact, d, large)


def _rel_bias(tbl, dist):
    return jnp.transpose(tbl[_t5_bucket(dist)], (2, 3, 0, 1)).astype(jnp.float32)


def _rows(arr, b, s0, n):
    start = (b, s0) + (0,) * (arr.ndim - 2)
    return lax.dynamic_slice(arr, start, (1, n) + arr.shape[2:])[0]


def _nsa_mixer(hm, rel_table, gate_b, cmp_pe, cmp_w1, cmp_w2):
    B, S, _ = hm.shape
    G, R, Dh = NSA_GROUPS, NSA_REP, HEAD_DIM
    q, kc, vc, ks, vs, kw, vw, gl = _split(hm, NSA_SIZES)
    q = (q * Dh ** -0.5).reshape(B, S, G, R, Dh)
    gates = jax.nn.sigmoid((gl + gate_b).astype(jnp.float32)).reshape(B, S, G, R, 3)
    kvh = lambda t: t.reshape(B, S, G, Dh)
    nc = (S - CMP_BLOCK) // CMP_STRIDE + 1
    cmp_start = jnp.arange(nc) * CMP_STRIDE
    cmp_end = cmp_start + CMP_BLOCK - 1
    blk_idx = cmp_start[:, None] + jnp.arange(CMP_BLOCK)[None, :]

    def compress(t, pe, w1, w2):
        tb = kvh(t)[:, blk_idx] + pe[:, None, :]
        tb = tb.transpose(0, 1, 3, 2, 4).reshape(B, nc, G, CMP_BLOCK * Dh)
        return jax.nn.gelu(tb @ w1) @ w2

    kc = compress(kc, cmp_pe[0], cmp_w1[0], cmp_w2[0])
    vc = compress(vc, cmp_pe[1], cmp_w1[1], cmp_w2[1])
    ns = S // SEL_BLOCK
    n_sel = min(SEL_TOPK, ns)
    sel_start = jnp.arange(ns) * SEL_BLOCK
    cmp_to_sel = ((cmp_start[:, None] < sel_start[None, :] + SEL_BLOCK)
                  & (cmp_end[:, None] >= sel_start[None, :])).astype(jnp.float32)
    ks = kvh(ks).reshape(B, ns, SEL_BLOCK, G, Dh).transpose(0, 3, 1, 2, 4)
    vs = kvh(vs).reshape(B, ns, SEL_BLOCK, G, Dh).transpose(0, 3, 1, 2, 4)
    kw = jnp.pad(kvh(kw), ((0, 0), (WINDOW, 0), (0, 0), (0, 0)))
    vw = jnp.pad(kvh(vw), ((0, 0), (WINDOW, 0), (0, 0), (0, 0)))
    tbl = rel_table.reshape(REL_BUCKETS, G, R)
    tbl_g = tbl.transpose(1, 0, 2)
    g_ix = jnp.arange(G)
    nq = S // Q_BLOCK

    def step(i):
        b, c = i // nq, i % nq
        s0 = c * Q_BLOCK
        tq = s0 + jnp.arange(Q_BLOCK)
        qb = _rows(q, b, s0, Q_BLOCK)
        gq = _rows(gates, b, s0, Q_BLOCK)
        kcb = lax.dynamic_index_in_dim(kc, b, 0, keepdims=False)
        vcb = lax.dynamic_index_in_dim(vc, b, 0, keepdims=False)
        dist_c = tq[:, None] - cmp_end[None, :]
        p_c = _masked_softmax(jnp.einsum('qgrd,ngd->grqn', qb, kcb) + _rel_bias(tbl, dist_c), dist_c >= 0)
        o_c = jnp.einsum('grqn,ngd->qgrd', p_c, vcb)
        imp = jnp.einsum('grqn,nm->gqm', p_c, cmp_to_sel)
        blk = jnp.arange(ns)[None, :]
        cur = (tq // SEL_BLOCK)[:, None]
        forced = (blk == 0) | (blk == cur) | (blk == cur - 1)
        score = jnp.where(blk <= cur, jnp.where(forced, FORCE_SCORE, imp), -1.0)
        top_s, top_i = lax.top_k(score, n_sel)
        ksb = lax.dynamic_index_in_dim(ks, b, 0, keepdims=False)
        vsb = lax.dynamic_index_in_dim(vs, b, 0, keepdims=False)
        ksel = ksb[g_ix[:, None, None], top_i]
        vsel = vsb[g_ix[:, None, None], top_i]
        dist_s = tq[None, :, None, None] - (top_i[..., None] * SEL_BLOCK + jnp.arange(SEL_BLOCK))
        mask_s = (top_s >= 0)[..., None] & (dist_s >= 0)
        bias_s = jnp.transpose(tbl_g[g_ix[:, None, None, None], _t5_bucket(dist_s)], (0, 4, 1, 2, 3))
        s_s = jnp.einsum('qgrd,gqnld->grqnl', qb, ksel).astype(jnp.float32) + bias_s
        nk = n_sel * SEL_BLOCK
        p_s = _masked_softmax(s_s.reshape(G, R, Q_BLOCK, nk), mask_s.reshape(G, 1, Q_BLOCK, nk))
        o_s = jnp.einsum('grqk,gqkd->qgrd', p_s, vsel.reshape(G, Q_BLOCK, nk, Dh))
        kwb = _rows(kw, b, s0, Q_BLOCK + WINDOW)
        vwb = _rows(vw, b, s0, Q_BLOCK + WINDOW)
        kpos = s0 - WINDOW + jnp.arange(Q_BLOCK + WINDOW)
        dist_w = tq[:, None] - kpos[None, :]
        mask_w = (dist_w >= 0) & (dist_w < WINDOW) & (kpos[None, :] >= 0)
        p_w = _masked_softmax(jnp.einsum('qgrd,kgd->grqk', qb, kwb) + _rel_bias(tbl, dist_w), mask_w)
        o_w = jnp.einsum('grqk,kgd->qgrd', p_w, vwb)
        o = gq[..., 0:1] * o_c + gq[..., 1:2] * o_s + gq[..., 2:3] * o_w
        return o.reshape(Q_BLOCK, TOK_WIDTH).astype(hm.dtype)

    out = lax.map(step, jnp.arange(B * nq))
    return out.reshape(B, S, TOK_WIDTH)


def _gla_mixer(hm, w_a2, b_a, b_og, norm_w):
    B, S, _ = hm.shape
    H, dk, dv, C = GLA_HEADS, GLA_DK, GLA_DV, GLA_CHUNK
    n = S // C
    f32 = jnp.float32
    q, k, v, alr, og = _split(hm, GLA_SIZES)
    gk = jax.nn.log_sigmoid((alr @ w_a2 + b_a).astype(f32)) / GLA_GATE_NORM
    rs = lambda t, d: t.astype(f32).reshape(B, n, C, H, d)
    q = rs(q, dk) * dk ** -0.5
    k = rs(k, dk)
    v = rs(v, dv)
    bcum = jnp.cumsum(rs(gk, dk), axis=2)
    blast = bcum[:, :, -1]
    qd = q * jnp.exp(bcum)
    kd = k * jnp.exp(-bcum)
    causal = jnp.tril(jnp.ones((C, C), bool))
    a = jnp.where(causal, jnp.einsum('bnchk,bnshk->bnhcs', qd, kd), 0.0)
    o_intra = jnp.einsum('bnhcs,bnshv->bnchv', a, v)
    d_state = jnp.einsum('bnchk,bnchv->bnhkv', k * jnp.exp(blast[:, :, None] - bcum), v)

    def chunk_step(state, inp):
        dec, ds = inp
        return dec[..., None] * state + ds, state

    _, s_prev = lax.scan(chunk_step, jnp.zeros((B, H, dk, dv), f32),
                         (jnp.exp(blast).transpose(1, 0, 2, 3), d_state.transpose(1, 0, 2, 3, 4)))
    s_prev = s_prev.transpose(1, 0, 2, 3, 4)
    o = (o_intra + jnp.einsum('bnchk,bnhkv->bnchv', qd, s_prev)).reshape(B, S, H, dv)
    o = o * lax.rsqrt(jnp.square(o).mean(-1, keepdims=True) + LN_EPS) * norm_w
    return o.reshape(B, S, TOK_WIDTH) * jax.nn.silu((og + b_og).astype(f32))


def _rwkv7_mixer(hm, mu, w0, w2, a0, a2, g2, k_k, k_a, r_k, ln_w, ln_b):
    B, S, _ = hm.shape
    H, N = RWKV_HEADS, HEAD_DIM
    f32 = jnp.float32
    hm = hm.astype(f32)
    prev = jnp.pad(hm, ((0, 0), (1, 0), (0, 0)))[:, :S]
    hm = hm + (prev - hm) * mu
    r, wl, k, v, al, gl = _split(hm, RWKV_SIZES)
    w = -jax.nn.softplus(-(w0 + jnp.tanh(wl) @ w2)) - 0.5
    decay = jnp.exp(-jnp.exp(w))
    a = jax.nn.sigmoid(a0 + al @ a2)
    g = jax.nn.sigmoid(gl) @ g2
    hd = lambda t: t.reshape(B, S, H, N)
    kk = hd(k * k_k)
    kk = kk / jnp.maximum(jnp.linalg.norm(kk, axis=-1, keepdims=True), 1e-12)
    k = k * (1.0 + (a - 1.0) * k_a)
    r, k, v, a, decay = hd(r), hd(k), hd(v), hd(a), hd(decay)

    def step(state, inp):
        r_t, w_t, k_t, v_t, a_t, b_t = inp
        sa = jnp.einsum('bhvk,bhk->bhv', state, a_t)
        state = state * w_t[:, :, None, :] + sa[..., None] * b_t[:, :, None, :] + v_t[..., None] * k_t[:, :, None, :]
        return state, jnp.einsum('bhvk,bhk->bhv', state, r_t)

    tm = lambda t: jnp.swapaxes(t, 0, 1)
    _, o = lax.scan(step, jnp.zeros((B, H, N, N), f32),
                    (tm(r), tm(decay), tm(k), tm(v), tm(-kk), tm(kk * a)))
    o = tm(o)
    o_mu = o.mean(-1, keepdims=True)
    o_var = jnp.square(o - o_mu).mean(-1, keepdims=True)
    o = ((o - o_mu) * lax.rsqrt(o_var + RWKV_GN_EPS)).reshape(B, S, TOK_WIDTH) * ln_w + ln_b
    o = o + ((r * k * r_k).sum(-1, keepdims=True) * v).reshape(B, S, TOK_WIDTH)
    return o * g


def _memory_attention(qm, mem, w_kv):
    B, S, _ = qm.shape
    M = mem.shape[1]
    kv = mem @ w_kv
    km = kv[..., :MEM_WIDTH].reshape(B, M, MEM_HEADS, HEAD_DIM)
    vm = kv[..., MEM_WIDTH:].reshape(B, M, MEM_HEADS, HEAD_DIM)
    qh = qm.reshape(B, S, MEM_HEADS, HEAD_DIM) * HEAD_DIM ** -0.5
    p = jax.nn.softmax(jnp.einsum('bshd,bmhd->bhsm', qh, km).astype(jnp.float32), axis=-1)
    return jnp.einsum('bhsm,bmhd->bshd', p, vm).reshape(B, S, MEM_WIDTH)


def _clamped_swiglu(h):
    glu = jnp.minimum(h[..., :D_FF], SWIGLU_LIMIT)
    lin = jnp.clip(h[..., D_FF:], -SWIGLU_LIMIT, SWIGLU_LIMIT)
    return glu * jax.nn.sigmoid(SWIGLU_ALPHA * glu) * (lin + 1.0)


def _moe(x, router_w, router_b, w_gu, b_gu, w_dn, b_dn):
    B, S, D = x.shape
    T = B * S
    A = T * TOP_K
    xt = x.reshape(T, D)
    logits = (xt @ router_w + router_b).astype(jnp.float32)
    top_val, top_idx = lax.top_k(logits, TOP_K)
    gates = jax.nn.softmax(top_val, axis=-1)
    flat_e = top_idx.reshape(-1)
    order = jnp.argsort(flat_e)
    sorted_e = flat_e[order]
    counts = jnp.bincount(flat_e, length=N_EXPERTS)
    padded = (counts + MOE_BLOCK - 1) // MOE_BLOCK * MOE_BLOCK
    pad_end = jnp.cumsum(padded)
    pad_start = pad_end - padded
    grp_start = jnp.cumsum(counts) - counts
    dest = pad_start[sorted_e] + jnp.arange(A) - grp_start[sorted_e]
    n_blocks = -(-A // MOE_BLOCK) + N_EXPERTS
    n_rows = n_blocks * MOE_BLOCK
    row_token = jnp.full((n_rows,), T, jnp.int32).at[dest].set((order // TOP_K).astype(jnp.int32))
    row_gate = jnp.zeros((n_rows,), jnp.float32).at[dest].set(gates.reshape(-1)[order])
    block_expert = jnp.minimum(jnp.searchsorted(pad_end, jnp.arange(n_blocks) * MOE_BLOCK, side='right'),
                               N_EXPERTS - 1)
    x_pad = jnp.concatenate([xt, jnp.zeros((1, D), xt.dtype)], axis=0)

    def expert_block(args):
        rows, gate, e = args
        h = x_pad[rows] @ w_gu[e] + b_gu[e]
        y = _clamped_swiglu(h.astype(jnp.float32)) @ w_dn[e] + b_dn[e]
        return y * gate[:, None]

    y_rows = lax.map(expert_block, (row_token.reshape(n_blocks, MOE_BLOCK),
                                    row_gate.reshape(n_blocks, MOE_BLOCK), block_expert))
    out = jnp.zeros((T + 1, D), y_rows.dtype).at[row_token].add(y_rows.reshape(n_rows, D))[:T]
    return out.reshape(B, S, D).astype(x.dtype)


def setup_inputs(seed: int = 0) -> dict:
    key = jax.random.key(seed)
    keys = iter(jax.random.split(key, 64))

    def nrm(shape, scale):
        return jax.random.normal(next(keys), shape, jnp.float32) * scale

    def near_one(shape):
        return 1.0 + nrm(shape, 0.02)

    D = D_MODEL
    return {
        'x': nrm((BATCH, SEQ, D), 1.0),
        'mem': nrm((BATCH, MEM_LEN, D), 1.0),
        'rel_table': nrm((REL_BUCKETS, NSA_HEADS), 0.5),
        'nsa_w_in': nrm((N_NSA, D, NSA_COLS), D ** -0.5),
        'nsa_gate_b': nrm((N_NSA, 3 * NSA_HEADS), 0.1),
        'nsa_cmp_pe': nrm((N_NSA, 2, CMP_BLOCK, HEAD_DIM), 0.1),
        'nsa_cmp_w1': nrm((N_NSA, 2, CMP_BLOCK * HEAD_DIM, CMP_HIDDEN), (CMP_BLOCK * HEAD_DIM) ** -0.5),
        'nsa_cmp_w2': nrm((N_NSA, 2, CMP_HIDDEN, HEAD_DIM), CMP_HIDDEN ** -0.5),
        'gla_w_in': nrm((N_GLA, D, GLA_COLS), D ** -0.5),
        'gla_w_a2': nrm((N_GLA, GLA_GATE_RANK, GLA_HEADS * GLA_DK), GLA_GATE_RANK ** -0.5),
        'gla_b_a': nrm((N_GLA, GLA_HEADS * GLA_DK), 0.1),
        'gla_b_og': nrm((N_GLA, TOK_WIDTH), 0.02),
        'gla_norm_w': near_one((N_GLA, GLA_DV)),
        'rwkv_w_in': nrm((N_RWKV, D, RWKV_COLS), D ** -0.5),
        'rwkv_mu': jax.random.uniform(next(keys), (N_RWKV, RWKV_TOK_COLS), jnp.float32),
        'rwkv_w0': nrm((N_RWKV, TOK_WIDTH), 0.5),
        'rwkv_w2': nrm((N_RWKV, RWKV_DECAY_LORA, TOK_WIDTH), 0.1),
        'rwkv_a0': nrm((N_RWKV, TOK_WIDTH), 0.1),
        'rwkv_a2': nrm((N_RWKV, RWKV_AAA_LORA, TOK_WIDTH), 0.1),
        'rwkv_g2': nrm((N_RWKV, RWKV_GATE_LORA, TOK_WIDTH), RWKV_GATE_LORA ** -0.5),
        'rwkv_k_k': near_one((N_RWKV, TOK_WIDTH)),
        'rwkv_k_a': near_one((N_RWKV, TOK_WIDTH)),
        'rwkv_r_k': nrm((N_RWKV, RWKV_HEADS, HEAD_DIM), 0.1),
        'rwkv_ln_w': near_one((N_RWKV, TOK_WIDTH)),
        'rwkv_ln_b': nrm((N_RWKV, TOK_WIDTH), 0.02),
        'mem_w_kv': nrm((DEPTH, D, 2 * MEM_WIDTH), D ** -0.5),
        'w_out': nrm((DEPTH, MIX_WIDTH, D), MIX_WIDTH ** -0.5 * DEEP_BETA),
        'ln1_g': near_one((DEPTH, D)),
        'ln1_b': nrm((DEPTH, D), 0.02),
        'ln2_g': near_one((DEPTH, D)),
        'ln2_b': nrm((DEPTH, D), 0.02),
        'router_w': nrm((DEPTH, D, N_EXPERTS), D ** -0.5),
        'router_b': nrm((DEPTH, N_EXPERTS), 0.01),
        'exp_w_gu': nrm((DEPTH, N_EXPERTS, D, 2 * D_FF), D ** -0.5),
        'exp_b_gu': nrm((DEPTH, N_EXPERTS, 2 * D_FF), 0.01),
        'exp_w_dn': nrm((DEPTH, N_EXPERTS, D_FF, D), D_FF ** -0.5 * DEEP_BETA),
        'exp_b_dn': nrm((DEPTH, N_EXPERTS, D), 0.01),
    }


def reference(x, mem, rel_table, nsa_w_in, nsa_gate_b, nsa_cmp_pe, nsa_cmp_w1, nsa_cmp_w2,
              gla_w_in, gla_w_a2, gla_b_a, gla_b_og, gla_norm_w,
              rwkv_w_in, rwkv_mu, rwkv_w0, rwkv_w2, rwkv_a0, rwkv_a2, rwkv_g2, rwkv_k_k, rwkv_k_a,
              rwkv_r_k, rwkv_ln_w, rwkv_ln_b,
              mem_w_kv, w_out, ln1_g, ln1_b, ln2_g, ln2_b,
              router_w, router_b, exp_w_gu, exp_b_gu, exp_w_dn, exp_b_dn):
    for i in range(DEPTH):
        kind, j = i % N_MIXERS, i // N_MIXERS
        if kind == 0:
            h = x @ nsa_w_in[j]
            y = _nsa_mixer(h[..., :-MEM_WIDTH], rel_table, nsa_gate_b[j], nsa_cmp_pe[j],
                           nsa_cmp_w1[j], nsa_cmp_w2[j])
        elif kind == 1:
            h = x @ gla_w_in[j]
            y = _gla_mixer(h[..., :-MEM_WIDTH], gla_w_a2[j], gla_b_a[j], gla_b_og[j], gla_norm_w[j])
        else:
            h = x @ rwkv_w_in[j]
            y = _rwkv7_mixer(h[..., :-MEM_WIDTH], rwkv_mu[j], rwkv_w0[j], rwkv_w2[j], rwkv_a0[j],
                             rwkv_a2[j], rwkv_g2[j], rwkv_k_k[j], rwkv_k_a[j], rwkv_r_k[j],
                             rwkv_ln_w[j], rwkv_ln_b[j])
        y_mem = _memory_attention(h[..., -MEM_WIDTH:], mem, mem_w_kv[i])
        mix = jnp.concatenate([y.astype(x.dtype), y_mem.astype(x.dtype)], axis=-1) @ w_out[i]
        x = _layer_norm(DEEP_ALPHA * x + mix, ln1_g[i], ln1_b[i])
        ffn = _moe(x, router_w[i], router_b[i], exp_w_gu[i], exp_b_gu[i], exp_w_dn[i], exp_b_dn[i])
        x = _layer_norm(DEEP_ALPHA * x + ffn, ln2_g[i], ln2_b[i])
    return x
```

```python
import contextlib
import numpy as np
import concourse.bass as bass
import concourse.mybir as mybir
from concourse.bass_utils import run_bass_kernel_spmd

F32 = mybir.dt.float32
BF16 = mybir.dt.bfloat16
ALU = mybir.AluOpType
AF = mybir.ActivationFunctionType
AX = mybir.AxisListType

D = 1024
S = 4096
NCORES = 8
DEPTH = 4
MEMW = 256
TOKW = 768
ALPHA = (2 * DEPTH) ** 0.25
LN_EPS = 1e-5
NSA_COLS = 2596
GLA_COLS = 2576
RWKV_COLS = 2816
NE = 32


class Op:
    __slots__ = ("eng", "fn", "deps", "need_inc", "val", "is_dma", "dsem", "dval", "coll")

    def __init__(self, eng, fn, is_dma):
        self.eng, self.fn, self.is_dma = eng, fn, is_dma
        self.deps, self.need_inc, self.val = [], False, 0
        self.dsem, self.dval = None, 0
        self.coll = False


class Tl:
    def __init__(self, t):
        self.t = t

    def __getitem__(self, idx):
        return self.t[idx]


class KB:
    ENGS = ("pe", "dve", "act", "pool", "sp")
    KD = 8

    def __init__(self, nc):
        self.nc = nc
        self.es = contextlib.ExitStack()
        self.e = {"pe": nc.tensor, "dve": nc.vector, "act": nc.scalar, "pool": nc.gpsimd, "sp": nc.sync}
        self.ops = []
        self.last = {e: None for e in self.ENGS}
        self.res = {}
        self.pending = {e: [] for e in self.ENGS}
        self.nd = {e: 0 for e in self.ENGS}
        self.slot_last = {}
        self.csem = {e: nc.alloc_semaphore(name="c_" + e) for e in self.ENGS}
        self.dsems = {}
        for q in ("sp", "pool", "act"):
            for s in range(self.KD):
                self.dsems[(q, s)] = nc.alloc_semaphore(name="d_%s%d" % (q, s))
        self.uid = 0
        self.colls = {}
        self._clear_sems()
        nc.all_engine_barrier()

    def _clear_sems(self):
        for h in list(self.csem.values()) + list(self.dsems.values()):
            self.nc.gpsimd.sem_clear(h)

    def name(self, p):
        self.uid += 1
        return "%s_%d" % (p, self.uid)

    def sb(self, st, shape, dt, name="sb"):
        return Tl(st.enter_context(self.nc.sbuf_tensor(self.name(name), list(shape), dt)))

    def ps(self, st, shape, dt, name="ps"):
        return Tl(st.enter_context(self.nc.psum_tensor(self.name(name), list(shape), dt)))

    def dram(self, shape, dt, name="scr"):
        return self.nc.dram_tensor(self.name(name), list(shape), dt)

    def coll(self, name, kind, ins, outs, r=(), w=()):
        sem = self.nc.alloc_semaphore(name="cc_" + name)
        self.nc.gpsimd.sem_clear(sem)
        self.dsems[("cc", name)] = sem
        o = self.op("pool", lambda E: E.collective_compute(kind, ALU.bypass, replica_groups=[list(range(NCORES))], ins=ins, outs=outs), r=r, w=w, dma=True, coll=("cc", name))
        self.colls[name] = o
        return o

    def need(self, name):
        o = self.colls[name]
        for e in self.ENGS:
            self.pending[e] = list(self.pending[e]) + [o]

    def op(self, eng, fn, r=(), w=(), dma=False, coll=None):
        o = Op(eng, fn, dma)
        deps = list(self.pending[eng])
        self.pending[eng] = []
        for k in r:
            st = self.res.get(k)
            if st:
                deps += st[0]
        for k in w:
            st = self.res.get(k)
            if st:
                deps += st[0]
                deps += st[1]
        seen = set()
        for d in deps:
            if d is o or id(d) in seen:
                continue
            if (not d.is_dma) and d.eng == eng and eng == "pe":
                continue
            seen.add(id(d))
            o.deps.append(d)
            if not d.is_dma:
                d.need_inc = True
        if coll is not None:
            o.coll = True
            o.dsem = coll
            o.dval = 1
        elif dma:
            slot = self.nd[eng] % self.KD
            o.dsem = (eng, slot)
            o.dval = 16 * (self.nd[eng] // self.KD + 1)
            prev = self.slot_last.get((eng, slot))
            if prev is not None and id(prev) not in seen:
                o.deps.append(prev)
            self.slot_last[(eng, slot)] = o
            self.nd[eng] += 1
        for k in r:
            if k in w:
                continue
            st = self.res.setdefault(k, ([], []))
            if not dma:
                st[1][:] = [x for x in st[1] if x.is_dma or x.eng != eng]
            st[1].append(o)
        for k in w:
            self.res[k] = ([o], [])
        self.ops.append(o)
        if not dma:
            self.last[eng] = o
        return o

    def dma(self, q, out, in_, r=(), w=(), **kw):
        return self.op(q, lambda E: E.dma_start(out=out, in_=in_, **kw), r=r, w=w, dma=True)

    def barrier(self):
        targets = [o for o in self.last.values() if o is not None and not o.is_dma]
        targets += list(self.slot_last.values())
        for e in self.ENGS:
            self.pending[e] = list(self.pending[e]) + targets
        self.res = {}

    def emit(self):
        self.barrier()
        final = self.pending["sp"]
        for d in final:
            if not d.is_dma:
                d.need_inc = True
        cnt = {e: 0 for e in self.ENGS}
        for o in self.ops:
            if (not o.is_dma) and o.need_inc:
                cnt[o.eng] += 1
                o.val = cnt[o.eng]
        waited = {}

        def do_wait(engname, E, d):
            if d.is_dma:
                key, val, sem = ("d",) + d.dsem, d.dval, self.dsems[d.dsem]
            else:
                key, val, sem = ("c", d.eng), d.val, self.csem[d.eng]
            if waited.get((engname, key), 0) >= val:
                return
            waited[(engname, key)] = val
            E.wait_ge(sem, val)

        for o in self.ops:
            E = self.e[o.eng]
            for d in o.deps:
                do_wait(o.eng, E, d)
            ins = o.fn(E)
            if o.coll:
                ins.then_inc(self.dsems[o.dsem])
            elif o.is_dma:
                ins.then_inc(self.dsems[o.dsem], 16)
            elif o.need_inc:
                ins.then_inc(self.csem[o.eng], 1)
        for d in final:
            do_wait("sp", self.e["sp"], d)
        self.nc.all_engine_barrier()
        self._clear_sems()
        self.nc.all_engine_barrier()
        self.es.close()


def evac(kb, i, out, in_, r, w):
    if i % 2 == 0:
        kb.op("dve", lambda E: E.tensor_copy(out=out, in_=in_), r=r, w=w)
    else:
        kb.op("act", lambda E: E.copy(out=out, in_=in_), r=r, w=w)


class Consts:
    def __init__(self, kb, ident_d):
        es = kb.es
        self.ident_f = kb.sb(es, [128, 128], F32, "identf")
        self.ident_b = kb.sb(es, [128, 128], BF16, "identb")
        kb.dma("sp", self.ident_f[:], ident_d, w=[self.ident_f])
        kb.op("dve", lambda E: E.tensor_copy(out=self.ident_b[:], in_=self.ident_f[:]), r=[self.ident_f], w=[self.ident_b])


def load_xT(kb, C, x_d, t0, nsub, xin, xb, xT, pst, ncolchunks=8, q="sp", off=0):
    cols = ncolchunks * 128
    kb.dma(q, xin[:, 0:nsub, 0:cols], x_d[t0:t0 + nsub * 128, 0:cols].rearrange("(s p) c -> p s c", p=128), w=[xin])
    kb.op("act", lambda E: E.copy(out=xb[:, 0:nsub, 0:cols], in_=xin[:, 0:nsub, 0:cols]), r=[xin], w=[xb])
    for kc in range(ncolchunks):
        p = pst[kc % len(pst)]
        for s in range(nsub):
            kb.op("pe", lambda E, s=s, kc=kc, p=p: E.transpose(out=p[:, s * 128:(s + 1) * 128], in_=xb[:, s, kc * 128:(kc + 1) * 128], identity=C.ident_b[:]),
                  r=[xb, C.ident_b], w=[p])
        evac(kb, kc, xT[:, kc, off:off + nsub * 128], p[:, 0:nsub * 128], r=[p], w=[xT])


def stage_proj(kb, C, x_d, w_d, ncols, hF_d, hT_d):
    with contextlib.ExitStack() as st:
        W = kb.sb(st, [128, 8, ncols], BF16, "W")
        for kc in range(8):
            kb.dma("pool", W[:, kc, :], w_d[kc * 128:(kc + 1) * 128, :], w=[W])
        xin = [kb.sb(st, [128, 4, D], F32, "xin") for _ in range(2)]
        xb = [kb.sb(st, [128, 4, D], BF16, "xb") for _ in range(2)]
        xT = [kb.sb(st, [128, 8, 512], BF16, "xT") for _ in range(2)]
        pst = [kb.ps(st, [128, 1024], BF16, "pst") for _ in range(2)]
        pm = [kb.ps(st, [128, 512], F32, "pm") for _ in range(4)]
        oF = [kb.sb(st, [128, 512], F32, "oF") for _ in range(3)]
        oT = [kb.sb(st, [128, ncols], F32, "oT") for _ in range(2)]
        nfc = (ncols + 127) // 128
        ncb = (ncols + 511) // 512
        cnt = 0
        for tt in range(S // 512):
            b = tt % 2
            load_xT(kb, C, x_d, tt * 512, 4, xin[b], xb[b], xT[b], pst)
            for c in range(nfc):
                mc = min(128, ncols - c * 128)
                p = pm[cnt % 4]
                o = oF[cnt % 3]
                for kc in range(8):
                    kb.op("pe", lambda E, p=p, c=c, mc=mc, kc=kc, b=b: E.matmul(out=p[0:mc, :], lhsT=W[:, kc, c * 128:c * 128 + mc], rhs=xT[b][:, kc, :], start=(kc == 0), stop=(kc == 7)),
                          r=[W, xT[b]], w=[p])
                evac(kb, cnt, o[0:mc, :], p[0:mc, :], r=[p], w=[o])
                kb.dma("sp", hF_d[c * 128:c * 128 + mc, tt * 512:(tt + 1) * 512], o[0:mc, :], r=[o])
                cnt += 1
            for s in range(4):
                ot = oT[s % 2]
                for cb in range(ncb):
                    nn = min(512, ncols - cb * 512)
                    p = pm[cnt % 4]
                    for kc in range(8):
                        kb.op("pe", lambda E, p=p, cb=cb, nn=nn, kc=kc, b=b, s=s: E.matmul(out=p[:, 0:nn], lhsT=xT[b][:, kc, s * 128:(s + 1) * 128], rhs=W[:, kc, cb * 512:cb * 512 + nn], start=(kc == 0), stop=(kc == 7)),
                              r=[W, xT[b]], w=[p])
                    evac(kb, cnt, ot[:, cb * 512:cb * 512 + nn], p[:, 0:nn], r=[p], w=[ot])
                    cnt += 1
                t0 = tt * 512 + s * 128
                kb.dma("pool", hT_d[t0:t0 + 128, :], ot[:], r=[ot])
        kb.barrier()


class LNBufs:
    def __init__(self, kb, st, g_d, b_d):
        self.g = kb.sb(st, [128, D], F32, "lng")
        self.b = kb.sb(st, [128, D], F32, "lnb")
        kb.dma("sp", self.g[:], g_d.partition_broadcast(128), w=[self.g])
        kb.dma("sp", self.b[:], b_d.partition_broadcast(128), w=[self.b])
        self.stats = kb.sb(st, [128, 2, 6], F32, "lnst")
        self.mv = kb.sb(st, [128, 2], F32, "lnmv")
        self.acc = kb.sb(st, [128, 2], F32, "lnacc")
        self.rstd = kb.sb(st, [128, 1], F32, "lnrs")


def ln_tile(kb, L, z, zap, out, outap):
    kb.op("act", lambda E: E.activation(out=outap, in_=zap, func=AF.Identity, accum_out=L.acc[:, 0:1]), r=[z], w=[out, L.acc])
    kb.op("act", lambda E: E.activation(out=outap, in_=zap, func=AF.Square, accum_out=L.acc[:, 1:2]), r=[z], w=[out, L.acc])
    kb.op("act", lambda E: E.mul(out=L.mv[:], in_=L.acc[:], mul=1.0 / D), r=[L.acc], w=[L.mv])
    kb.op("dve", lambda E: E.scalar_tensor_tensor(out=L.rstd[:], in0=L.mv[:, 0:1], scalar=-1.0, in1=L.mv[:, 0:1], op0=ALU.mult, op1=ALU.mult), r=[L.mv], w=[L.rstd])
    kb.op("dve", lambda E: E.tensor_tensor(out=L.rstd[:], in0=L.rstd[:], in1=L.mv[:, 1:2], op=ALU.add), r=[L.mv, L.rstd], w=[L.rstd])
    kb.op("act", lambda E: E.activation(out=L.rstd[:], in_=L.rstd[:], func=AF.Sqrt, bias=L.epsb[:], scale=1.0), r=[L.rstd, L.epsb], w=[L.rstd])
    kb.op("dve", lambda E: E.reciprocal(out=L.rstd[:], in_=L.rstd[:]), r=[L.rstd], w=[L.rstd])
    kb.op("dve", lambda E: E.tensor_scalar(out=zap, in0=zap, scalar1=L.mv[:, 0:1], scalar2=L.rstd[:, 0:1], op0=ALU.subtract, op1=ALU.mult), r=[z, L.mv, L.rstd], w=[z])
    kb.op("dve", lambda E: E.tensor_tensor(out=zap, in0=zap, in1=L.g[:], op=ALU.mult), r=[z, L.g], w=[z])
    kb.op("dve", lambda E: E.tensor_tensor(out=outap, in0=zap, in1=L.b[:], op=ALU.add), r=[z, L.b], w=[out])


def make_eps(kb, st, L):
    L.epsb = kb.sb(st, [128, 1], F32, "eps")
    kb.op("dve", lambda E: E.memset(L.epsb[:], LN_EPS), w=[L.epsb])


def stage_memattn(kb, C, mem_d, wkv_d, hF_d, qc0, mix_d):
    with contextlib.ExitStack() as st:
        W = kb.sb(st, [128, 8, 512], BF16, "Wkv")
        for kc in range(8):
            kb.dma("pool", W[:, kc, :], wkv_d[kc * 128:(kc + 1) * 128, :], w=[W])
        xin = kb.sb(st, [128, 2, D], F32, "min")
        xb = kb.sb(st, [128, 2, D], BF16, "mb")
        memT = kb.sb(st, [128, 8, 256], BF16, "memT")
        pst = [kb.ps(st, [128, 1024], BF16, "pst") for _ in range(2)]
        load_xT(kb, C, mem_d, 0, 2, xin, xb, memT, pst)
        kT = kb.sb(st, [64, 4, 256], BF16, "kT")
        Vx = kb.sb(st, [128, 2, 4, 65], BF16, "Vx")
        kb.op("dve", lambda E: E.memset(Vx[:], 1.0), w=[Vx])
        pm = [kb.ps(st, [128, 512], F32, "pm") for _ in range(2)]
        for h in range(4):
            p = pm[h % 2]
            for kc in range(8):
                kb.op("pe", lambda E, p=p, h=h, kc=kc: E.matmul(out=p[0:64, 0:256], lhsT=W[:, kc, h * 64:(h + 1) * 64], rhs=memT[:, kc, :], start=(kc == 0), stop=(kc == 7)), r=[W, memT], w=[p])
            evac(kb, h, kT[:, h, :], p[0:64, 0:256], r=[p], w=[kT])
        for mc in range(2):
            p = pm[mc % 2]
            for kc in range(8):
                kb.op("pe", lambda E, p=p, mc=mc, kc=kc: E.matmul(out=p[:, 0:256], lhsT=memT[:, kc, mc * 128:(mc + 1) * 128], rhs=W[:, kc, 256:512], start=(kc == 0), stop=(kc == 7)), r=[W, memT], w=[p])
            kb.op("dve", lambda E, p=p, mc=mc: E.tensor_copy(out=Vx[:, mc, :, 0:64], in_=p[:, 0:256].rearrange("p (h d) -> p h d", d=64)), r=[p], w=[Vx])
        qm = [kb.sb(st, [64, 4, 512], BF16, "qm") for _ in range(2)]
        pT = [kb.sb(st, [128, 512], BF16, "pT") for _ in range(4)]
        po = [kb.ps(st, [128, 4, 65], F32, "po") for _ in range(2)]
        ym = [kb.sb(st, [128, 4, 256], F32, "ym") for _ in range(2)]
        rc = [kb.sb(st, [128, 4], F32, "rc") for _ in range(2)]
        cnt = 0
        for tt in range(S // 512):
            q = qm[tt % 2]
            y = ym[tt % 2]
            kb.dma("pool", q[:], hF_d[qc0:qc0 + 256, tt * 512:(tt + 1) * 512].rearrange("(h d) t -> d h t", d=64), w=[q])
            for h in range(4):
                pts = []
                for mc in range(2):
                    p = pm[cnt % 2]
                    t = pT[cnt % 4]
                    cnt += 1
                    kb.op("pe", lambda E, p=p, h=h, mc=mc, q=q: E.matmul(out=p[:], lhsT=kT[:, h, mc * 128:(mc + 1) * 128], rhs=q[:, h, :], start=True, stop=True), r=[kT, q], w=[p])
                    kb.op("act", lambda E, p=p, t=t: E.activation(out=t[:], in_=p[:], func=AF.Exp, scale=0.125), r=[p], w=[t])
                    pts.append(t)
                o = po[h % 2]
                for qs in range(4):
                    for mc in range(2):
                        kb.op("pe", lambda E, o=o, qs=qs, mc=mc, h=h, t=pts[mc]: E.matmul(out=o[:, qs, :], lhsT=t[:, qs * 128:(qs + 1) * 128], rhs=Vx[:, mc, h, :], start=(mc == 0), stop=(mc == 1)), r=[pts[mc], Vx], w=[o])
                r_ = rc[h % 2]
                kb.op("dve", lambda E, o=o, r_=r_: E.reciprocal(out=r_[:], in_=o[:, :, 64]), r=[o], w=[r_])
                for qs in range(4):
                    kb.op("dve", lambda E, o=o, r_=r_, qs=qs, h=h, y=y: E.tensor_scalar(out=y[:, qs, h * 64:(h + 1) * 64], in0=o[:, qs, 0:64], scalar1=r_[:, qs:qs + 1], scalar2=None, op0=ALU.mult), r=[o, r_], w=[y])
            kb.dma("sp", mix_d[tt * 512:(tt + 1) * 512, TOKW:D].rearrange("(s p) c -> p s c", p=128), y[:], r=[y])
        kb.barrier()


def stage_outproj_ln(kb, C, mix_d, wout_d, xres_d, g_d, b_d, xout_d):
    with contextlib.ExitStack() as st:
        W = kb.sb(st, [128, 8, D], BF16, "Wo")
        for kc in range(8):
            kb.dma("pool", W[:, kc, :], wout_d[kc * 128:(kc + 1) * 128, :], w=[W])
        L = LNBufs(kb, st, g_d, b_d)
        make_eps(kb, st, L)
        xin = [kb.sb(st, [128, 4, D], F32, "xin") for _ in range(2)]
        xb = [kb.sb(st, [128, 4, D], BF16, "xb") for _ in range(2)]
        xT = [kb.sb(st, [128, 8, 512], BF16, "xT") for _ in range(2)]
        xr = [kb.sb(st, [128, 4, D], F32, "xr") for _ in range(2)]
        z = [kb.sb(st, [128, D], F32, "z") for _ in range(2)]
        zo = [kb.sb(st, [128, D], F32, "zo") for _ in range(2)]
        pst = [kb.ps(st, [128, 1024], BF16, "pst") for _ in range(2)]
        pm = [kb.ps(st, [128, 512], F32, "pm") for _ in range(4)]
        cnt = 0
        for tt in range(S // 512):
            b = tt % 2
            load_xT(kb, C, mix_d, tt * 512, 4, xin[b], xb[b], xT[b], pst)
            kb.dma("pool", xr[b][:], xres_d[tt * 512:(tt + 1) * 512, :].rearrange("(s p) c -> p s c", p=128), w=[xr[b]])
            for s in range(4):
                zz = z[s % 2]
                oo = zo[s % 2]
                for hh in range(2):
                    p = pm[cnt % 4]
                    cnt += 1
                    for kc in range(8):
                        kb.op("pe", lambda E, p=p, kc=kc, b=b, s=s, hh=hh: E.matmul(out=p[:], lhsT=xT[b][:, kc, s * 128:(s + 1) * 128], rhs=W[:, kc, hh * 512:(hh + 1) * 512], start=(kc == 0), stop=(kc == 7)), r=[xT[b], W], w=[p])
                    kb.op("dve", lambda E, p=p, zz=zz, b=b, s=s, hh=hh: E.scalar_tensor_tensor(out=zz[:, hh * 512:(hh + 1) * 512], in0=xr[b][:, s, hh * 512:(hh + 1) * 512], scalar=ALPHA, in1=p[:], op0=ALU.mult, op1=ALU.add), r=[xr[b], p], w=[zz])
                ln_tile(kb, L, zz, zz[:], oo, oo[:])
                t0 = tt * 512 + s * 128
                kb.dma("sp", xout_d[t0:t0 + 128, :], oo[:], r=[oo])
        kb.barrier()


def stage_moe(kb, C, x1_d, wr_d, br_d, wg_d, wd_d, bgu_d, bdn_d, g_d, b_d, xout_d):
    with contextlib.ExitStack() as st:
        Wr = kb.sb(st, [128, 8, NE], F32, "Wr")
        kb.dma("sp", Wr[:], wr_d.rearrange("(kc p) e -> p kc e", p=128), w=[Wr])
        brb = kb.sb(st, [128, NE], F32, "brb")
        kb.dma("sp", brb[:], br_d.partition_broadcast(128), w=[brb])
        bdn = kb.sb(st, [NE, D], F32, "bdn")
        kb.dma("sp", bdn[:], bdn_d, w=[bdn])
        bguT = kb.sb(st, [128, 16, NE], F32, "bguT")
        with contextlib.ExitStack() as st0:
            braw = kb.sb(st0, [NE, 2 * D], F32, "braw")
            kb.dma("sp", braw[:], bgu_d, w=[braw])
            pb = kb.ps(st0, [128, 16, NE], F32, "pb")
            for c in range(16):
                kb.op("pe", lambda E, c=c: E.transpose(out=pb[:, c, :], in_=braw[:, c * 128:(c + 1) * 128], identity=C.ident_f[0:NE, 0:NE]), r=[braw, C.ident_f], w=[pb])
            kb.op("dve", lambda E: E.tensor_copy(out=bguT[:], in_=pb[:]), r=[pb], w=[bguT])
            kb.barrier()
        L = LNBufs(kb, st, g_d, b_d)
        make_eps(kb, st, L)
        Wg = [kb.sb(st, [128, 8, 2 * D], BF16, "Wg") for _ in range(2)]
        Wd = [kb.sb(st, [128, 8, D], BF16, "Wd") for _ in range(2)]
        xT = kb.sb(st, [128, 8, 1024], BF16, "xT")
        yacc = [kb.sb(st, [128, D], F32, "yacc") for _ in range(8)]
        gates = kb.sb(st, [128, 8, NE], F32, "gates")
        gT = kb.sb(st, [NE, 8, 128], F32, "gT")
        ecnt = 0
        for sup in range(S // 1024):
            T0 = sup * 1024
            with contextlib.ExitStack() as s1:
                xin = kb.sb(s1, [128, 4, D], F32, "xin")
                xb = kb.sb(s1, [128, 4, D], BF16, "xb")
                xTf = [kb.sb(s1, [128, 8, 128], F32, "xTf") for _ in range(2)]
                pst = [kb.ps(s1, [128, 1024], BF16, "pst") for _ in range(2)]
                ptf = [kb.ps(s1, [128, 4, 128], F32, "ptf") for _ in range(2)]
                plg = [kb.ps(s1, [128, 512], F32, "plg") for _ in range(2)]
                sm = [dict(lg=kb.sb(s1, [128, NE], F32, "lg"), m8=kb.sb(s1, [128, 8], F32, "m8"), nm=kb.sb(s1, [128, 1], F32, "nm"),
                           mk=kb.sb(s1, [128, NE], F32, "mk"), ex=kb.sb(s1, [128, NE], F32, "ex"), ss=kb.sb(s1, [128, 1], F32, "ss")) for _ in range(2)]
                for half in range(2):
                    load_xT(kb, C, x1_d, T0 + half * 512, 4, xin, xb, xT, pst, off=half * 512)
                    for s_ in range(4):
                        tile = half * 4 + s_
                        xf = xTf[tile % 2]
                        for kc in range(8):
                            p = ptf[kc // 4]
                            kb.op("pe", lambda E, p=p, kc=kc, s_=s_: E.transpose(out=p[:, kc % 4, :], in_=xin[:, s_, kc * 128:(kc + 1) * 128], identity=C.ident_f[:]), r=[xin, C.ident_f], w=[p])
                            if kc % 4 == 3:
                                evac(kb, kc // 4, xf[:, kc - 3:kc + 1, :], p[:], r=[p], w=[xf])
                        pl_ = plg[tile % 2]
                        for kc in range(8):
                            kb.op("pe", lambda E, pl_=pl_, kc=kc, xf=xf: E.matmul(out=pl_[:, 0:NE], lhsT=xf[:, kc, :], rhs=Wr[:, kc, :], start=(kc == 0), stop=(kc == 7)), r=[xf, Wr], w=[pl_])
                        m = sm[tile % 2]
                        kb.op("dve", lambda E, m=m, pl_=pl_: E.tensor_tensor(out=m["lg"][:], in0=pl_[:, 0:NE], in1=brb[:], op=ALU.add), r=[pl_, brb], w=[m["lg"]])
                        kb.op("dve", lambda E, m=m: E.max(out=m["m8"][:], in_=m["lg"][:]), r=[m["lg"]], w=[m["m8"]])
                        kb.op("dve", lambda E, m=m: E.tensor_scalar(out=m["mk"][:], in0=m["lg"][:], scalar1=m["m8"][:, 3:4], scalar2=None, op0=ALU.is_ge), r=[m["lg"], m["m8"]], w=[m["mk"]])
                        kb.op("dve", lambda E, m=m: E.tensor_scalar(out=m["nm"][:], in0=m["m8"][:, 0:1], scalar1=-1.0, scalar2=None, op0=ALU.mult), r=[m["m8"]], w=[m["nm"]])
                        kb.op("act", lambda E, m=m: E.activation(out=m["ex"][:], in_=m["lg"][:], func=AF.Exp, bias=m["nm"][:], scale=1.0), r=[m["lg"], m["nm"]], w=[m["ex"]])
                        kb.op("dve", lambda E, m=m: E.tensor_tensor(out=m["ex"][:], in0=m["ex"][:], in1=m["mk"][:], op=ALU.mult), r=[m["ex"], m["mk"]], w=[m["ex"]])
                        kb.op("dve", lambda E, m=m: E.tensor_reduce(out=m["ss"][:], in_=m["ex"][:], axis=AX.X, op=ALU.add), r=[m["ex"]], w=[m["ss"]])
                        kb.op("dve", lambda E, m=m: E.reciprocal(out=m["ss"][:], in_=m["ss"][:]), r=[m["ss"]], w=[m["ss"]])
                        kb.op("dve", lambda E, m=m, tile=tile: E.tensor_scalar(out=gates[:, tile, :], in0=m["ex"][:], scalar1=m["ss"][:, 0:1], scalar2=None, op0=ALU.mult), r=[m["ex"], m["ss"]], w=[gates])
                        pg_ = plg[tile % 2]
                        kb.op("pe", lambda E, pg_=pg_, tile=tile: E.transpose(out=pg_[0:NE, 0:128], in_=gates[:, tile, :], identity=C.ident_f[:]), r=[gates, C.ident_f], w=[pg_])
                        kb.op("act", lambda E, pg_=pg_, tile=tile: E.copy(out=gT[:, tile, :], in_=pg_[0:NE, 0:128]), r=[pg_], w=[gT])
                for tile in range(8):
                    for hh in range(2):
                        p = plg[(tile * 2 + hh) % 2]
                        kb.op("pe", lambda E, p=p, tile=tile, hh=hh: E.matmul(out=p[:], lhsT=gT[:, tile, :], rhs=bdn[:, hh * 512:(hh + 1) * 512], start=True, stop=True), r=[gT, bdn], w=[p])
                        evac(kb, hh, yacc[tile][:, hh * 512:(hh + 1) * 512], p[:], r=[p], w=[yacc[tile]])
                kb.barrier()
            with contextlib.ExitStack() as s2:
                actT = [kb.sb(s2, [128, 8, 512], BF16, "actT") for _ in range(2)]
                glu = [kb.sb(s2, [128, 512], F32, "glu") for _ in range(2)]
                sig = [kb.sb(s2, [128, 512], F32, "sig") for _ in range(2)]
                lin = [kb.sb(s2, [128, 512], F32, "lin") for _ in range(2)]
                pg = [kb.ps(s2, [128, 512], F32, "pg") for _ in range(2)]
                pl = [kb.ps(s2, [128, 512], F32, "pl") for _ in range(2)]
                py = [kb.ps(s2, [128, 512], F32, "py") for _ in range(2)]
                cnt = 0
                ycnt = 0
                for e in range(NE):
                    b = ecnt % 2
                    ecnt += 1
                    for hk in range(2):
                        kb.dma("sp", Wg[b][:, hk * 4:(hk + 1) * 4, :], wg_d[e, hk * 512:(hk + 1) * 512, :].rearrange("(kc p) n -> p kc n", p=128), w=[Wg[b]])
                    kb.dma("sp", Wd[b][:], wd_d[e].rearrange("(kc p) n -> p kc n", p=128), w=[Wd[b]])
                    for t5 in range(2):
                        aT = actT[(e * 2 + t5) % 2]
                        for fc in range(8):
                            i = cnt % 2
                            cnt += 1
                            for kc in range(8):
                                kb.op("pe", lambda E, i=i, kc=kc, fc=fc, b=b, t5=t5: E.matmul(out=pg[i][:], lhsT=Wg[b][:, kc, fc * 128:(fc + 1) * 128], rhs=xT[:, kc, t5 * 512:(t5 + 1) * 512], start=(kc == 0), stop=(kc == 7)), r=[Wg[b], xT], w=[pg[i]])
                            for kc in range(8):
                                kb.op("pe", lambda E, i=i, kc=kc, fc=fc, b=b, t5=t5: E.matmul(out=pl[i][:], lhsT=Wg[b][:, kc, D + fc * 128:D + (fc + 1) * 128], rhs=xT[:, kc, t5 * 512:(t5 + 1) * 512], start=(kc == 0), stop=(kc == 7)), r=[Wg[b], xT], w=[pl[i]])
                            kb.op("dve", lambda E, i=i, fc=fc, e=e: E.tensor_scalar(out=glu[i][:], in0=pg[i][:], scalar1=bguT[:, fc, e:e + 1], scalar2=7.0, op0=ALU.add, op1=ALU.min), r=[pg[i], bguT], w=[glu[i]])
                            kb.op("act", lambda E, i=i: E.activation(out=sig[i][:], in_=glu[i][:], func=AF.Sigmoid, scale=1.702), r=[glu[i]], w=[sig[i]])
                            kb.op("dve", lambda E, i=i, fc=fc, e=e: E.tensor_scalar(out=lin[i][:], in0=pl[i][:], scalar1=bguT[:, 8 + fc, e:e + 1], scalar2=7.0, op0=ALU.add, op1=ALU.min), r=[pl[i], bguT], w=[lin[i]])
                            kb.op("dve", lambda E, i=i: E.tensor_scalar(out=lin[i][:], in0=lin[i][:], scalar1=-7.0, scalar2=1.0, op0=ALU.max, op1=ALU.add), r=[lin[i]], w=[lin[i]])
                            kb.op("dve", lambda E, i=i: E.tensor_tensor(out=lin[i][:], in0=lin[i][:], in1=glu[i][:], op=ALU.mult), r=[lin[i], glu[i]], w=[lin[i]])
                            kb.op("dve", lambda E, i=i, fc=fc, aT=aT: E.tensor_tensor(out=aT[:, fc, :], in0=lin[i][:], in1=sig[i][:], op=ALU.mult), r=[lin[i], sig[i]], w=[aT])
                        for sub in range(4):
                            tile = t5 * 4 + sub
                            for hh in range(2):
                                p = py[ycnt % 2]
                                ycnt += 1
                                for fc in range(8):
                                    kb.op("pe", lambda E, p=p, fc=fc, sub=sub, hh=hh, b=b, aT=aT: E.matmul(out=p[:], lhsT=aT[:, fc, sub * 128:(sub + 1) * 128], rhs=Wd[b][:, fc, hh * 512:(hh + 1) * 512], start=(fc == 0), stop=(fc == 7)), r=[aT, Wd[b]], w=[p])
                                kb.op("dve", lambda E, p=p, tile=tile, hh=hh, e=e: E.scalar_tensor_tensor(out=yacc[tile][:, hh * 512:(hh + 1) * 512], in0=p[:], scalar=gates[:, tile, e:e + 1], in1=yacc[tile][:, hh * 512:(hh + 1) * 512], op0=ALU.mult, op1=ALU.add), r=[p, gates, yacc[tile]], w=[yacc[tile]])
                kb.barrier()
            with contextlib.ExitStack() as s3:
                xr = [kb.sb(s3, [128, D], F32, "xr") for _ in range(2)]
                zo = [kb.sb(s3, [128, D], F32, "zo") for _ in range(2)]
                for tile in range(8):
                    t0 = T0 + tile * 128
                    x_ = xr[tile % 2]
                    o_ = zo[tile % 2]
                    kb.dma("pool", x_[:], x1_d[t0:t0 + 128, :], w=[x_])
                    kb.op("dve", lambda E, x_=x_, tile=tile: E.scalar_tensor_tensor(out=yacc[tile][:], in0=x_[:], scalar=ALPHA, in1=yacc[tile][:], op0=ALU.mult, op1=ALU.add), r=[x_, yacc[tile]], w=[yacc[tile]])
                    ln_tile(kb, L, yacc[tile], yacc[tile][:], o_, o_[:])
                    kb.dma("sp", xout_d[t0:t0 + 128, :], o_[:], r=[o_])
                kb.barrier()


GLA_H, GLA_DK, GLA_DV = 4, 96, 192


def gla_consts_np():
    s = np.arange(128)[:, None]
    c = np.arange(128)[None, :]
    same = (s // 64) == (c // 64)
    tri_incl = np.where(same & (s <= c), -1.0 / 16.0, 0.0)
    tri_up = np.where(same & (s > c), -1.0 / 16.0, 0.0)
    m01 = np.where(same & (s <= c), 1.0, 0.0)
    return np.stack([tri_incl, tri_up, m01]).astype(np.float32)


def stage_gla(kb, C, hF_d, hT_d, wa2_d, ba_d, bog_d, nw_d, gc_d, mix_d):
    H, DK, DV = GLA_H, GLA_DK, GLA_DV
    with contextlib.ExitStack() as st:
        gc = kb.sb(st, [128, 3, 128], F32, "gc")
        kb.dma("sp", gc[:], gc_d.rearrange("a s c -> s a c"), w=[gc])
        wa2 = kb.sb(st, [16, 384], F32, "wa2")
        kb.dma("sp", wa2[:], wa2_d, w=[wa2])
        bab = kb.sb(st, [128, 384], F32, "bab")
        kb.dma("sp", bab[:], ba_d.partition_broadcast(128), w=[bab])
        bogb = kb.sb(st, [128, TOKW], F32, "bogb")
        kb.dma("sp", bogb[:], bog_d.partition_broadcast(128), w=[bogb])
        nwb = kb.sb(st, [128, DV], F32, "nwb")
        kb.dma("sp", nwb[:], nw_d.partition_broadcast(128), w=[nwb])
        epsb = kb.sb(st, [128, 1], F32, "eps")
        kb.op("dve", lambda E: E.memset(epsb[:], LN_EPS), w=[epsb])
        Sf = [kb.sb(st, [DK, DV], F32, "Sf") for _ in range(H)]
        Sb = [[kb.sb(st, [DK, DV], BF16, "Sb") for _ in range(2)] for _ in range(H)]
        for h in range(H):
            kb.op("dve", lambda E, h=h: E.memset(Sf[h][:], 0.0), w=[Sf[h]])
            kb.op("dve", lambda E, h=h: E.memset(Sb[h][1][:], 0.0), w=[Sb[h][1]])
        NB = 2
        tm = [kb.sb(st, [128, 1936], F32, "tm") for _ in range(NB)]
        qkT = [kb.sb(st, [DK, 2, H, 128], F32, "qkT") for _ in range(NB)]
        alrT = [kb.sb(st, [16, 128], F32, "alrT") for _ in range(NB)]
        G = [kb.sb(st, [128, 384], F32, "G") for _ in range(NB)]
        vb = [kb.sb(st, [128, TOKW], BF16, "vb") for _ in range(NB)]
        sg = [kb.sb(st, [128, TOKW], F32, "sg") for _ in range(NB)]
        kdec = [kb.sb(st, [128, 384], BF16, "kdec") for _ in range(NB)]
        edec = [kb.sb(st, [128, 384], F32, "edec") for _ in range(NB)]
        yt = [kb.sb(st, [128, TOKW], F32, "yt") for _ in range(NB)]
        eb = [kb.sb(st, [DK, 128], F32, "eb") for _ in range(4)]
        enb = [kb.sb(st, [DK, 128], F32, "enb") for _ in range(4)]
        qd = [kb.sb(st, [DK, 128], BF16, "qd") for _ in range(4)]
        kd = [kb.sb(st, [DK, 128], BF16, "kd") for _ in range(4)]
        qd0 = [kb.sb(st, [DK, 128], BF16, "qd0") for _ in range(4)]
        qd1 = [kb.sb(st, [DK, 128], BF16, "qd1") for _ in range(4)]
        for i in range(4):
            kb.op("dve", lambda E, i=i: E.memset(qd0[i][:], 0.0), w=[qd0[i]])
            kb.op("dve", lambda E, i=i: E.memset(qd1[i][:], 0.0), w=[qd1[i]])
        AT = [kb.sb(st, [128, 128], BF16, "AT") for _ in range(2)]
        ebl = [kb.sb(st, [DK, 2], F32, "ebl") for _ in range(4)]
        ssq = [kb.sb(st, [128, 1], F32, "ssq") for _ in range(2)]
        rstd = [kb.sb(st, [128, 1], F32, "rstd") for _ in range(2)]
        junk = kb.sb(st, [128, DV], F32, "junk")
        otmp = [kb.sb(st, [128, DV], F32, "otmp") for _ in range(2)]
        pz = kb.ps(st, [128, 512], F32, "pz")
        pdec = kb.ps(st, [128, 512], F32, "pdec")
        pbc = [kb.ps(st, [128, 512], F32, "pbc") for _ in range(2)]
        pA = kb.ps(st, [128, 512], F32, "pA")
        po = [kb.ps(st, [128, 512], F32, "po") for _ in range(2)]
        pds = kb.ps(st, [128, 512], F32, "pds")
        hc = 0
        for tt in range(S // 128):
            b = tt % NB
            t0 = tt * 128
            kb.dma("sp", tm[b][:], hT_d[t0:t0 + 128, 384:2320], w=[tm[b]])
            kb.dma("pool", qkT[b][:], hF_d[0:768, t0:t0 + 128].rearrange("(a h d) t -> d a h t", a=2, h=H), w=[qkT[b]])
            kb.dma("pool", alrT[b][:], hF_d[1536:1552, t0:t0 + 128], w=[alrT[b]])
            k_tm = tm[b][:, 0:384]
            v_tm = tm[b][:, 384:1152]
            og_tm = tm[b][:, 1168:1936]
            kb.op("pe", lambda E, b=b: E.matmul(out=pz[:, 0:384], lhsT=alrT[b][:], rhs=wa2[:], start=True, stop=True), r=[alrT[b], wa2], w=[pz])
            kb.op("dve", lambda E, b=b: E.tensor_tensor(out=G[b][:], in0=pz[:, 0:384], in1=bab[:], op=ALU.add), r=[pz, bab], w=[G[b]])
            kb.op("act", lambda E, b=b: E.activation(out=G[b][:], in_=G[b][:], func=AF.Exp, scale=-1.0), r=[G[b]], w=[G[b]])
            kb.op("act", lambda E, b=b: E.activation(out=G[b][:], in_=G[b][:], func=AF.Ln, bias=1.0, scale=1.0), r=[G[b]], w=[G[b]])
            kb.op("act", lambda E, b=b: E.copy(out=vb[b][:], in_=tm[b][:, 384:1152]), r=[tm[b]], w=[vb[b]])
            kb.op("dve", lambda E, b=b: E.tensor_tensor(out=sg[b][:], in0=tm[b][:, 1168:1936], in1=bogb[:], op=ALU.add), r=[tm[b], bogb], w=[sg[b]])
            kb.op("act", lambda E, b=b: E.activation(out=sg[b][:], in_=sg[b][:], func=AF.Silu), r=[sg[b]], w=[sg[b]])
            kb.op("pe", lambda E, b=b: E.matmul(out=pdec[:, 0:384], lhsT=gc[:, 1, :], rhs=G[b][:], start=True, stop=True), r=[gc, G[b]], w=[pdec])
            kb.op("act", lambda E, b=b: E.activation(out=edec[b][:], in_=pdec[:, 0:384], func=AF.Exp), r=[pdec], w=[edec[b]])
            kb.op("dve", lambda E, b=b: E.tensor_tensor(out=kdec[b][:], in0=tm[b][:, 0:384], in1=edec[b][:], op=ALU.mult), r=[tm[b], edec[b]], w=[kdec[b]])
            for h in range(H):
                i = hc % 4
                i2 = hc % 2
                hc += 1
                pb_ = pbc[i2]
                kb.op("pe", lambda E, b=b, h=h, pb_=pb_: E.matmul(out=pb_[0:DK, 0:128], lhsT=G[b][:, h * DK:(h + 1) * DK], rhs=gc[:, 0, :], start=True, stop=True), r=[G[b], gc], w=[pb_])
                kb.op("act", lambda E, i=i, pb_=pb_: E.activation(out=eb[i][:], in_=pb_[0:DK, 0:128], func=AF.Exp), r=[pb_], w=[eb[i]])
                kb.op("act", lambda E, i=i, pb_=pb_: E.activation(out=enb[i][:], in_=pb_[0:DK, 0:128], func=AF.Exp, scale=-1.0), r=[pb_], w=[enb[i]])
                kb.op("dve", lambda E, i=i, b=b, h=h: E.scalar_tensor_tensor(out=qd[i][:], in0=qkT[b][:, 0, h, :], scalar=float(DK) ** -0.5, in1=eb[i][:], op0=ALU.mult, op1=ALU.mult), r=[qkT[b], eb[i]], w=[qd[i]])
                kb.op("dve", lambda E, i=i, b=b, h=h: E.tensor_tensor(out=kd[i][:], in0=qkT[b][:, 1, h, :], in1=enb[i][:], op=ALU.mult), r=[qkT[b], enb[i]], w=[kd[i]])
                kb.op("act", lambda E, i=i: E.copy(out=qd0[i][:, 0:64], in_=qd[i][:, 0:64]), r=[qd[i]], w=[qd0[i]])
                kb.op("act", lambda E, i=i: E.copy(out=qd1[i][:, 64:128], in_=qd[i][:, 64:128]), r=[qd[i]], w=[qd1[i]])
                kb.op("act", lambda E, i=i: E.copy(out=ebl[i][:, 0:1], in_=eb[i][:, 63:64]), r=[eb[i]], w=[ebl[i]])
                kb.op("act", lambda E, i=i: E.copy(out=ebl[i][:, 1:2], in_=eb[i][:, 127:128]), r=[eb[i]], w=[ebl[i]])
                kb.op("pe", lambda E, i=i: E.matmul(out=pA[:, 0:128], lhsT=kd[i][:], rhs=qd[i][:], start=True, stop=True), r=[kd[i], qd[i]], w=[pA])
                at = AT[i2]
                kb.op("dve", lambda E, at=at: E.tensor_tensor(out=at[:], in0=pA[:, 0:128], in1=gc[:, 2, :], op=ALU.mult), r=[pA, gc], w=[at])
                o = po[i2]
                sb_in = Sb[h][1]
                sb_mid = Sb[h][0]
                kb.op("pe", lambda E, o=o, at=at, b=b, h=h: E.matmul(out=o[:, 0:DV], lhsT=at[:], rhs=vb[b][:, h * DV:(h + 1) * DV], start=True, stop=False), r=[at, vb[b]], w=[o])
                kb.op("pe", lambda E, o=o, i=i, sb_in=sb_in: E.matmul(out=o[:, 0:DV], lhsT=qd0[i][:], rhs=sb_in[:], start=False, stop=False), r=[qd0[i], sb_in], w=[o])
                for j in range(2):
                    kb.op("pe", lambda E, b=b, h=h, j=j: E.matmul(out=pds[0:DK, 0:DV], lhsT=kdec[b][j * 64:(j + 1) * 64, h * DK:(h + 1) * DK], rhs=vb[b][j * 64:(j + 1) * 64, h * DV:(h + 1) * DV], start=True, stop=True), r=[kdec[b], vb[b]], w=[pds])
                    kb.op("dve", lambda E, h=h, i=i, j=j: E.scalar_tensor_tensor(out=Sf[h][:], in0=Sf[h][:], scalar=ebl[i][:, j:j + 1], in1=pds[0:DK, 0:DV], op0=ALU.mult, op1=ALU.add), r=[Sf[h], ebl[i], pds], w=[Sf[h]])
                    dst = sb_mid if j == 0 else sb_in
                    kb.op("act", lambda E, h=h, dst=dst: E.copy(out=dst[:], in_=Sf[h][:]), r=[Sf[h]], w=[dst])
                    if j == 0:
                        kb.op("pe", lambda E, o=o, i=i, sb_mid=sb_mid: E.matmul(out=o[:, 0:DV], lhsT=qd1[i][:], rhs=sb_mid[:], start=False, stop=True), r=[qd1[i], sb_mid], w=[o])
                sq = ssq[i2]
                rs = rstd[i2]
                ot = otmp[i2]
                kb.op("act", lambda E, o=o, sq=sq: E.activation(out=junk[:], in_=o[:, 0:DV], func=AF.Square, accum_out=sq[:]), r=[o], w=[junk, sq])
                kb.op("act", lambda E, sq=sq, rs=rs: E.activation(out=rs[:], in_=sq[:], func=AF.Sqrt, bias=epsb[:], scale=1.0 / DV), r=[sq, epsb], w=[rs])
                kb.op("dve", lambda E, rs=rs: E.reciprocal(out=rs[:], in_=rs[:]), r=[rs], w=[rs])
                kb.op("dve", lambda E, o=o, rs=rs, ot=ot: E.scalar_tensor_tensor(out=ot[:], in0=o[:, 0:DV], scalar=rs[:, 0:1], in1=nwb[:], op0=ALU.mult, op1=ALU.mult), r=[o, rs, nwb], w=[ot])
                kb.op("dve", lambda E, ot=ot, b=b, h=h: E.tensor_tensor(out=yt[b][:, h * DV:(h + 1) * DV], in0=ot[:], in1=sg[b][:, h * DV:(h + 1) * DV], op=ALU.mult), r=[ot, sg[b]], w=[yt[b]])
            kb.dma("sp", mix_d[t0:t0 + 128, 0:TOKW], yt[b][:], r=[yt[b]])
        kb.barrier()


def TT(kb, out, in0, in1, op, r, w, eng="dve"):
    return kb.op(eng, lambda E: E.tensor_tensor(out=out, in0=in0, in1=in1, op=op), r=r, w=w)


def TS(kb, out, in0, s1, s2, op0, op1, r, w, eng="dve"):
    if op1 is None:
        return kb.op(eng, lambda E: E.tensor_scalar(out=out, in0=in0, scalar1=s1, scalar2=None, op0=op0), r=r, w=w)
    return kb.op(eng, lambda E: E.tensor_scalar(out=out, in0=in0, scalar1=s1, scalar2=s2, op0=op0, op1=op1), r=r, w=w)


def STT(kb, out, in0, scalar, in1, op0, op1, r, w):
    return kb.op("dve", lambda E: E.scalar_tensor_tensor(out=out, in0=in0, scalar=scalar, in1=in1, op0=op0, op1=op1), r=r, w=w)


def ACT(kb, out, in_, func, r, w, bias=None, scale=None, accum_out=None):
    kw = {}
    if bias is not None:
        kw["bias"] = bias
    if scale is not None:
        kw["scale"] = scale
    if accum_out is not None:
        kw["accum_out"] = accum_out
    return kb.op("act", lambda E: E.activation(out=out, in_=in_, func=func, **kw), r=r, w=w)


def MM(kb, out, lhsT, rhs, start, stop, r, w):
    return kb.op("pe", lambda E: E.matmul(out=out, lhsT=lhsT, rhs=rhs, start=start, stop=stop), r=r, w=w)


def TR(kb, out, in_, ident, r, w):
    return kb.op("pe", lambda E: E.transpose(out=out, in_=in_, identity=ident), r=r, w=w)


def CP(kb, out, in_, r, w, eng="dve"):
    if eng == "act":
        return kb.op("act", lambda E: E.copy(out=out, in_=in_), r=r, w=w)
    return kb.op("dve", lambda E: E.tensor_copy(out=out, in_=in_), r=r, w=w)


def RED(kb, out, in_, op, r, w):
    return kb.op("dve", lambda E: E.tensor_reduce(out=out, in_=in_, axis=AX.X, op=op), r=r, w=w)


def MSET(kb, ap, val, w):
    return kb.op("dve", lambda E: E.memset(ap, val), w=w)


def bcast_row(kb, st, d_ap, n, name):
    t = kb.sb(st, [128, n], F32, name)
    kb.dma("sp", t[:], d_ap.partition_broadcast(128), w=[t])
    return t


RW_H = 12


def perm_view(ap):
    return ap.rearrange("t (hp j k) -> t hp j k", hp=2, j=6)


def nat_view(ap):
    return ap.rearrange("t (j hp k) -> t hp j k", hp=2, j=6)


def store_perm(kb, q, dram_rows, tile_ap, r):
    for hp in range(2):
        kb.dma(q, dram_rows[:, hp * 384:(hp + 1) * 384].rearrange("t (j k) -> t j k", k=64), nat_view(tile_ap)[:, hp], r=r)


def load_perm(kb, q, tile, dram_rows):
    for hp in range(2):
        kb.dma(q, nat_view(tile[:])[:, hp], dram_rows[:, hp * 384:(hp + 1) * 384].rearrange("t (j k) -> t j k", k=64), w=[(tile, hp)])


def stage_rwkv(kb, C, hT_d, P, mix_d, scr):
    Wd, Kd, Ad, Bd, Rd = scr["W"], scr["K"], scr["A"], scr["B"], scr["R"]
    Vd, Gd, VPd, Od = scr["V"], scr["G"], scr["VP"], scr["O"]
    with contextlib.ExitStack() as st:
        mub = bcast_row(kb, st, P["mu"], 2560, "mub")
        w0b = bcast_row(kb, st, P["w0"], TOKW, "w0b")
        a0b = bcast_row(kb, st, P["a0"], TOKW, "a0b")
        kkb = bcast_row(kb, st, P["k_k"], TOKW, "kkb")
        kab = bcast_row(kb, st, P["k_a"], TOKW, "kab")
        w2b = kb.sb(st, [64, TOKW], BF16, "w2b")
        a2b = kb.sb(st, [64, TOKW], BF16, "a2b")
        g2b = kb.sb(st, [128, TOKW], BF16, "g2b")
        kb.dma("pool", w2b[:], P["w2"], w=[w2b])
        kb.dma("pool", a2b[:], P["a2"], w=[a2b])
        kb.dma("pool", g2b[:], P["g2"], w=[g2b])
        NB = 2
        hc = [kb.sb(st, [128, 2560], F32, "hc") for _ in range(NB)]
        hp_ = [kb.sb(st, [128, 2560], F32, "hp") for _ in range(NB)]
        twT = [kb.sb(st, [64, 128], BF16, "twT") for _ in range(NB)]
        alT = [kb.sb(st, [64, 128], BF16, "alT") for _ in range(NB)]
        sgT = [kb.sb(st, [128, 128], BF16, "sgT") for _ in range(NB)]
        Wt = [kb.sb(st, [128, TOKW], F32, "Wt") for _ in range(NB)]
        At = [kb.sb(st, [128, TOKW], F32, "At") for _ in range(NB)]
        Bt = [kb.sb(st, [128, TOKW], F32, "Bt") for _ in range(NB)]
        Kt = [kb.sb(st, [128, TOKW], F32, "Kt") for _ in range(NB)]
        Gt = [kb.sb(st, [128, TOKW], F32, "Gt") for _ in range(NB)]
        aa = [kb.sb(st, [128, TOKW], F32, "aa") for _ in range(NB)]
        sq = [kb.sb(st, [128, TOKW], F32, "sq") for _ in range(NB)]
        ss = [kb.sb(st, [128, RW_H], F32, "ss") for _ in range(NB)]
        vps = [kb.sb(st, [128, 6, 128], F32, "vps") for _ in range(NB)]
        ptr = kb.ps(st, [128, 512], F32, "ptr")
        pw = [kb.ps(st, [128, 512], F32, "pw") for _ in range(2)]
        pa = [kb.ps(st, [128, 512], F32, "pa") for _ in range(2)]
        pgp = [kb.ps(st, [128, 512], F32, "pgp") for _ in range(2)]
        pvp = kb.ps(st, [128, 512], F32, "pvp")
        for b in range(NB):
            MSET(kb, hp_[b][0:1, :], 0.0, w=[hp_[b]])
        for tt in range(S // 128):
            b = tt % NB
            t0 = tt * 128
            H_, HP = hc[b], hp_[b]
            kb.dma("sp", H_[:], hT_d[t0:t0 + 128, 0:2560], w=[H_])
            if tt == 0:
                kb.dma("pool", HP[1:128, :], hT_d[0:127, 0:2560], w=[HP])
            else:
                kb.dma("pool", HP[:], hT_d[t0 - 1:t0 + 127, 0:2560], w=[HP])
            TT(kb, HP[:], HP[:], H_[:], ALU.subtract, r=[HP, H_], w=[HP])
            TT(kb, HP[:], HP[:], mub[:], ALU.mult, r=[HP, mub], w=[HP])
            TT(kb, H_[:], H_[:], HP[:], ALU.add, r=[H_, HP], w=[H_])
            r_ = H_[:, 0:768]
            k_ = H_[:, 832:1600]
            v_ = H_[:, 1600:2368]
            TR(kb, ptr[0:64, 0:128], H_[:, 768:832], C.ident_f[:], r=[H_, C.ident_f], w=[ptr])
            TR(kb, ptr[0:64, 128:256], H_[:, 2368:2432], C.ident_f[:], r=[H_, C.ident_f], w=[ptr])
            TR(kb, ptr[:, 256:384], H_[:, 2432:2560], C.ident_f[:], r=[H_, C.ident_f], w=[ptr])
            ACT(kb, twT[b][:], ptr[0:64, 0:128], AF.Tanh, r=[ptr], w=[twT[b]])
            CP(kb, alT[b][:], ptr[0:64, 128:256], r=[ptr], w=[alT[b]])
            ACT(kb, sgT[b][:], ptr[:, 256:384], AF.Sigmoid, r=[ptr], w=[sgT[b]])
            for (lh, rh, pp) in ((twT[b], w2b, pw), (alT[b], a2b, pa), (sgT[b], g2b, pgp)):
                MM(kb, pp[0][:, 0:512], lh[:], rh[:, 0:512], True, True, r=[lh, rh], w=[pp[0]])
                MM(kb, pp[1][:, 0:256], lh[:], rh[:, 512:768], True, True, r=[lh, rh], w=[pp[1]])
            W_, A_, B_, K_, G_, a_, q_ = Wt[b], At[b], Bt[b], Kt[b], Gt[b], aa[b], sq[b]
            for (c0, c1, hh) in ((0, 512, 0), (512, 768, 1)):
                n = c1 - c0
                TT(kb, W_[:, c0:c1], pw[hh][:, 0:n], w0b[:, c0:c1], ALU.add, r=[pw[hh], w0b], w=[W_])
                TT(kb, a_[:, c0:c1], pa[hh][:, 0:n], a0b[:, c0:c1], ALU.add, r=[pa[hh], a0b], w=[a_])
                CP(kb, G_[:, c0:c1], pgp[hh][:, 0:n], r=[pgp[hh]], w=[G_], eng="act")
            ACT(kb, W_[:], W_[:], AF.Sigmoid, r=[W_], w=[W_])
            ACT(kb, W_[:], W_[:], AF.Exp, r=[W_], w=[W_], scale=-float(np.exp(-0.5)))
            ACT(kb, a_[:], a_[:], AF.Sigmoid, r=[a_], w=[a_])
            TT(kb, A_[:], k_, kkb[:], ALU.mult, r=[H_, kkb], w=[A_])
            TT(kb, q_[:], A_[:], A_[:], ALU.mult, r=[A_], w=[q_])
            RED(kb, ss[b][:], q_[:].rearrange("p (h k) -> p h k", k=64), ALU.add, r=[q_], w=[ss[b]])
            ACT(kb, ss[b][:], ss[b][:], AF.Sqrt, r=[ss[b]], w=[ss[b]])
            TS(kb, ss[b][:], ss[b][:], 1e-12, None, ALU.max, None, r=[ss[b]], w=[ss[b]])
            kb.op("dve", lambda E, s_=ss[b]: E.reciprocal(out=s_[:], in_=s_[:]), r=[ss[b]], w=[ss[b]])
            TT(kb, A_[:].rearrange("p (h k) -> p h k", k=64), A_[:].rearrange("p (h k) -> p h k", k=64), ss[b][:, :].unsqueeze(2).broadcast_to([128, RW_H, 64]), ALU.mult, r=[A_, ss[b]], w=[A_])
            TT(kb, B_[:], A_[:], a_[:], ALU.mult, r=[A_, a_], w=[B_])
            TS(kb, A_[:], A_[:], -1.0, None, ALU.mult, None, r=[A_], w=[A_])
            STT(kb, q_[:], a_[:], -1.0, kab[:], ALU.add, ALU.mult, r=[a_, kab], w=[q_])
            STT(kb, K_[:], q_[:], 1.0, k_, ALU.add, ALU.mult, r=[q_, H_], w=[K_])
            for j in range(6):
                TR(kb, pvp[:, 0:128] if j % 2 == 0 else pvp[:, 128:256], H_[:, 1600 + j * 128:1600 + (j + 1) * 128], C.ident_f[:], r=[H_, C.ident_f], w=[pvp])
                CP(kb, vps[b][:, j, :], pvp[:, 0:128] if j % 2 == 0 else pvp[:, 128:256], r=[pvp], w=[vps[b]], eng="act" if j % 2 else "dve")
            rows = slice(t0, t0 + 128)
            store_perm(kb, "sp", Wd[rows, :], W_[:], r=[W_])
            store_perm(kb, "pool", Kd[rows, :], K_[:], r=[K_])
            store_perm(kb, "sp", Ad[rows, :], A_[:], r=[A_])
            store_perm(kb, "pool", Bd[rows, :], B_[:], r=[B_])
            store_perm(kb, "sp", Rd[rows, :], H_[:, 0:768], r=[H_])
            kb.dma("pool", Vd[rows, :], H_[:, 1600:2368], r=[H_])
            kb.dma("sp", Gd[rows, :], G_[:], r=[G_])
            kb.dma("pool", VPd[:, :, t0:t0 + 128], vps[b][:], r=[vps[b]])
        kb.barrier()

    with contextlib.ExitStack() as st:
        T = 8
        Sst = kb.sb(st, [128, 6, 64], F32, "Sst")
        SK = [("S", j) for j in range(6)]
        MSET(kb, Sst[:], 0.0, w=SK)
        BC = [kb.sb(st, [128, T, 5, 384], F32, "BC") for _ in range(2)]
        vP = [kb.sb(st, [128, 6, 512], F32, "vP") for _ in range(2)]
        oP = [kb.sb(st, [128, 6, 512], F32, "oP") for _ in range(2)]
        tmp = [kb.sb(st, [128, 6, 64], F32, "tmp") for _ in range(2)]
        sa = [kb.sb(st, [128, 6], F32, "sa") for _ in range(2)]
        otile = [kb.sb(st, [128, TOKW], F32, "otile") for _ in range(2)]
        pto = [kb.ps(st, [128, 512], F32, "pto") for _ in range(2)]
        srcs = (Ad, Wd, Bd, Kd, Rd)
        qs = ("sp", "act", "pool")
        for blk in range(S // T):
            bb = blk % 2
            t0 = blk * T
            g = (t0 // 512) % 2
            if t0 % 512 == 0:
                kb.dma("sp", vP[g][:], VPd[:, :, t0:t0 + 512], w=[vP[g]])
            for vi in range(5):
                for hp in range(2):
                    kb.dma(qs[(vi * 2 + hp) % 3], BC[bb][hp * 64:(hp + 1) * 64, :, vi, :], srcs[vi][t0:t0 + T, hp * 384:(hp + 1) * 384].partition_broadcast(64), w=[("BC", bb, vi, hp)])
            for tl in range(T):
                tg = (t0 + tl) % 512
                ti = tl % 2

                def bc(vi, bb=bb, tl=tl):
                    return BC[bb][:, tl, vi, :].rearrange("p (j k) -> p j k", k=64)

                def bk(vi, bb=bb):
                    return [("BC", bb, vi, 0), ("BC", bb, vi, 1)]

                TT(kb, tmp[ti][:], Sst[:], bc(0), ALU.mult, r=SK + bk(0), w=[tmp[ti]])
                RED(kb, sa[ti][:], tmp[ti][:], ALU.add, r=[tmp[ti]], w=[sa[ti]])
                TT(kb, Sst[:], Sst[:], bc(1), ALU.mult, r=SK + bk(1), w=SK)
                Bv, Kv = bc(2), bc(3)
                for j in range(6):
                    STT(kb, Sst[:, j, :], Bv[:, j, :], sa[ti][:, j:j + 1], Sst[:, j, :], ALU.mult, ALU.add, r=[sa[ti], SK[j]] + bk(2), w=[SK[j]])
                for j in range(6):
                    STT(kb, Sst[:, j, :], Kv[:, j, :], vP[g][:, j, tg:tg + 1], Sst[:, j, :], ALU.mult, ALU.add, r=[vP[g], SK[j]] + bk(3), w=[SK[j]])
                TT(kb, tmp[ti][:], Sst[:], bc(4), ALU.mult, r=SK + bk(4), w=[tmp[ti]])
                RED(kb, oP[g][:, :, tg], tmp[ti][:], ALU.add, r=[tmp[ti]], w=[oP[g]])
            if (t0 + T) % 512 == 0:
                G0 = t0 + T - 512
                for sub in range(4):
                    ot = otile[sub % 2]
                    for j in range(6):
                        p = pto[j % 2]
                        TR(kb, p[:, 0:128], oP[g][:, j, sub * 128:(sub + 1) * 128], C.ident_f[:], r=[oP[g], C.ident_f], w=[p])
                        CP(kb, ot[:, j * 128:(j + 1) * 128], p[:, 0:128], r=[p], w=[ot], eng="act")
                    kb.dma("sp", Od[G0 + sub * 128:G0 + (sub + 1) * 128, :], ot[:], r=[ot])
        kb.barrier()
    with contextlib.ExitStack() as st:
        lnw = bcast_row(kb, st, P["ln_w"], TOKW, "lnw")
        lnb = bcast_row(kb, st, P["ln_b"], TOKW, "lnb")
        rkb = bcast_row(kb, st, P["r_k"], TOKW, "rkb")
        NB = 2
        o_ = [kb.sb(st, [128, TOKW], F32, "o") for _ in range(NB)]
        r_ = [kb.sb(st, [128, TOKW], F32, "r") for _ in range(NB)]
        k_ = [kb.sb(st, [128, TOKW], F32, "k") for _ in range(NB)]
        v_ = [kb.sb(st, [128, TOKW], F32, "v") for _ in range(NB)]
        g_ = [kb.sb(st, [128, TOKW], F32, "g") for _ in range(NB)]
        q_ = [kb.sb(st, [128, TOKW], F32, "q") for _ in range(NB)]
        s1 = [kb.sb(st, [128, RW_H], F32, "s1") for _ in range(NB)]
        s2 = [kb.sb(st, [128, RW_H], F32, "s2") for _ in range(NB)]
        s3 = [kb.sb(st, [128, RW_H], F32, "s3") for _ in range(NB)]

        def h3(ap):
            return ap.rearrange("p (h k) -> p h k", k=64)

        def b3(ap):
            return ap.unsqueeze(2).broadcast_to([128, RW_H, 64])

        for tt in range(S // 128):
            b = tt % NB
            rows = slice(tt * 128, (tt + 1) * 128)
            O, R_, K_, V_, G_, Q_ = o_[b], r_[b], k_[b], v_[b], g_[b], q_[b]
            kb.dma("sp", O[:], Od[rows, :], w=[O])
            load_perm(kb, "pool", R_, Rd[rows, :])
            load_perm(kb, "pool", K_, Kd[rows, :])
            kb.dma("sp", V_[:], Vd[rows, :], w=[V_])
            kb.dma("sp", G_[:], Gd[rows, :], w=[G_])
            RED(kb, s1[b][:], h3(O[:]), ALU.add, r=[O], w=[s1[b]])
            TT(kb, Q_[:], O[:], O[:], ALU.mult, r=[O], w=[Q_])
            RED(kb, s2[b][:], h3(Q_[:]), ALU.add, r=[Q_], w=[s2[b]])
            TS(kb, s1[b][:], s1[b][:], 1.0 / 64, None, ALU.mult, None, r=[s1[b]], w=[s1[b]])
            STT(kb, s3[b][:], s1[b][:], -1.0, s1[b][:], ALU.mult, ALU.mult, r=[s1[b]], w=[s3[b]])
            STT(kb, s2[b][:], s2[b][:], 1.0 / 64, s3[b][:], ALU.mult, ALU.add, r=[s2[b], s3[b]], w=[s2[b]])
            TS(kb, s2[b][:], s2[b][:], 64e-5, None, ALU.add, None, r=[s2[b]], w=[s2[b]])
            ACT(kb, s2[b][:], s2[b][:], AF.Sqrt, r=[s2[b]], w=[s2[b]])
            kb.op("dve", lambda E, x=s2[b]: E.reciprocal(out=x[:], in_=x[:]), r=[s2[b]], w=[s2[b]])
            TT(kb, h3(O[:]), h3(O[:]), b3(s1[b][:, :]), ALU.subtract, r=[O, s1[b]], w=[O])
            TT(kb, h3(O[:]), h3(O[:]), b3(s2[b][:, :]), ALU.mult, r=[O, s2[b]], w=[O])
            TT(kb, O[:], O[:], lnw[:], ALU.mult, r=[O, lnw], w=[O])
            TT(kb, O[:], O[:], lnb[:], ALU.add, r=[O, lnb], w=[O])
            TT(kb, Q_[:], R_[:], K_[:], ALU.mult, r=[(R_, 0), (R_, 1), (K_, 0), (K_, 1), Q_], w=[Q_])
            TT(kb, Q_[:], Q_[:], rkb[:], ALU.mult, r=[Q_, rkb], w=[Q_])
            RED(kb, s3[b][:], h3(Q_[:]), ALU.add, r=[Q_], w=[s3[b]])
            TT(kb, h3(Q_[:]), h3(V_[:]), b3(s3[b][:, :]), ALU.mult, r=[V_, s3[b]], w=[Q_])
            TT(kb, O[:], O[:], Q_[:], ALU.add, r=[O, Q_], w=[O])
            TT(kb, O[:], O[:], G_[:], ALU.mult, r=[O, G_], w=[O])
            kb.dma("sp", mix_d[rows, 0:TOKW], O[:], r=[O])
        kb.barrier()


NSA_BIG = 1.0e4
NEG = -1.0e30


def _t5_bucket_np(dist):
    d = np.maximum(dist, 0)
    scaled = (np.log(np.maximum(d, 1).astype(np.float32) / np.float32(16)) / np.float32(np.log(128 / 16))).astype(np.float32)
    large = np.minimum(16 + (scaled * np.float32(16)).astype(np.int32), 31)
    return np.where(d < 16, d, large)


def nsa_tables_np(rel_table):
    kk = np.arange(128)[:, None]
    qq = np.arange(512)[None, :]
    out = np.empty((12, 18, 128, 512), np.float32)
    for o in range(5):
        dist = qq - kk - 128 * (o - 1)
        bk = _t5_bucket_np(dist)
        for h in range(12):
            out[h, o] = np.where(dist >= 0, rel_table[bk, h], NEG)
    for o in range(8):
        dist = qq - kk - 128 * (o - 4)
        bk = _t5_bucket_np(dist)
        for h in range(12):
            out[h, 5 + o] = np.where((dist >= 0) & (dist < 512), rel_table[bk, h], NEG)
    for o in range(5):
        dist = 512 * o + qq - 16 * kk - 31
        bk = _t5_bucket_np(dist)
        for h in range(12):
            out[h, 13 + o] = np.where(dist >= 0, rel_table[bk, h], NEG)
    return out.reshape(216, 128 * 512)


def nsa_static_np():
    q = np.arange(S)[:, None]
    m = np.arange(64)[None, :]
    cur = q // 64
    causal = m <= cur
    forced = (m == 0) | (m == cur) | (m == cur - 1)
    A = np.where(causal & ~forced, 1.0, 0.0).astype(np.float32)
    B = np.where(causal & forced, NSA_BIG, np.where(causal, 0.0, -1.0)).astype(np.float32)
    AB = np.stack([A, B]).reshape(2, 32, 128, 64).transpose(2, 0, 1, 3).copy()
    n = np.arange(256)[:, None]
    cs, ce = n * 16, n * 16 + 31
    c2s = ((cs < m * 64 + 64) & (ce >= m * 64) & (n < 255)).astype(np.float32)
    c2s = c2s.reshape(2, 128, 64).transpose(1, 0, 2).copy()
    E = np.zeros((64, 32, 128), np.float32)
    for j in range(32):
        for k in range(128):
            E[2 * j + k // 64, j, k] = NSA_BIG
    return AB, c2s, E


def stage_nsa(kb, C, hF_d, hT_d, P, tab_d, mix_d):
    QC, KCC, VCC, KSC, VSC, KWC, VWC, GLC = 0, 768, 1024, 1280, 1536, 1792, 2048, 2304
    with contextlib.ExitStack() as st:
        KC = [kb.sb(st, [64, 256], BF16, "KC") for _ in range(4)]
        VCx = [kb.sb(st, [128, 2, 129], BF16, "VCx") for _ in range(4)]
        c2s = kb.sb(st, [128, 2, 64], F32, "c2s")
        kb.dma("sp", c2s[:], P["c2s"], w=[c2s])
        CH = bcast_row(kb, st, P["rel31"], 12, "CH")
        gbb = bcast_row(kb, st, P["gate_b"], 36, "gbb")
        GATE = kb.sb(st, [128, 32, 36], F32, "GATE")
        kb.dma("sp", GATE[:], hT_d[:, GLC:GLC + 36].rearrange("(t p) c -> p t c", p=128), w=[GATE])
        TT(kb, GATE[:], GATE[:], gbb[:, :].unsqueeze(1).broadcast_to([128, 32, 36]), ALU.add, r=[GATE, gbb], w=[GATE])
        ACT(kb, GATE[:], GATE[:], AF.Sigmoid, r=[GATE], w=[GATE])
        import os as _os
        _stop = int(_os.environ.get("NSA_STOP", "99"))
        if _stop == -1:
            kb.barrier()
            return
        with contextlib.ExitStack() as s0:
            w1b = [kb.sb(s0, [64, 32, 256], BF16, "w1b") for _ in range(2)]
            w2b = [kb.sb(s0, [128, 2, 64], BF16, "w2b") for _ in range(2)]
            peT = [kb.sb(s0, [64, 32, 2], BF16, "peT") for _ in range(2)]
            cv = [kb.sb(s0, [128, 2], F32, "cv") for _ in range(2)]
            praw = kb.sb(s0, [32, 2, 64], F32, "praw")
            kb.dma("sp", praw[:], P["pe"].rearrange("a l d -> l a d"), w=[praw])
            pp = kb.ps(s0, [128, 512], F32, "pp")
            ph = [kb.ps(s0, [128, 512], F32, "ph") for _ in range(2)]
            pk = kb.ps(s0, [128, 512], F32, "pk")
            pv = [kb.ps(s0, [128, 512], F32, "pv") for _ in range(2)]
            pcv = [pp, ph[0], ph[1], pk]
            for a in range(2):
                kb.dma("pool", w1b[a][:], P["w1"][a].rearrange("(l d) j -> d l j", d=64), w=[w1b[a]])
                kb.dma("pool", w2b[a][:], P["w2"][a].rearrange("(c j) d -> j c d", j=128), w=[w2b[a]])
                TR(kb, pp[0:64, a * 32:(a + 1) * 32], praw[:, a, :], C.ident_f[0:32, 0:32], r=[praw, C.ident_f], w=[pp])
                CP(kb, peT[a][:, :, 0], pp[0:64, a * 32:(a + 1) * 32], r=[pp], w=[peT[a]])
                CP(kb, peT[a][:, :, 1], pp[0:64, a * 32:(a + 1) * 32], r=[pp], w=[peT[a]])
            _c0 = int(_os.environ.get("NSA_C0", "99"))
            if _c0 == 1:
                kb.barrier()
                return
            for a in range(2):
                for jc in range(2):
                    for l in range(32):
                        MM(kb, pcv[a * 2 + jc][:, 0:2], w1b[a][:, l, jc * 128:(jc + 1) * 128], peT[a][:, l, :], l == 0, l == 31, r=[w1b[a], peT[a]], w=[pcv[a * 2 + jc]])
                for jc in range(2):
                    CP(kb, cv[a][:, jc:jc + 1], pcv[a * 2 + jc][:, 0:1], r=[pcv[a * 2 + jc]], w=[cv[a]])
            if _c0 == 2:
                kb.barrier()
                return
            kvT = [kb.sb(s0, [64, S], BF16, "kvT") for _ in range(2)]
            xg = [kb.sb(s0, [128, 256], F32, "xg") for _ in range(2)]
            x2 = [kb.sb(s0, [128, 256], F32, "x2") for _ in range(2)]
            gT = [kb.sb(s0, [128, 2, 256], BF16, "gT") for _ in range(2)]
            for a in range(2):
                MSET(kb, VCx[0][:], 0.0, w=[VCx[0]]) if a == 0 else None
            for g in range(1, 4):
                MSET(kb, VCx[g][:], 0.0, w=[VCx[g]])
            i = 0
            for g in range(4):
                for a in range(2):
                    src = kvT[i % 2]
                    G_ = gT[i % 2]
                    i += 1
                    c0 = (KCC if a == 0 else VCC) + g * 64
                    kb.dma("pool", src[:], hF_d[c0:c0 + 64, :], w=[src])
                    if a == 0:
                        MSET(kb, G_[:, :, 255:256], 0.0, w=[("gpad", id(G_))])
                    for jc in range(2):
                        p = ph[jc]
                        for l in range(32):
                            MM(kb, p[:, 0:255], w1b[a][:, l, jc * 128:(jc + 1) * 128], src[:, l:l + 16 * 254 + 1:16], l == 0, l == 31, r=[w1b[a], src], w=[p])
                        if _c0 == 3:
                            continue
                        X, X2 = xg[jc], x2[jc]
                        ACT(kb, X[:, 0:255], p[:, 0:255], AF.Identity, r=[p, cv[a]], w=[X], bias=cv[a][:, jc:jc + 1], scale=1.0)
                        TT(kb, X2[:, 0:255], X[:, 0:255], X[:, 0:255], ALU.mult, r=[X], w=[X2])
                        TS(kb, X2[:, 0:255], X2[:, 0:255], 0.044715, 1.0, ALU.mult, ALU.add, r=[X2], w=[X2])
                        TT(kb, X2[:, 0:255], X2[:, 0:255], X[:, 0:255], ALU.mult, r=[X, X2], w=[X2])
                        ACT(kb, X2[:, 0:255], X2[:, 0:255], AF.Tanh, r=[X2], w=[X2], scale=0.7978845608028654)
                        STT(kb, X2[:, 0:255], X2[:, 0:255], 1.0, X[:, 0:255], ALU.add, ALU.mult, r=[X, X2], w=[X2])
                        TS(kb, G_[:, jc, 0:255], X2[:, 0:255], 0.5, None, ALU.mult, None, r=[X2], w=[G_])
                    if _c0 in (3, 4):
                        continue
                    if a == 0:
                        for jc in range(2):
                            MM(kb, pk[0:64, 0:256], w2b[0][:, jc, :], G_[:, jc, :], jc == 0, jc == 1, r=[w2b[0], G_, ("gpad", id(G_))], w=[pk])
                        CP(kb, KC[g][:], pk[0:64, 0:256], r=[pk], w=[KC[g]])
                    else:
                        for nc_ in range(2):
                            nn = 128 if nc_ == 0 else 127
                            for jc in range(2):
                                MM(kb, pv[nc_][0:nn, 0:64], G_[:, jc, nc_ * 128:nc_ * 128 + nn], w2b[1][:, jc, :], jc == 0, jc == 1, r=[w2b[1], G_], w=[pv[nc_]])
                            CP(kb, VCx[g][0:nn, nc_, 0:64], pv[nc_][0:nn, 0:64], r=[pv[nc_]], w=[VCx[g]])
                            MSET(kb, VCx[g][0:nn, nc_, 64:65], 1.0, w=[VCx[g]])
                            CP(kb, VCx[g][0:nn, nc_, 65:129], c2s[0:nn, nc_, :], r=[c2s], w=[VCx[g]], eng="act")
            kb.barrier()
        import os as _os
        _stop = int(_os.environ.get("NSA_STOP", "99"))
        if _stop == 0:
            return
        Y = kb.sb(st, [128, 32, 192], F32, "Y")
        IMP = kb.sb(st, [128, 32, 64], F32, "IMP")
        SELT = kb.sb(st, [64, S], BF16, "SELT")
        qT = [kb.sb(st, [64, S], BF16, "qT") for _ in range(2)]
        sc_f = [kb.sb(st, [128, 512], F32, "scf") for _ in range(3)]
        pT = [kb.sb(st, [128, 512], BF16, "pT") for _ in range(3)]
        rd = [kb.sb(st, [128, 1], F32, "rd") for _ in range(4)]
        psc = [kb.ps(st, [128, 512], F32, "psc") for _ in range(3)]
        pacc = [kb.ps(st, [128, 512], F32, "pacc") for _ in range(4)]
        qi = 0
        sci = 0

        def load_q(h):
            nonlocal qi
            t = qT[qi % 2]
            qi += 1
            kb.dma("pool", t[:], hF_d[QC + h * 64:QC + (h + 1) * 64, :], w=[t])
            TS(kb, t[:], t[:], 0.125, None, ALU.mult, None, r=[t], w=[t])
            return t

        def finish(acc, ncol, h, br, I, qs, init):
            r_ = rd[qs]
            tile = I * 4 + qs
            TS(kb, r_[:], acc[:, 64:65], 1e-30, None, ALU.max, None, r=[acc], w=[r_])
            kb.op("dve", lambda E: E.reciprocal(out=r_[:], in_=r_[:]), r=[r_], w=[r_])
            hl = h % 3
            if ncol > 65:
                if hl == 0:
                    TS(kb, IMP[:, tile, :], acc[:, 65:129], r_[:, 0:1], None, ALU.mult, None, r=[acc, r_], w=[("IMP", tile)])
                else:
                    STT(kb, IMP[:, tile, :], acc[:, 65:129], r_[:, 0:1], IMP[:, tile, :], ALU.mult, ALU.add, r=[acc, r_, ("IMP", tile)], w=[("IMP", tile)])
            TT(kb, r_[:], r_[:], GATE[:, tile, h * 3 + br:h * 3 + br + 1], ALU.mult, r=[r_, GATE], w=[r_])
            ydst = Y[:, tile, hl * 64:(hl + 1) * 64]
            if init:
                TS(kb, ydst, acc[:, 0:64], r_[:, 0:1], None, ALU.mult, None, r=[acc, r_], w=[("Y", tile, hl)])
            else:
                STT(kb, ydst, acc[:, 0:64], r_[:, 0:1], ydst, ALU.mult, ALU.add, r=[acc, r_, ("Y", tile, hl)], w=[("Y", tile, hl)])

        for g in range(4):
            with contextlib.ExitStack() as s1:
                bC = kb.sb(s1, [128, 5, 512], F32, "bC")
                for r in range(3):
                    h = g * 3 + r
                    q_ = load_q(h)
                    kb.dma("sp", bC[:], tab_d[h * 18 + 13:h * 18 + 18, :].rearrange("o (k q) -> k o q", q=512), w=[bC])
                    for I in range(8):
                        ets = []
                        for nc_ in range(2):
                            off = I - 4 * nc_
                            if off < 0:
                                continue
                            p = psc[sci % 3]
                            e_ = pT[sci % 3]
                            f_ = sc_f[sci % 3]
                            sci += 1
                            MM(kb, p[:], KC[g][:, nc_ * 128:(nc_ + 1) * 128], q_[:, I * 512:(I + 1) * 512], True, True, r=[KC[g], q_], w=[p])
                            if off < 5:
                                TT(kb, f_[:], p[:], bC[:, off, :], ALU.add, r=[p, bC], w=[f_])
                                ACT(kb, e_[:], f_[:], AF.Exp, r=[f_], w=[e_])
                            else:
                                ACT(kb, e_[:], p[:], AF.Exp, r=[p, CH], w=[e_], bias=CH[:, h:h + 1], scale=1.0)
                            ets.append((nc_, e_))
                        for qs in range(4):
                            acc = pacc[qs]
                            for ii, (nc_, e_) in enumerate(ets):
                                MM(kb, acc[:, 0:129], e_[:, qs * 128:(qs + 1) * 128], VCx[g][:, nc_, :], ii == 0, ii == len(ets) - 1, r=[e_, VCx[g]], w=[acc])
                            finish(acc, 129, h, 0, I, qs, True)
                kb.barrier()
            if _stop == 1:
                return
            with contextlib.ExitStack() as s2:
                AB = kb.sb(s2, [128, 2, 32, 64], F32, "AB")
                kb.dma("sp", AB[:], P["AB"], w=[AB])
                scs = [kb.sb(s2, [128, 64], F32, "scs") for _ in range(2)]
                sc2 = [kb.sb(s2, [128, 64], F32, "sc2") for _ in range(2)]
                m1 = [kb.sb(s2, [128, 8], F32, "m1") for _ in range(2)]
                m2 = [kb.sb(s2, [128, 8], F32, "m2") for _ in range(2)]
                selb = [kb.sb(s2, [128, 64], BF16, "selb") for _ in range(2)]
                ptb = kb.ps(s2, [128, 1024], BF16, "ptb")
                for tile in range(32):
                    i = tile % 2
                    TT(kb, scs[i][:], IMP[:, tile, :], AB[:, 0, tile, :], ALU.mult, r=[("IMP", tile), AB], w=[scs[i]])
                    TT(kb, scs[i][:], scs[i][:], AB[:, 1, tile, :], ALU.add, r=[scs[i], AB], w=[scs[i]])
                    kb.op("dve", lambda E, i=i: E.max(out=m1[i][:], in_=scs[i][:]), r=[scs[i]], w=[m1[i]])
                    kb.op("dve", lambda E, i=i: E.match_replace(out=sc2[i][:], in_to_replace=m1[i][:], in_values=scs[i][:], imm_value=-2.0), r=[scs[i], m1[i]], w=[sc2[i]])
                    kb.op("dve", lambda E, i=i: E.max(out=m2[i][:], in_=sc2[i][:]), r=[sc2[i]], w=[m2[i]])
                    TS(kb, m2[i][:, 7:8], m2[i][:, 7:8], 0.0, None, ALU.max, None, r=[m2[i]], w=[m2[i]])
                    TS(kb, selb[i][:], scs[i][:], m2[i][:, 7:8], -1.0, ALU.is_ge, ALU.add, r=[scs[i], m2[i]], w=[selb[i]])
                    TR(kb, ptb[0:64, (tile % 4) * 128:(tile % 4 + 1) * 128], selb[i][:], C.ident_b[:], r=[selb[i], C.ident_b], w=[ptb])
                    if tile % 4 == 3:
                        CP(kb, SELT[:, (tile - 3) * 128:(tile + 1) * 128], ptb[0:64, 0:512], r=[ptb], w=[SELT], eng="act")
                kb.barrier()
            if _stop == 2:
                return
            with contextlib.ExitStack() as s3:
                bS = kb.sb(s3, [128, 13, 512], F32, "bS")
                Eall = kb.sb(s3, [64, 32, 128], BF16, "Eall")
                kb.dma("pool", Eall[:], P["E"], w=[Eall])
                ksT = kb.sb(s3, [64, S], BF16, "ksT")
                kwT = kb.sb(s3, [64, S], BF16, "kwT")
                kb.dma("pool", ksT[:], hF_d[KSC + g * 64:KSC + (g + 1) * 64, :], w=[ksT])
                kb.dma("pool", kwT[:], hF_d[KWC + g * 64:KWC + (g + 1) * 64, :], w=[kwT])
                VSx = kb.sb(s3, [128, 32, 65], BF16, "VSx")
                VWx = kb.sb(s3, [128, 32, 65], BF16, "VWx")
                MSET(kb, VSx[:, :, 64:65], 1.0, w=[("vs1",)])
                MSET(kb, VWx[:, :, 64:65], 1.0, w=[("vw1",)])
                kb.dma("pool", VSx[:, :, 0:64], hT_d[:, VSC + g * 64:VSC + (g + 1) * 64].rearrange("(t p) c -> p t c", p=128), w=[VSx])
                kb.dma("pool", VWx[:, :, 0:64], hT_d[:, VWC + g * 64:VWC + (g + 1) * 64].rearrange("(t p) c -> p t c", p=128), w=[VWx])
                for r in range(3):
                    h = g * 3 + r
                    q_ = load_q(h)
                    kb.dma("sp", bS[:], tab_d[h * 18:h * 18 + 13, :].rearrange("o (k q) -> k o q", q=512), w=[bS])
                    for I in range(8):
                        for j in range(4 * I + 4):
                            off = j - 4 * I
                            p = psc[sci % 3]
                            e_ = pT[sci % 3]
                            f_ = sc_f[sci % 3]
                            sci += 1
                            MM(kb, p[:], ksT[:, j * 128:(j + 1) * 128], q_[:, I * 512:(I + 1) * 512], True, False, r=[ksT, q_], w=[p])
                            MM(kb, p[:], Eall[:, j, :], SELT[:, I * 512:(I + 1) * 512], False, True, r=[Eall, SELT], w=[p])
                            if off >= -1:
                                TT(kb, f_[:], p[:], bS[:, off + 1, :], ALU.add, r=[p, bS], w=[f_])
                                ACT(kb, e_[:], f_[:], AF.Exp, r=[f_], w=[e_])
                            else:
                                ACT(kb, e_[:], p[:], AF.Exp, r=[p, CH], w=[e_], bias=CH[:, h:h + 1], scale=1.0)
                            for qs in range(4):
                                if j > 4 * I + qs:
                                    continue
                                MM(kb, pacc[qs][:, 0:65], e_[:, qs * 128:(qs + 1) * 128], VSx[:, j, :], j == 0, j == 4 * I + qs, r=[e_, VSx, ("vs1",)], w=[pacc[qs]])
                        for qs in range(4):
                            finish(pacc[qs], 65, h, 1, I, qs, False)
                        for j in range(max(0, 4 * I - 4), 4 * I + 4):
                            off = j - 4 * I
                            p = psc[sci % 3]
                            e_ = pT[sci % 3]
                            f_ = sc_f[sci % 3]
                            sci += 1
                            MM(kb, p[:], kwT[:, j * 128:(j + 1) * 128], q_[:, I * 512:(I + 1) * 512], True, True, r=[kwT, q_], w=[p])
                            TT(kb, f_[:], p[:], bS[:, 5 + off + 4, :], ALU.add, r=[p, bS], w=[f_])
                            ACT(kb, e_[:], f_[:], AF.Exp, r=[f_], w=[e_])
                            for qs in range(4):
                                lo = max(0, 4 * I + qs - 4)
                                hi = 4 * I + qs
                                if j < lo or j > hi:
                                    continue
                                MM(kb, pacc[qs][:, 0:65], e_[:, qs * 128:(qs + 1) * 128], VWx[:, j, :], j == lo, j == hi, r=[e_, VWx, ("vw1",)], w=[pacc[qs]])
                        for qs in range(4):
                            finish(pacc[qs], 65, h, 2, I, qs, False)
                kb.dma("sp", mix_d[:, g * 192:(g + 1) * 192].rearrange("(t p) c -> p t c", p=128), Y[:], r=[("Y", t_, hl_) for t_ in range(32) for hl_ in range(3)])
                kb.barrier()


LAYER_KIND = ["nsa", "gla", "rwkv", "nsa"]
LAYER_COLS = [NSA_COLS, GLA_COLS, RWKV_COLS, NSA_COLS]


def gathered_specs():
    sp = [("nsa_w_in0", D, NSA_COLS, F32), ("nsa_w_in1", D, NSA_COLS, F32), ("gla_w_in", D, GLA_COLS, F32), ("rwkv_w_in", D, RWKV_COLS, F32),
          ("cmp_w1_0", 4096, 256, F32), ("cmp_w1_1", 4096, 256, F32), ("tab", 216 * 128, 512, F32)]
    for l in range(DEPTH):
        sp += [("mem_w_kv%d" % l, D, 512, F32), ("w_out%d" % l, D, D, F32)]
    for l in range(DEPTH):
        sp += [("wg%d" % l, NE * D, 2 * D, BF16), ("wd%d" % l, NE * D, D, BF16)]
    return sp


SMALL_SPECS = dict(
    ident=[128, 128], glac=[3, 128, 128], nsa_AB=[128, 2, 32, 64], nsa_c2s=[128, 2, 64], nsa_E=[64, 32, 128], rel31=[12],
    nsa_gate_b=[2, 36], nsa_pe=[2, 2, 32, 64], nsa_w2=[2, 2, 256, 64],
    gla_w_a2=[16, 384], gla_b_a=[384], gla_b_og=[768], gla_norm_w=[192],
    rwkv_mu=[2560], rwkv_w0=[768], rwkv_w2=[64, 768], rwkv_a0=[768], rwkv_a2=[64, 768], rwkv_g2=[128, 768], rwkv_k_k=[768], rwkv_k_a=[768],
    rwkv_r_k=[768], rwkv_ln_w=[768], rwkv_ln_b=[768],
    ln1_g=[4, D], ln1_b=[4, D], ln2_g=[4, D], ln2_b=[4, D], router_w=[4, D, NE], router_b=[4, NE], exp_b_gu=[4, NE, 2 * D], exp_b_dn=[4, NE, D],
)


MODE = "replicate"
NUSE = 4


def build_program(depth=DEPTH, mode=MODE, nseq=None):
    nc = bass.Bass("TRN2", target_bir_lowering=False)
    if nseq is None:
        nseq = 1 if mode == "allgather" else NCORES // NUSE

    def ein(n, shape, dt=F32):
        return nc.dram_tensor(n, list(shape), dt, kind="ExternalInput").ap()

    x_all = ein("x", [nseq, S, D])
    mem_all = ein("mem", [nseq, 256, D])
    sm = {k: ein(k, v) for k, v in SMALL_SPECS.items()}
    y_all = nc.dram_tensor("y", [nseq, S, D], F32, kind="ExternalOutput").ap()
    kb = KB(nc)
    C = Consts(kb, sm["ident"])
    G = {}
    for (name, rows, cols, dt) in gathered_specs():
        step = 1024
        if mode == "allgather":
            rs = rows // NCORES
            src = ein("sh_" + name, [rs, cols])
            bounce = kb.dram([rs, cols], dt, "bn_" + name).ap()
            full = kb.dram([rows, cols], dt, "g_" + name).ap()
            for r0 in range(0, rs, step):
                r1 = min(rs, r0 + step)
                kb.dma("pool", bounce[r0:r1, :], src[r0:r1, :], w=[("bn", name, r0)])
            kb.coll(name, "AllGather", [bounce], [full], r=[("bn", name, r0) for r0 in range(0, rs, step)], w=[("g", name)])
            G[name] = full
        else:
            src = ein("sh_" + name, [rows, cols])
            if dt == F32:
                G[name] = src
            else:
                full = kb.dram([rows, cols], dt, "g_" + name).ap()
                for r0 in range(0, rows, step):
                    kb.dma("pool", full[r0:r0 + step, :], src[r0:r0 + step, :], w=[("g", name, r0)])
                G[name] = full

    def need(nm):
        if mode == "allgather":
            kb.need(nm)

    hF = kb.dram([RWKV_COLS, S], F32, "hF").ap()
    hT = kb.dram([S, RWKV_COLS], F32, "hT").ap()
    mix = kb.dram([S, D], F32, "mix").ap()
    x1 = kb.dram([S, D], F32, "x1").ap()
    xs = [kb.dram([S, D], F32, "xs%d" % i).ap() for i in range(2)]
    rscr = {k: kb.dram([S, TOKW], F32, "rw" + k).ap() for k in "WKABRVGO"}
    rscr["VP"] = kb.dram([128, 6, S], F32, "rwVP").ap()
    if mode != "allgather":
        kb.barrier()
    for sq in range(nseq):
        x_in = x_all[sq]
        mem_d = mem_all[sq]
        y_d = y_all[sq]
        nsa_i = 0
        for l in range(depth):
            kind = LAYER_KIND[l]
            ncols = LAYER_COLS[l]
            hFv, hTv = hF[0:ncols, :], hT[:, 0:ncols]
            if kind == "nsa":
                wname = "nsa_w_in%d" % nsa_i
            elif kind == "gla":
                wname = "gla_w_in"
            else:
                wname = "rwkv_w_in"
            need(wname)
            stage_proj(kb, C, x_in, G[wname], ncols, hFv, hTv)
            if kind == "nsa":
                j = nsa_i
                nsa_i += 1
                need("tab")
                need("cmp_w1_%d" % j)
                P = dict(gate_b=sm["nsa_gate_b"][j], pe=sm["nsa_pe"][j], w1=G["cmp_w1_%d" % j].rearrange("(a r) c -> a r c", a=2), w2=sm["nsa_w2"][j],
                         rel31=sm["rel31"], AB=sm["nsa_AB"], c2s=sm["nsa_c2s"], E=sm["nsa_E"])
                stage_nsa(kb, C, hFv, hTv, P, G["tab"].rearrange("(o k) q -> o (k q)", k=128), mix)
            elif kind == "gla":
                stage_gla(kb, C, hFv, hTv, sm["gla_w_a2"], sm["gla_b_a"], sm["gla_b_og"], sm["gla_norm_w"], sm["glac"], mix)
            else:
                P = {k: sm["rwkv_" + k] for k in ("mu", "w0", "w2", "a0", "a2", "g2", "k_k", "k_a", "r_k", "ln_w", "ln_b")}
                stage_rwkv(kb, C, hTv, P, mix, rscr)
            need("mem_w_kv%d" % l)
            stage_memattn(kb, C, mem_d, G["mem_w_kv%d" % l], hFv, ncols - MEMW, mix)
            need("w_out%d" % l)
            stage_outproj_ln(kb, C, mix, G["w_out%d" % l], x_in, sm["ln1_g"][l], sm["ln1_b"][l], x1)
            need("wg%d" % l)
            need("wd%d" % l)
            xo = y_d if l == depth - 1 else xs[l % 2]
            stage_moe(kb, C, x1, sm["router_w"][l], sm["router_b"][l], G["wg%d" % l].rearrange("(e r) n -> e r n", e=NE), G["wd%d" % l].rearrange("(e r) n -> e r n", e=NE),
                      sm["exp_b_gu"][l], sm["exp_b_dn"][l], sm["ln2_g"][l], sm["ln2_b"][l], xo)
            x_in = xo
    kb.emit()
    return nc


def host_inputs(inp, mode=MODE):
    f = lambda a: np.ascontiguousarray(np.asarray(a, dtype=np.float32))
    AB, c2s, E = nsa_static_np()
    rel = f(inp["rel_table"])
    small = dict(
        ident=np.eye(128, dtype=np.float32), glac=gla_consts_np(), nsa_AB=AB, nsa_c2s=c2s, nsa_E=E, rel31=f(rel[31]),
        nsa_gate_b=f(inp["nsa_gate_b"]), nsa_pe=f(inp["nsa_cmp_pe"]), nsa_w2=f(inp["nsa_cmp_w2"]),
        gla_w_a2=f(inp["gla_w_a2"][0]), gla_b_a=f(inp["gla_b_a"][0]), gla_b_og=f(inp["gla_b_og"][0]), gla_norm_w=f(inp["gla_norm_w"][0]),
        ln1_g=f(inp["ln1_g"]), ln1_b=f(inp["ln1_b"]), ln2_g=f(inp["ln2_g"]), ln2_b=f(inp["ln2_b"]),
        router_w=f(inp["router_w"]), router_b=f(inp["router_b"]), exp_b_gu=f(inp["exp_b_gu"]), exp_b_dn=f(inp["exp_b_dn"]),
    )
    for k in ("mu", "w0", "w2", "a0", "a2", "g2", "k_k", "k_a", "r_k", "ln_w", "ln_b"):
        small["rwkv_" + k] = f(inp["rwkv_" + k][0]).reshape(SMALL_SPECS["rwkv_" + k])
    full = {
        "nsa_w_in0": f(inp["nsa_w_in"][0]), "nsa_w_in1": f(inp["nsa_w_in"][1]), "gla_w_in": f(inp["gla_w_in"][0]), "rwkv_w_in": f(inp["rwkv_w_in"][0]),
        "cmp_w1_0": f(inp["nsa_cmp_w1"][0]).reshape(4096, 256), "cmp_w1_1": f(inp["nsa_cmp_w1"][1]).reshape(4096, 256),
        "tab": nsa_tables_np(rel).reshape(216 * 128, 512),
    }
    for l in range(DEPTH):
        full["mem_w_kv%d" % l] = f(inp["mem_w_kv"][l])
        full["w_out%d" % l] = f(inp["w_out"][l])
    maps = []
    x = np.asarray(inp["x"], dtype=np.float32)
    mem = np.asarray(inp["mem"], dtype=np.float32)
    wgu = np.asarray(inp["exp_w_gu"], dtype=np.float32)
    wdn = np.asarray(inp["exp_w_dn"], dtype=np.float32)
    if mode == "allgather":
        for c in range(NCORES):
            m = {"x": f(x[c:c + 1]), "mem": f(mem[c:c + 1])}
            m.update(small)
            for k, v in full.items():
                rs = v.shape[0] // NCORES
                m["sh_" + k] = np.ascontiguousarray(v[c * rs:(c + 1) * rs])
            for l in range(DEPTH):
                m["sh_wg%d" % l] = np.ascontiguousarray(wgu[l, 4 * c:4 * c + 4]).reshape(4 * D, 2 * D)
                m["sh_wd%d" % l] = np.ascontiguousarray(wdn[l, 4 * c:4 * c + 4]).reshape(4 * D, D)
            maps.append(m)
    else:
        nseq = NCORES // NUSE
        shared = dict(small)
        for k, v in full.items():
            shared["sh_" + k] = v
        for l in range(DEPTH):
            shared["sh_wg%d" % l] = np.ascontiguousarray(wgu[l]).reshape(NE * D, 2 * D)
            shared["sh_wd%d" % l] = np.ascontiguousarray(wdn[l]).reshape(NE * D, D)
        for c in range(NUSE):
            m = {"x": f(x[c * nseq:(c + 1) * nseq]), "mem": f(mem[c * nseq:(c + 1) * nseq])}
            m.update(shared)
            maps.append(m)
    return maps


_NC_CACHE = {}


def kernel(**inputs):
    if "nc" not in _NC_CACHE:
        _NC_CACHE["nc"] = build_program()
    nc = _NC_CACHE["nc"]
    maps = host_inputs(inputs)
    res = run_bass_kernel_spmd(nc, maps, core_ids=list(range(len(maps))))
    return np.concatenate([np.asarray(r["y"], dtype=np.float32) for r in res.results], axis=0)
```

```python
import contextlib
import numpy as np
import concourse.bass as bass
import concourse.mybir as mybir
from concourse.bass_utils import run_bass_kernel_spmd

F32 = mybir.dt.float32
BF16 = mybir.dt.bfloat16
ALU = mybir.AluOpType
AF = mybir.ActivationFunctionType
AX = mybir.AxisListType

D = 1024
S = 4096
NCORES = 8
DEPTH = 4
MEMW = 256
TOKW = 768
ALPHA = (2 * DEPTH) ** 0.25
LN_EPS = 1e-5
NSA_COLS = 2596
GLA_COLS = 2576
RWKV_COLS = 2816
NE = 32


class Op:
    __slots__ = ("eng", "fn", "deps", "need_inc", "val", "is_dma", "dsem", "dval", "coll")

    def __init__(self, eng, fn, is_dma):
        self.eng, self.fn, self.is_dma = eng, fn, is_dma
        self.deps, self.need_inc, self.val = [], False, 0
        self.dsem, self.dval = None, 0
        self.coll = False


class Tl:
    def __init__(self, t):
        self.t = t

    def __getitem__(self, idx):
        return self.t[idx]


class KB:
    ENGS = ("pe", "dve", "act", "pool", "sp")
    KD = 8

    def __init__(self, nc):
        self.nc = nc
        self.es = contextlib.ExitStack()
        self.e = {"pe": nc.tensor, "dve": nc.vector, "act": nc.scalar, "pool": nc.gpsimd, "sp": nc.sync}
        self.ops = []
        self.last = {e: None for e in self.ENGS}
        self.res = {}
        self.pending = {e: [] for e in self.ENGS}
        self.nd = {e: 0 for e in self.ENGS}
        self.slot_last = {}
        self.csem = {e: nc.alloc_semaphore(name="c_" + e) for e in self.ENGS}
        self.dsems = {}
        for q in ("sp", "pool", "act"):
            for s in range(self.KD):
                self.dsems[(q, s)] = nc.alloc_semaphore(name="d_%s%d" % (q, s))
        self.uid = 0
        self.colls = {}
        self._clear_sems()
        nc.all_engine_barrier()

    def _clear_sems(self):
        for h in list(self.csem.values()) + list(self.dsems.values()):
            self.nc.gpsimd.sem_clear(h)

    def name(self, p):
        self.uid += 1
        return "%s_%d" % (p, self.uid)

    def sb(self, st, shape, dt, name="sb"):
        return Tl(st.enter_context(self.nc.sbuf_tensor(self.name(name), list(shape), dt)))

    def ps(self, st, shape, dt, name="ps"):
        return Tl(st.enter_context(self.nc.psum_tensor(self.name(name), list(shape), dt)))

    def dram(self, shape, dt, name="scr"):
        return self.nc.dram_tensor(self.name(name), list(shape), dt)

    def coll(self, name, kind, ins, outs, r=(), w=()):
        sem = self.nc.alloc_semaphore(name="cc_" + name)
        self.nc.gpsimd.sem_clear(sem)
        self.dsems[("cc", name)] = sem
        o = self.op("pool", lambda E: E.collective_compute(kind, ALU.bypass, replica_groups=[list(range(NCORES))], ins=ins, outs=outs), r=r, w=w, dma=True, coll=("cc", name))
        self.colls[name] = o
        return o

    def need(self, name):
        o = self.colls[name]
        for e in self.ENGS:
            self.pending[e] = list(self.pending[e]) + [o]

    def op(self, eng, fn, r=(), w=(), dma=False, coll=None):
        o = Op(eng, fn, dma)
        deps = list(self.pending[eng])
        self.pending[eng] = []
        for k in r:
            st = self.res.get(k)
            if st:
                deps += st[0]
        for k in w:
            st = self.res.get(k)
            if st:
                deps += st[0]
                deps += st[1]
        seen = set()
        for d in deps:
            if d is o or id(d) in seen:
                continue
            if (not d.is_dma) and d.eng == eng and eng == "pe":
                continue
            seen.add(id(d))
            o.deps.append(d)
            if not d.is_dma:
                d.need_inc = True
        if coll is not None:
            o.coll = True
            o.dsem = coll
            o.dval = 1
        elif dma:
            slot = self.nd[eng] % self.KD
            o.dsem = (eng, slot)
            o.dval = 16 * (self.nd[eng] // self.KD + 1)
            prev = self.slot_last.get((eng, slot))
            if prev is not None and id(prev) not in seen:
                o.deps.append(prev)
            self.slot_last[(eng, slot)] = o
            self.nd[eng] += 1
        for k in r:
            if k in w:
                continue
            st = self.res.setdefault(k, ([], []))
            if not dma:
                st[1][:] = [x for x in st[1] if x.is_dma or x.eng != eng]
            st[1].append(o)
        for k in w:
            self.res[k] = ([o], [])
        self.ops.append(o)
        if not dma:
            self.last[eng] = o
        return o

    def dma(self, q, out, in_, r=(), w=(), **kw):
        return self.op(q, lambda E: E.dma_start(out=out, in_=in_, **kw), r=r, w=w, dma=True)

    def barrier(self):
        targets = [o for o in self.last.values() if o is not None and not o.is_dma]
        targets += list(self.slot_last.values())
        for e in self.ENGS:
            self.pending[e] = list(self.pending[e]) + targets
        self.res = {}

    def emit(self):
        self.barrier()
        final = self.pending["sp"]
        for d in final:
            if not d.is_dma:
                d.need_inc = True
        cnt = {e: 0 for e in self.ENGS}
        for o in self.ops:
            if (not o.is_dma) and o.need_inc:
                cnt[o.eng] += 1
                o.val = cnt[o.eng]
        waited = {}

        def do_wait(engname, E, d):
            if d.is_dma:
                key, val, sem = ("d",) + d.dsem, d.dval, self.dsems[d.dsem]
            else:
                key, val, sem = ("c", d.eng), d.val, self.csem[d.eng]
            if waited.get((engname, key), 0) >= val:
                return
            waited[(engname, key)] = val
            E.wait_ge(sem, val)

        for o in self.ops:
            E = self.e[o.eng]
            for d in o.deps:
                do_wait(o.eng, E, d)
            ins = o.fn(E)
            if o.coll:
                ins.then_inc(self.dsems[o.dsem])
            elif o.is_dma:
                ins.then_inc(self.dsems[o.dsem], 16)
            elif o.need_inc:
                ins.then_inc(self.csem[o.eng], 1)
        for d in final:
            do_wait("sp", self.e["sp"], d)
        self.nc.all_engine_barrier()
        self._clear_sems()
        self.nc.all_engine_barrier()
        self.es.close()


def evac(kb, i, out, in_, r, w):
    if i % 2 == 0:
        kb.op("dve", lambda E: E.tensor_copy(out=out, in_=in_), r=r, w=w)
    else:
        kb.op("act", lambda E: E.copy(out=out, in_=in_), r=r, w=w)


class Consts:
    def __init__(self, kb, ident_d):
        es = kb.es
        self.ident_f = kb.sb(es, [128, 128], F32, "identf")
        self.ident_b = kb.sb(es, [128, 128], BF16, "identb")
        kb.dma("sp", self.ident_f[:], ident_d, w=[self.ident_f])
        kb.op("dve", lambda E: E.tensor_copy(out=self.ident_b[:], in_=self.ident_f[:]), r=[self.ident_f], w=[self.ident_b])


def load_xT(kb, C, x_d, t0, nsub, xin, xb, xT, pst, ncolchunks=8, q="sp", off=0):
    cols = ncolchunks * 128
    kb.dma(q, xin[:, 0:nsub, 0:cols], x_d[t0:t0 + nsub * 128, 0:cols].rearrange("(s p) c -> p s c", p=128), w=[xin])
    kb.op("act", lambda E: E.copy(out=xb[:, 0:nsub, 0:cols], in_=xin[:, 0:nsub, 0:cols]), r=[xin], w=[xb])
    for kc in range(ncolchunks):
        p = pst[kc % len(pst)]
        for s in range(nsub):
            kb.op("pe", lambda E, s=s, kc=kc, p=p: E.transpose(out=p[:, s * 128:(s + 1) * 128], in_=xb[:, s, kc * 128:(kc + 1) * 128], identity=C.ident_b[:]),
                  r=[xb, C.ident_b], w=[p])
        evac(kb, kc, xT[:, kc, off:off + nsub * 128], p[:, 0:nsub * 128], r=[p], w=[xT])


def stage_proj(kb, C, x_d, w_d, ncols, hF_d, hT_d):
    with contextlib.ExitStack() as st:
        W = kb.sb(st, [128, 8, ncols], BF16, "W")
        for kc in range(8):
            kb.dma("pool", W[:, kc, :], w_d[kc * 128:(kc + 1) * 128, :], w=[W])
        xin = [kb.sb(st, [128, 4, D], F32, "xin") for _ in range(2)]
        xb = [kb.sb(st, [128, 4, D], BF16, "xb") for _ in range(2)]
        xT = [kb.sb(st, [128, 8, 512], BF16, "xT") for _ in range(2)]
        pst = [kb.ps(st, [128, 1024], BF16, "pst") for _ in range(2)]
        pm = [kb.ps(st, [128, 512], F32, "pm") for _ in range(4)]
        oF = [kb.sb(st, [128, 512], F32, "oF") for _ in range(3)]
        oT = [kb.sb(st, [128, ncols], F32, "oT") for _ in range(2)]
        nfc = (ncols + 127) // 128
        ncb = (ncols + 511) // 512
        cnt = 0
        for tt in range(S // 512):
            b = tt % 2
            load_xT(kb, C, x_d, tt * 512, 4, xin[b], xb[b], xT[b], pst)
            for c in range(nfc):
                mc = min(128, ncols - c * 128)
                p = pm[cnt % 4]
                o = oF[cnt % 3]
                for kc in range(8):
                    kb.op("pe", lambda E, p=p, c=c, mc=mc, kc=kc, b=b: E.matmul(out=p[0:mc, :], lhsT=W[:, kc, c * 128:c * 128 + mc], rhs=xT[b][:, kc, :], start=(kc == 0), stop=(kc == 7)),
                          r=[W, xT[b]], w=[p])
                evac(kb, cnt, o[0:mc, :], p[0:mc, :], r=[p], w=[o])
                kb.dma("sp", hF_d[c * 128:c * 128 + mc, tt * 512:(tt + 1) * 512], o[0:mc, :], r=[o])
                cnt += 1
            for s in range(4):
                ot = oT[s % 2]
                for cb in range(ncb):
                    nn = min(512, ncols - cb * 512)
                    p = pm[cnt % 4]
                    for kc in range(8):
                        kb.op("pe", lambda E, p=p, cb=cb, nn=nn, kc=kc, b=b, s=s: E.matmul(out=p[:, 0:nn], lhsT=xT[b][:, kc, s * 128:(s + 1) * 128], rhs=W[:, kc, cb * 512:cb * 512 + nn], start=(kc == 0), stop=(kc == 7)),
                              r=[W, xT[b]], w=[p])
                    evac(kb, cnt, ot[:, cb * 512:cb * 512 + nn], p[:, 0:nn], r=[p], w=[ot])
                    cnt += 1
                t0 = tt * 512 + s * 128
                kb.dma("pool", hT_d[t0:t0 + 128, :], ot[:], r=[ot])
        kb.barrier()


class LNBufs:
    def __init__(self, kb, st, g_d, b_d):
        self.g = kb.sb(st, [128, D], F32, "lng")
        self.b = kb.sb(st, [128, D], F32, "lnb")
        kb.dma("sp", self.g[:], g_d.partition_broadcast(128), w=[self.g])
        kb.dma("sp", self.b[:], b_d.partition_broadcast(128), w=[self.b])
        self.stats = kb.sb(st, [128, 2, 6], F32, "lnst")
        self.mv = kb.sb(st, [128, 2], F32, "lnmv")
        self.acc = kb.sb(st, [128, 2], F32, "lnacc")
        self.rstd = kb.sb(st, [128, 1], F32, "lnrs")


def ln_tile(kb, L, z, zap, out, outap):
    kb.op("act", lambda E: E.activation(out=outap, in_=zap, func=AF.Identity, accum_out=L.acc[:, 0:1]), r=[z], w=[out, L.acc])
    kb.op("act", lambda E: E.activation(out=outap, in_=zap, func=AF.Square, accum_out=L.acc[:, 1:2]), r=[z], w=[out, L.acc])
    kb.op("act", lambda E: E.mul(out=L.mv[:], in_=L.acc[:], mul=1.0 / D), r=[L.acc], w=[L.mv])
    kb.op("dve", lambda E: E.scalar_tensor_tensor(out=L.rstd[:], in0=L.mv[:, 0:1], scalar=-1.0, in1=L.mv[:, 0:1], op0=ALU.mult, op1=ALU.mult), r=[L.mv], w=[L.rstd])
    kb.op("dve", lambda E: E.tensor_tensor(out=L.rstd[:], in0=L.rstd[:], in1=L.mv[:, 1:2], op=ALU.add), r=[L.mv, L.rstd], w=[L.rstd])
    kb.op("act", lambda E: E.activation(out=L.rstd[:], in_=L.rstd[:], func=AF.Sqrt, bias=L.epsb[:], scale=1.0), r=[L.rstd, L.epsb], w=[L.rstd])
    kb.op("dve", lambda E: E.reciprocal(out=L.rstd[:], in_=L.rstd[:]), r=[L.rstd], w=[L.rstd])
    kb.op("dve", lambda E: E.tensor_scalar(out=zap, in0=zap, scalar1=L.mv[:, 0:1], scalar2=L.rstd[:, 0:1], op0=ALU.subtract, op1=ALU.mult), r=[z, L.mv, L.rstd], w=[z])
    kb.op("dve", lambda E: E.tensor_tensor(out=zap, in0=zap, in1=L.g[:], op=ALU.mult), r=[z, L.g], w=[z])
    kb.op("dve", lambda E: E.tensor_tensor(out=outap, in0=zap, in1=L.b[:], op=ALU.add), r=[z, L.b], w=[out])


def make_eps(kb, st, L):
    L.epsb = kb.sb(st, [128, 1], F32, "eps")
    kb.op("dve", lambda E: E.memset(L.epsb[:], LN_EPS), w=[L.epsb])


def stage_memattn(kb, C, mem_d, wkv_d, hF_d, qc0, mix_d):
    with contextlib.ExitStack() as st:
        W = kb.sb(st, [128, 8, 512], BF16, "Wkv")
        for kc in range(8):
            kb.dma("pool", W[:, kc, :], wkv_d[kc * 128:(kc + 1) * 128, :], w=[W])
        xin = kb.sb(st, [128, 2, D], F32, "min")
        xb = kb.sb(st, [128, 2, D], BF16, "mb")
        memT = kb.sb(st, [128, 8, 256], BF16, "memT")
        pst = [kb.ps(st, [128, 1024], BF16, "pst") for _ in range(2)]
        load_xT(kb, C, mem_d, 0, 2, xin, xb, memT, pst)
        kT = kb.sb(st, [64, 4, 256], BF16, "kT")
        Vx = kb.sb(st, [128, 2, 4, 65], BF16, "Vx")
        kb.op("dve", lambda E: E.memset(Vx[:], 1.0), w=[Vx])
        pm = [kb.ps(st, [128, 512], F32, "pm") for _ in range(2)]
        for h in range(4):
            p = pm[h % 2]
            for kc in range(8):
                kb.op("pe", lambda E, p=p, h=h, kc=kc: E.matmul(out=p[0:64, 0:256], lhsT=W[:, kc, h * 64:(h + 1) * 64], rhs=memT[:, kc, :], start=(kc == 0), stop=(kc == 7)), r=[W, memT], w=[p])
            evac(kb, h, kT[:, h, :], p[0:64, 0:256], r=[p], w=[kT])
        for mc in range(2):
            p = pm[mc % 2]
            for kc in range(8):
                kb.op("pe", lambda E, p=p, mc=mc, kc=kc: E.matmul(out=p[:, 0:256], lhsT=memT[:, kc, mc * 128:(mc + 1) * 128], rhs=W[:, kc, 256:512], start=(kc == 0), stop=(kc == 7)), r=[W, memT], w=[p])
            kb.op("dve", lambda E, p=p, mc=mc: E.tensor_copy(out=Vx[:, mc, :, 0:64], in_=p[:, 0:256].rearrange("p (h d) -> p h d", d=64)), r=[p], w=[Vx])
        qm = [kb.sb(st, [64, 4, 512], BF16, "qm") for _ in range(2)]
        pT = [kb.sb(st, [128, 512], BF16, "pT") for _ in range(4)]
        po = [kb.ps(st, [128, 4, 65], F32, "po") for _ in range(2)]
        ym = [kb.sb(st, [128, 4, 256], F32, "ym") for _ in range(2)]
        rc = [kb.sb(st, [128, 4], F32, "rc") for _ in range(2)]
        cnt = 0
        for tt in range(S // 512):
            q = qm[tt % 2]
            y = ym[tt % 2]
            kb.dma("pool", q[:], hF_d[qc0:qc0 + 256, tt * 512:(tt + 1) * 512].rearrange("(h d) t -> d h t", d=64), w=[q])
            for h in range(4):
                pts = []
                for mc in range(2):
                    p = pm[cnt % 2]
                    t = pT[cnt % 4]
                    cnt += 1
                    kb.op("pe", lambda E, p=p, h=h, mc=mc, q=q: E.matmul(out=p[:], lhsT=kT[:, h, mc * 128:(mc + 1) * 128], rhs=q[:, h, :], start=True, stop=True), r=[kT, q], w=[p])
                    kb.op("act", lambda E, p=p, t=t: E.activation(out=t[:], in_=p[:], func=AF.Exp, scale=0.125), r=[p], w=[t])
                    pts.append(t)
                o = po[h % 2]
                for qs in range(4):
                    for mc in range(2):
                        kb.op("pe", lambda E, o=o, qs=qs, mc=mc, h=h, t=pts[mc]: E.matmul(out=o[:, qs, :], lhsT=t[:, qs * 128:(qs + 1) * 128], rhs=Vx[:, mc, h, :], start=(mc == 0), stop=(mc == 1)), r=[pts[mc], Vx], w=[o])
                r_ = rc[h % 2]
                kb.op("dve", lambda E, o=o, r_=r_: E.reciprocal(out=r_[:], in_=o[:, :, 64]), r=[o], w=[r_])
                for qs in range(4):
                    kb.op("dve", lambda E, o=o, r_=r_, qs=qs, h=h, y=y: E.tensor_scalar(out=y[:, qs, h * 64:(h + 1) * 64], in0=o[:, qs, 0:64], scalar1=r_[:, qs:qs + 1], scalar2=None, op0=ALU.mult), r=[o, r_], w=[y])
            kb.dma("sp", mix_d[tt * 512:(tt + 1) * 512, TOKW:D].rearrange("(s p) c -> p s c", p=128), y[:], r=[y])
        kb.barrier()


def stage_outproj_ln(kb, C, mix_d, wout_d, xres_d, g_d, b_d, xout_d):
    with contextlib.ExitStack() as st:
        W = kb.sb(st, [128, 8, D], BF16, "Wo")
        for kc in range(8):
            kb.dma("pool", W[:, kc, :], wout_d[kc * 128:(kc + 1) * 128, :], w=[W])
        L = LNBufs(kb, st, g_d, b_d)
        make_eps(kb, st, L)
        xin = [kb.sb(st, [128, 4, D], F32, "xin") for _ in range(2)]
        xb = [kb.sb(st, [128, 4, D], BF16, "xb") for _ in range(2)]
        xT = [kb.sb(st, [128, 8, 512], BF16, "xT") for _ in range(2)]
        xr = [kb.sb(st, [128, 4, D], F32, "xr") for _ in range(2)]
        z = [kb.sb(st, [128, D], F32, "z") for _ in range(2)]
        zo = [kb.sb(st, [128, D], F32, "zo") for _ in range(2)]
        pst = [kb.ps(st, [128, 1024], BF16, "pst") for _ in range(2)]
        pm = [kb.ps(st, [128, 512], F32, "pm") for _ in range(4)]
        cnt = 0
        for tt in range(S // 512):
            b = tt % 2
            load_xT(kb, C, mix_d, tt * 512, 4, xin[b], xb[b], xT[b], pst)
            kb.dma("pool", xr[b][:], xres_d[tt * 512:(tt + 1) * 512, :].rearrange("(s p) c -> p s c", p=128), w=[xr[b]])
            for s in range(4):
                zz = z[s % 2]
                oo = zo[s % 2]
                for hh in range(2):
                    p = pm[cnt % 4]
                    cnt += 1
                    for kc in range(8):
                        kb.op("pe", lambda E, p=p, kc=kc, b=b, s=s, hh=hh: E.matmul(out=p[:], lhsT=xT[b][:, kc, s * 128:(s + 1) * 128], rhs=W[:, kc, hh * 512:(hh + 1) * 512], start=(kc == 0), stop=(kc == 7)), r=[xT[b], W], w=[p])
                    kb.op("dve", lambda E, p=p, zz=zz, b=b, s=s, hh=hh: E.scalar_tensor_tensor(out=zz[:, hh * 512:(hh + 1) * 512], in0=xr[b][:, s, hh * 512:(hh + 1) * 512], scalar=ALPHA, in1=p[:], op0=ALU.mult, op1=ALU.add), r=[xr[b], p], w=[zz])
                ln_tile(kb, L, zz, zz[:], oo, oo[:])
                t0 = tt * 512 + s * 128
                kb.dma("sp", xout_d[t0:t0 + 128, :], oo[:], r=[oo])
        kb.barrier()


def stage_moe(kb, C, x1_d, wr_d, br_d, wg_d, wd_d, bgu_d, bdn_d, g_d, b_d, xout_d):
    with contextlib.ExitStack() as st:
        Wr = kb.sb(st, [128, 8, NE], F32, "Wr")
        kb.dma("sp", Wr[:], wr_d.rearrange("(kc p) e -> p kc e", p=128), w=[Wr])
        brb = kb.sb(st, [128, NE], F32, "brb")
        kb.dma("sp", brb[:], br_d.partition_broadcast(128), w=[brb])
        bdn = kb.sb(st, [NE, D], F32, "bdn")
        kb.dma("sp", bdn[:], bdn_d, w=[bdn])
        bguT = kb.sb(st, [128, 16, NE], F32, "bguT")
        with contextlib.ExitStack() as st0:
            braw = kb.sb(st0, [NE, 2 * D], F32, "braw")
            kb.dma("sp", braw[:], bgu_d, w=[braw])
            pb = kb.ps(st0, [128, 16, NE], F32, "pb")
            for c in range(16):
                kb.op("pe", lambda E, c=c: E.transpose(out=pb[:, c, :], in_=braw[:, c * 128:(c + 1) * 128], identity=C.ident_f[0:NE, 0:NE]), r=[braw, C.ident_f], w=[pb])
            kb.op("dve", lambda E: E.tensor_copy(out=bguT[:], in_=pb[:]), r=[pb], w=[bguT])
            kb.barrier()
        L = LNBufs(kb, st, g_d, b_d)
        make_eps(kb, st, L)
        Wg = [kb.sb(st, [128, 8, 2 * D], BF16, "Wg") for _ in range(2)]
        Wd = [kb.sb(st, [128, 8, D], BF16, "Wd") for _ in range(2)]
        xT = kb.sb(st, [128, 8, 1024], BF16, "xT")
        yacc = [kb.sb(st, [128, D], F32, "yacc") for _ in range(8)]
        gates = kb.sb(st, [128, 8, NE], F32, "gates")
        gT = kb.sb(st, [NE, 8, 128], F32, "gT")
        ecnt = 0
        for sup in range(S // 1024):
            T0 = sup * 1024
            with contextlib.ExitStack() as s1:
                xin = kb.sb(s1, [128, 4, D], F32, "xin")
                xb = kb.sb(s1, [128, 4, D], BF16, "xb")
                xTf = [kb.sb(s1, [128, 8, 128], F32, "xTf") for _ in range(2)]
                pst = [kb.ps(s1, [128, 1024], BF16, "pst") for _ in range(2)]
                ptf = [kb.ps(s1, [128, 4, 128], F32, "ptf") for _ in range(2)]
                plg = [kb.ps(s1, [128, 512], F32, "plg") for _ in range(2)]
                sm = [dict(lg=kb.sb(s1, [128, NE], F32, "lg"), m8=kb.sb(s1, [128, 8], F32, "m8"), nm=kb.sb(s1, [128, 1], F32, "nm"),
                           mk=kb.sb(s1, [128, NE], F32, "mk"), ex=kb.sb(s1, [128, NE], F32, "ex"), ss=kb.sb(s1, [128, 1], F32, "ss")) for _ in range(2)]
                for half in range(2):
                    load_xT(kb, C, x1_d, T0 + half * 512, 4, xin, xb, xT, pst, off=half * 512)
                    for s_ in range(4):
                        tile = half * 4 + s_
                        xf = xTf[tile % 2]
                        for kc in range(8):
                            p = ptf[kc // 4]
                            kb.op("pe", lambda E, p=p, kc=kc, s_=s_: E.transpose(out=p[:, kc % 4, :], in_=xin[:, s_, kc * 128:(kc + 1) * 128], identity=C.ident_f[:]), r=[xin, C.ident_f], w=[p])
                            if kc % 4 == 3:
                                evac(kb, kc // 4, xf[:, kc - 3:kc + 1, :], p[:], r=[p], w=[xf])
                        pl_ = plg[tile % 2]
                        for kc in range(8):
                            kb.op("pe", lambda E, pl_=pl_, kc=kc, xf=xf: E.matmul(out=pl_[:, 0:NE], lhsT=xf[:, kc, :], rhs=Wr[:, kc, :], start=(kc == 0), stop=(kc == 7)), r=[xf, Wr], w=[pl_])
                        m = sm[tile % 2]
                        kb.op("dve", lambda E, m=m, pl_=pl_: E.tensor_tensor(out=m["lg"][:], in0=pl_[:, 0:NE], in1=brb[:], op=ALU.add), r=[pl_, brb], w=[m["lg"]])
                        kb.op("dve", lambda E, m=m: E.max(out=m["m8"][:], in_=m["lg"][:]), r=[m["lg"]], w=[m["m8"]])
                        kb.op("dve", lambda E, m=m: E.tensor_scalar(out=m["mk"][:], in0=m["lg"][:], scalar1=m["m8"][:, 3:4], scalar2=None, op0=ALU.is_ge), r=[m["lg"], m["m8"]], w=[m["mk"]])
                        kb.op("dve", lambda E, m=m: E.tensor_scalar(out=m["nm"][:], in0=m["m8"][:, 0:1], scalar1=-1.0, scalar2=None, op0=ALU.mult), r=[m["m8"]], w=[m["nm"]])
                        kb.op("act", lambda E, m=m: E.activation(out=m["ex"][:], in_=m["lg"][:], func=AF.Exp, bias=m["nm"][:], scale=1.0), r=[m["lg"], m["nm"]], w=[m["ex"]])
                        kb.op("dve", lambda E, m=m: E.tensor_tensor(out=m["ex"][:], in0=m["ex"][:], in1=m["mk"][:], op=ALU.mult), r=[m["ex"], m["mk"]], w=[m["ex"]])
                        kb.op("dve", lambda E, m=m: E.tensor_reduce(out=m["ss"][:], in_=m["ex"][:], axis=AX.X, op=ALU.add), r=[m["ex"]], w=[m["ss"]])
                        kb.op("dve", lambda E, m=m: E.reciprocal(out=m["ss"][:], in_=m["ss"][:]), r=[m["ss"]], w=[m["ss"]])
                        kb.op("dve", lambda E, m=m, tile=tile: E.tensor_scalar(out=gates[:, tile, :], in0=m["ex"][:], scalar1=m["ss"][:, 0:1], scalar2=None, op0=ALU.mult), r=[m["ex"], m["ss"]], w=[gates])
                        pg_ = plg[tile % 2]
                        kb.op("pe", lambda E, pg_=pg_, tile=tile: E.transpose(out=pg_[0:NE, 0:128], in_=gates[:, tile, :], identity=C.ident_f[:]), r=[gates, C.ident_f], w=[pg_])
                        kb.op("act", lambda E, pg_=pg_, tile=tile: E.copy(out=gT[:, tile, :], in_=pg_[0:NE, 0:128]), r=[pg_], w=[gT])
                for tile in range(8):
                    for hh in range(2):
                        p = plg[(tile * 2 + hh) % 2]
                        kb.op("pe", lambda E, p=p, tile=tile, hh=hh: E.matmul(out=p[:], lhsT=gT[:, tile, :], rhs=bdn[:, hh * 512:(hh + 1) * 512], start=True, stop=True), r=[gT, bdn], w=[p])
                        evac(kb, hh, yacc[tile][:, hh * 512:(hh + 1) * 512], p[:], r=[p], w=[yacc[tile]])
                kb.barrier()
            with contextlib.ExitStack() as s2:
                actT = [kb.sb(s2, [128, 8, 512], BF16, "actT") for _ in range(2)]
                glu = [kb.sb(s2, [128, 512], F32, "glu") for _ in range(2)]
                sig = [kb.sb(s2, [128, 512], F32, "sig") for _ in range(2)]
                lin = [kb.sb(s2, [128, 512], F32, "lin") for _ in range(2)]
                pg = [kb.ps(s2, [128, 512], F32, "pg") for _ in range(2)]
                pl = [kb.ps(s2, [128, 512], F32, "pl") for _ in range(2)]
                py = [kb.ps(s2, [128, 512], F32, "py") for _ in range(2)]
                cnt = 0
                ycnt = 0
                for e in range(NE):
                    b = ecnt % 2
                    ecnt += 1
                    for hk in range(2):
                        kb.dma("sp", Wg[b][:, hk * 4:(hk + 1) * 4, :], wg_d[e, hk * 512:(hk + 1) * 512, :].rearrange("(kc p) n -> p kc n", p=128), w=[Wg[b]])
                    kb.dma("sp", Wd[b][:], wd_d[e].rearrange("(kc p) n -> p kc n", p=128), w=[Wd[b]])
                    for t5 in range(2):
                        aT = actT[(e * 2 + t5) % 2]
                        for fc in range(8):
                            i = cnt % 2
                            cnt += 1
                            for kc in range(8):
                                kb.op("pe", lambda E, i=i, kc=kc, fc=fc, b=b, t5=t5: E.matmul(out=pg[i][:], lhsT=Wg[b][:, kc, fc * 128:(fc + 1) * 128], rhs=xT[:, kc, t5 * 512:(t5 + 1) * 512], start=(kc == 0), stop=(kc == 7)), r=[Wg[b], xT], w=[pg[i]])
                            for kc in range(8):
                                kb.op("pe", lambda E, i=i, kc=kc, fc=fc, b=b, t5=t5: E.matmul(out=pl[i][:], lhsT=Wg[b][:, kc, D + fc * 128:D + (fc + 1) * 128], rhs=xT[:, kc, t5 * 512:(t5 + 1) * 512], start=(kc == 0), stop=(kc == 7)), r=[Wg[b], xT], w=[pl[i]])
                            kb.op("dve", lambda E, i=i, fc=fc, e=e: E.tensor_scalar(out=glu[i][:], in0=pg[i][:], scalar1=bguT[:, fc, e:e + 1], scalar2=7.0, op0=ALU.add, op1=ALU.min), r=[pg[i], bguT], w=[glu[i]])
                            kb.op("act", lambda E, i=i: E.activation(out=sig[i][:], in_=glu[i][:], func=AF.Sigmoid, scale=1.702), r=[glu[i]], w=[sig[i]])
                            kb.op("dve", lambda E, i=i, fc=fc, e=e: E.tensor_scalar(out=lin[i][:], in0=pl[i][:], scalar1=bguT[:, 8 + fc, e:e + 1], scalar2=7.0, op0=ALU.add, op1=ALU.min), r=[pl[i], bguT], w=[lin[i]])
                            kb.op("dve", lambda E, i=i: E.tensor_scalar(out=lin[i][:], in0=lin[i][:], scalar1=-7.0, scalar2=1.0, op0=ALU.max, op1=ALU.add), r=[lin[i]], w=[lin[i]])
                            kb.op("dve", lambda E, i=i: E.tensor_tensor(out=lin[i][:], in0=lin[i][:], in1=glu[i][:], op=ALU.mult), r=[lin[i], glu[i]], w=[lin[i]])
                            kb.op("dve", lambda E, i=i, fc=fc, aT=aT: E.tensor_tensor(out=aT[:, fc, :], in0=lin[i][:], in1=sig[i][:], op=ALU.mult), r=[lin[i], sig[i]], w=[aT])
                        for sub in range(4):
                            tile = t5 * 4 + sub
                            for hh in range(2):
                                p = py[ycnt % 2]
                                ycnt += 1
                                for fc in range(8):
                                    kb.op("pe", lambda E, p=p, fc=fc, sub=sub, hh=hh, b=b, aT=aT: E.matmul(out=p[:], lhsT=aT[:, fc, sub * 128:(sub + 1) * 128], rhs=Wd[b][:, fc, hh * 512:(hh + 1) * 512], start=(fc == 0), stop=(fc == 7)), r=[aT, Wd[b]], w=[p])
                                kb.op("dve", lambda E, p=p, tile=tile, hh=hh, e=e: E.scalar_tensor_tensor(out=yacc[tile][:, hh * 512:(hh + 1) * 512], in0=p[:], scalar=gates[:, tile, e:e + 1], in1=yacc[tile][:, hh * 512:(hh + 1) * 512], op0=ALU.mult, op1=ALU.add), r=[p, gates, yacc[tile]], w=[yacc[tile]])
                kb.barrier()
            with contextlib.ExitStack() as s3:
                xr = [kb.sb(s3, [128, D], F32, "xr") for _ in range(2)]
                zo = [kb.sb(s3, [128, D], F32, "zo") for _ in range(2)]
                for tile in range(8):
                    t0 = T0 + tile * 128
                    x_ = xr[tile % 2]
                    o_ = zo[tile % 2]
                    kb.dma("pool", x_[:], x1_d[t0:t0 + 128, :], w=[x_])
                    kb.op("dve", lambda E, x_=x_, tile=tile: E.scalar_tensor_tensor(out=yacc[tile][:], in0=x_[:], scalar=ALPHA, in1=yacc[tile][:], op0=ALU.mult, op1=ALU.add), r=[x_, yacc[tile]], w=[yacc[tile]])
                    ln_tile(kb, L, yacc[tile], yacc[tile][:], o_, o_[:])
                    kb.dma("sp", xout_d[t0:t0 + 128, :], o_[:], r=[o_])
                kb.barrier()


GLA_H, GLA_DK, GLA_DV = 4, 96, 192


def gla_consts_np():
    s = np.arange(128)[:, None]
    c = np.arange(128)[None, :]
    same = (s // 64) == (c // 64)
    tri_incl = np.where(same & (s <= c), -1.0 / 16.0, 0.0)
    tri_up = np.where(same & (s > c), -1.0 / 16.0, 0.0)
    m01 = np.where(same & (s <= c), 1.0, 0.0)
    return np.stack([tri_incl, tri_up, m01]).astype(np.float32)


def stage_gla(kb, C, hF_d, hT_d, wa2_d, ba_d, bog_d, nw_d, gc_d, mix_d):
    H, DK, DV = GLA_H, GLA_DK, GLA_DV
    with contextlib.ExitStack() as st:
        gc = kb.sb(st, [128, 3, 128], F32, "gc")
        kb.dma("sp", gc[:], gc_d.rearrange("a s c -> s a c"), w=[gc])
        wa2 = kb.sb(st, [16, 384], F32, "wa2")
        kb.dma("sp", wa2[:], wa2_d, w=[wa2])
        bab = kb.sb(st, [128, 384], F32, "bab")
        kb.dma("sp", bab[:], ba_d.partition_broadcast(128), w=[bab])
        bogb = kb.sb(st, [128, TOKW], F32, "bogb")
        kb.dma("sp", bogb[:], bog_d.partition_broadcast(128), w=[bogb])
        nwb = kb.sb(st, [128, DV], F32, "nwb")
        kb.dma("sp", nwb[:], nw_d.partition_broadcast(128), w=[nwb])
        epsb = kb.sb(st, [128, 1], F32, "eps")
        kb.op("dve", lambda E: E.memset(epsb[:], LN_EPS), w=[epsb])
        Sf = [kb.sb(st, [DK, DV], F32, "Sf") for _ in range(H)]
        Sb = [[kb.sb(st, [DK, DV], BF16, "Sb") for _ in range(2)] for _ in range(H)]
        for h in range(H):
            kb.op("dve", lambda E, h=h: E.memset(Sf[h][:], 0.0), w=[Sf[h]])
            kb.op("dve", lambda E, h=h: E.memset(Sb[h][1][:], 0.0), w=[Sb[h][1]])
        NB = 2
        tm = [kb.sb(st, [128, 1936], F32, "tm") for _ in range(NB)]
        qkT = [kb.sb(st, [DK, 2, H, 128], F32, "qkT") for _ in range(NB)]
        alrT = [kb.sb(st, [16, 128], F32, "alrT") for _ in range(NB)]
        G = [kb.sb(st, [128, 384], F32, "G") for _ in range(NB)]
        vb = [kb.sb(st, [128, TOKW], BF16, "vb") for _ in range(NB)]
        sg = [kb.sb(st, [128, TOKW], F32, "sg") for _ in range(NB)]
        kdec = [kb.sb(st, [128, 384], BF16, "kdec") for _ in range(NB)]
        edec = [kb.sb(st, [128, 384], F32, "edec") for _ in range(NB)]
        yt = [kb.sb(st, [128, TOKW], F32, "yt") for _ in range(NB)]
        eb = [kb.sb(st, [DK, 128], F32, "eb") for _ in range(4)]
        enb = [kb.sb(st, [DK, 128], F32, "enb") for _ in range(4)]
        qd = [kb.sb(st, [DK, 128], BF16, "qd") for _ in range(4)]
        kd = [kb.sb(st, [DK, 128], BF16, "kd") for _ in range(4)]
        qd0 = [kb.sb(st, [DK, 128], BF16, "qd0") for _ in range(4)]
        qd1 = [kb.sb(st, [DK, 128], BF16, "qd1") for _ in range(4)]
        for i in range(4):
            kb.op("dve", lambda E, i=i: E.memset(qd0[i][:], 0.0), w=[qd0[i]])
            kb.op("dve", lambda E, i=i: E.memset(qd1[i][:], 0.0), w=[qd1[i]])
        AT = [kb.sb(st, [128, 128], BF16, "AT") for _ in range(2)]
        ebl = [kb.sb(st, [DK, 2], F32, "ebl") for _ in range(4)]
        ssq = [kb.sb(st, [128, 1], F32, "ssq") for _ in range(2)]
        rstd = [kb.sb(st, [128, 1], F32, "rstd") for _ in range(2)]
        junk = kb.sb(st, [128, DV], F32, "junk")
        otmp = [kb.sb(st, [128, DV], F32, "otmp") for _ in range(2)]
        pz = kb.ps(st, [128, 512], F32, "pz")
        pdec = kb.ps(st, [128, 512], F32, "pdec")
        pbc = [kb.ps(st, [128, 512], F32, "pbc") for _ in range(2)]
        pA = kb.ps(st, [128, 512], F32, "pA")
        po = [kb.ps(st, [128, 512], F32, "po") for _ in range(2)]
        pds = kb.ps(st, [128, 512], F32, "pds")
        hc = 0
        for tt in range(S // 128):
            b = tt % NB
            t0 = tt * 128
            kb.dma("sp", tm[b][:], hT_d[t0:t0 + 128, 384:2320], w=[tm[b]])
            kb.dma("pool", qkT[b][:], hF_d[0:768, t0:t0 + 128].rearrange("(a h d) t -> d a h t", a=2, h=H), w=[qkT[b]])
            kb.dma("pool", alrT[b][:], hF_d[1536:1552, t0:t0 + 128], w=[alrT[b]])
            k_tm = tm[b][:, 0:384]
            v_tm = tm[b][:, 384:1152]
            og_tm = tm[b][:, 1168:1936]
            kb.op("pe", lambda E, b=b: E.matmul(out=pz[:, 0:384], lhsT=alrT[b][:], rhs=wa2[:], start=True, stop=True), r=[alrT[b], wa2], w=[pz])
            kb.op("dve", lambda E, b=b: E.tensor_tensor(out=G[b][:], in0=pz[:, 0:384], in1=bab[:], op=ALU.add), r=[pz, bab], w=[G[b]])
            kb.op("act", lambda E, b=b: E.activation(out=G[b][:], in_=G[b][:], func=AF.Exp, scale=-1.0), r=[G[b]], w=[G[b]])
            kb.op("act", lambda E, b=b: E.activation(out=G[b][:], in_=G[b][:], func=AF.Ln, bias=1.0, scale=1.0), r=[G[b]], w=[G[b]])
            kb.op("act", lambda E, b=b: E.copy(out=vb[b][:], in_=tm[b][:, 384:1152]), r=[tm[b]], w=[vb[b]])
            kb.op("dve", lambda E, b=b: E.tensor_tensor(out=sg[b][:], in0=tm[b][:, 1168:1936], in1=bogb[:], op=ALU.add), r=[tm[b], bogb], w=[sg[b]])
            kb.op("act", lambda E, b=b: E.activation(out=sg[b][:], in_=sg[b][:], func=AF.Silu), r=[sg[b]], w=[sg[b]])
            kb.op("pe", lambda E, b=b: E.matmul(out=pdec[:, 0:384], lhsT=gc[:, 1, :], rhs=G[b][:], start=True, stop=True), r=[gc, G[b]], w=[pdec])
            kb.op("act", lambda E, b=b: E.activation(out=edec[b][:], in_=pdec[:, 0:384], func=AF.Exp), r=[pdec], w=[edec[b]])
            kb.op("dve", lambda E, b=b: E.tensor_tensor(out=kdec[b][:], in0=tm[b][:, 0:384], in1=edec[b][:], op=ALU.mult), r=[tm[b], edec[b]], w=[kdec[b]])
            for h in range(H):
                i = hc % 4
                i2 = hc % 2
                hc += 1
                pb_ = pbc[i2]
                kb.op("pe", lambda E, b=b, h=h, pb_=pb_: E.matmul(out=pb_[0:DK, 0:128], lhsT=G[b][:, h * DK:(h + 1) * DK], rhs=gc[:, 0, :], start=True, stop=True), r=[G[b], gc], w=[pb_])
                kb.op("act", lambda E, i=i, pb_=pb_: E.activation(out=eb[i][:], in_=pb_[0:DK, 0:128], func=AF.Exp), r=[pb_], w=[eb[i]])
                kb.op("act", lambda E, i=i, pb_=pb_: E.activation(out=enb[i][:], in_=pb_[0:DK, 0:128], func=AF.Exp, scale=-1.0), r=[pb_], w=[enb[i]])
                kb.op("dve", lambda E, i=i, b=b, h=h: E.scalar_tensor_tensor(out=qd[i][:], in0=qkT[b][:, 0, h, :], scalar=float(DK) ** -0.5, in1=eb[i][:], op0=ALU.mult, op1=ALU.mult), r=[qkT[b], eb[i]], w=[qd[i]])
                kb.op("dve", lambda E, i=i, b=b, h=h: E.tensor_tensor(out=kd[i][:], in0=qkT[b][:, 1, h, :], in1=enb[i][:], op=ALU.mult), r=[qkT[b], enb[i]], w=[kd[i]])
                kb.op("act", lambda E, i=i: E.copy(out=qd0[i][:, 0:64], in_=qd[i][:, 0:64]), r=[qd[i]], w=[qd0[i]])
                kb.op("act", lambda E, i=i: E.copy(out=qd1[i][:, 64:128], in_=qd[i][:, 64:128]), r=[qd[i]], w=[qd1[i]])
                kb.op("act", lambda E, i=i: E.copy(out=ebl[i][:, 0:1], in_=eb[i][:, 63:64]), r=[eb[i]], w=[ebl[i]])
                kb.op("act", lambda E, i=i: E.copy(out=ebl[i][:, 1:2], in_=eb[i][:, 127:128]), r=[eb[i]], w=[ebl[i]])
                kb.op("pe", lambda E, i=i: E.matmul(out=pA[:, 0:128], lhsT=kd[i][:], rhs=qd[i][:], start=True, stop=True), r=[kd[i], qd[i]], w=[pA])
                at = AT[i2]
                kb.op("dve", lambda E, at=at: E.tensor_tensor(out=at[:], in0=pA[:, 0:128], in1=gc[:, 2, :], op=ALU.mult), r=[pA, gc], w=[at])
                o = po[i2]
                sb_in = Sb[h][1]
                sb_mid = Sb[h][0]
                kb.op("pe", lambda E, o=o, at=at, b=b, h=h: E.matmul(out=o[:, 0:DV], lhsT=at[:], rhs=vb[b][:, h * DV:(h + 1) * DV], start=True, stop=False), r=[at, vb[b]], w=[o])
                kb.op("pe", lambda E, o=o, i=i, sb_in=sb_in: E.matmul(out=o[:, 0:DV], lhsT=qd0[i][:], rhs=sb_in[:], start=False, stop=False), r=[qd0[i], sb_in], w=[o])
                for j in range(2):
                    kb.op("pe", lambda E, b=b, h=h, j=j: E.matmul(out=pds[0:DK, 0:DV], lhsT=kdec[b][j * 64:(j + 1) * 64, h * DK:(h + 1) * DK], rhs=vb[b][j * 64:(j + 1) * 64, h * DV:(h + 1) * DV], start=True, stop=True), r=[kdec[b], vb[b]], w=[pds])
                    kb.op("dve", lambda E, h=h, i=i, j=j: E.scalar_tensor_tensor(out=Sf[h][:], in0=Sf[h][:], scalar=ebl[i][:, j:j + 1], in1=pds[0:DK, 0:DV], op0=ALU.mult, op1=ALU.add), r=[Sf[h], ebl[i], pds], w=[Sf[h]])
                    dst = sb_mid if j == 0 else sb_in
                    kb.op("act", lambda E, h=h, dst=dst: E.copy(out=dst[:], in_=Sf[h][:]), r=[Sf[h]], w=[dst])
                    if j == 0:
                        kb.op("pe", lambda E, o=o, i=i, sb_mid=sb_mid: E.matmul(out=o[:, 0:DV], lhsT=qd1[i][:], rhs=sb_mid[:], start=False, stop=True), r=[qd1[i], sb_mid], w=[o])
                sq = ssq[i2]
                rs = rstd[i2]
                ot = otmp[i2]
                kb.op("act", lambda E, o=o, sq=sq: E.activation(out=junk[:], in_=o[:, 0:DV], func=AF.Square, accum_out=sq[:]), r=[o], w=[junk, sq])
                kb.op("act", lambda E, sq=sq, rs=rs: E.activation(out=rs[:], in_=sq[:], func=AF.Sqrt, bias=epsb[:], scale=1.0 / DV), r=[sq, epsb], w=[rs])
                kb.op("dve", lambda E, rs=rs: E.reciprocal(out=rs[:], in_=rs[:]), r=[rs], w=[rs])
                kb.op("dve", lambda E, o=o, rs=rs, ot=ot: E.scalar_tensor_tensor(out=ot[:], in0=o[:, 0:DV], scalar=rs[:, 0:1], in1=nwb[:], op0=ALU.mult, op1=ALU.mult), r=[o, rs, nwb], w=[ot])
                kb.op("dve", lambda E, ot=ot, b=b, h=h: E.tensor_tensor(out=yt[b][:, h * DV:(h + 1) * DV], in0=ot[:], in1=sg[b][:, h * DV:(h + 1) * DV], op=ALU.mult), r=[ot, sg[b]], w=[yt[b]])
            kb.dma("sp", mix_d[t0:t0 + 128, 0:TOKW], yt[b][:], r=[yt[b]])
        kb.barrier()


def TT(kb, out, in0, in1, op, r, w, eng="dve"):
    return kb.op(eng, lambda E: E.tensor_tensor(out=out, in0=in0, in1=in1, op=op), r=r, w=w)


def TS(kb, out, in0, s1, s2, op0, op1, r, w, eng="dve"):
    if op1 is None:
        return kb.op(eng, lambda E: E.tensor_scalar(out=out, in0=in0, scalar1=s1, scalar2=None, op0=op0), r=r, w=w)
    return kb.op(eng, lambda E: E.tensor_scalar(out=out, in0=in0, scalar1=s1, scalar2=s2, op0=op0, op1=op1), r=r, w=w)


def STT(kb, out, in0, scalar, in1, op0, op1, r, w):
    return kb.op("dve", lambda E: E.scalar_tensor_tensor(out=out, in0=in0, scalar=scalar, in1=in1, op0=op0, op1=op1), r=r, w=w)


def ACT(kb, out, in_, func, r, w, bias=None, scale=None, accum_out=None):
    kw = {}
    if bias is not None:
        kw["bias"] = bias
    if scale is not None:
        kw["scale"] = scale
    if accum_out is not None:
        kw["accum_out"] = accum_out
    return kb.op("act", lambda E: E.activation(out=out, in_=in_, func=func, **kw), r=r, w=w)


def MM(kb, out, lhsT, rhs, start, stop, r, w):
    return kb.op("pe", lambda E: E.matmul(out=out, lhsT=lhsT, rhs=rhs, start=start, stop=stop), r=r, w=w)


def TR(kb, out, in_, ident, r, w):
    return kb.op("pe", lambda E: E.transpose(out=out, in_=in_, identity=ident), r=r, w=w)


def CP(kb, out, in_, r, w, eng="dve"):
    if eng == "act":
        return kb.op("act", lambda E: E.copy(out=out, in_=in_), r=r, w=w)
    return kb.op("dve", lambda E: E.tensor_copy(out=out, in_=in_), r=r, w=w)


def RED(kb, out, in_, op, r, w):
    return kb.op("dve", lambda E: E.tensor_reduce(out=out, in_=in_, axis=AX.X, op=op), r=r, w=w)


def MSET(kb, ap, val, w):
    return kb.op("dve", lambda E: E.memset(ap, val), w=w)


def bcast_row(kb, st, d_ap, n, name):
    t = kb.sb(st, [128, n], F32, name)
    kb.dma("sp", t[:], d_ap.partition_broadcast(128), w=[t])
    return t


RW_H = 12


def perm_view(ap):
    return ap.rearrange("t (hp j k) -> t hp j k", hp=2, j=6)


def nat_view(ap):
    return ap.rearrange("t (j hp k) -> t hp j k", hp=2, j=6)


def store_perm(kb, q, dram_rows, tile_ap, r):
    for hp in range(2):
        kb.dma(q, dram_rows[:, hp * 384:(hp + 1) * 384].rearrange("t (j k) -> t j k", k=64), nat_view(tile_ap)[:, hp], r=r)


def load_perm(kb, q, tile, dram_rows):
    for hp in range(2):
        kb.dma(q, nat_view(tile[:])[:, hp], dram_rows[:, hp * 384:(hp + 1) * 384].rearrange("t (j k) -> t j k", k=64), w=[(tile, hp)])


def stage_rwkv(kb, C, hT_d, P, mix_d, scr):
    Wd, Kd, Ad, Bd, Rd = scr["W"], scr["K"], scr["A"], scr["B"], scr["R"]
    Vd, Gd, VPd, Od = scr["V"], scr["G"], scr["VP"], scr["O"]
    with contextlib.ExitStack() as st:
        mub = bcast_row(kb, st, P["mu"], 2560, "mub")
        w0b = bcast_row(kb, st, P["w0"], TOKW, "w0b")
        a0b = bcast_row(kb, st, P["a0"], TOKW, "a0b")
        kkb = bcast_row(kb, st, P["k_k"], TOKW, "kkb")
        kab = bcast_row(kb, st, P["k_a"], TOKW, "kab")
        w2b = kb.sb(st, [64, TOKW], BF16, "w2b")
        a2b = kb.sb(st, [64, TOKW], BF16, "a2b")
        g2b = kb.sb(st, [128, TOKW], BF16, "g2b")
        kb.dma("pool", w2b[:], P["w2"], w=[w2b])
        kb.dma("pool", a2b[:], P["a2"], w=[a2b])
        kb.dma("pool", g2b[:], P["g2"], w=[g2b])
        NB = 2
        hc = [kb.sb(st, [128, 2560], F32, "hc") for _ in range(NB)]
        hp_ = [kb.sb(st, [128, 2560], F32, "hp") for _ in range(NB)]
        twT = [kb.sb(st, [64, 128], BF16, "twT") for _ in range(NB)]
        alT = [kb.sb(st, [64, 128], BF16, "alT") for _ in range(NB)]
        sgT = [kb.sb(st, [128, 128], BF16, "sgT") for _ in range(NB)]
        Wt = [kb.sb(st, [128, TOKW], F32, "Wt") for _ in range(NB)]
        At = [kb.sb(st, [128, TOKW], F32, "At") for _ in range(NB)]
        Bt = [kb.sb(st, [128, TOKW], F32, "Bt") for _ in range(NB)]
        Kt = [kb.sb(st, [128, TOKW], F32, "Kt") for _ in range(NB)]
        Gt = [kb.sb(st, [128, TOKW], F32, "Gt") for _ in range(NB)]
        aa = [kb.sb(st, [128, TOKW], F32, "aa") for _ in range(NB)]
        sq = [kb.sb(st, [128, TOKW], F32, "sq") for _ in range(NB)]
        ss = [kb.sb(st, [128, RW_H], F32, "ss") for _ in range(NB)]
        vps = [kb.sb(st, [128, 6, 128], F32, "vps") for _ in range(NB)]
        ptr = kb.ps(st, [128, 512], F32, "ptr")
        pw = [kb.ps(st, [128, 512], F32, "pw") for _ in range(2)]
        pa = [kb.ps(st, [128, 512], F32, "pa") for _ in range(2)]
        pgp = [kb.ps(st, [128, 512], F32, "pgp") for _ in range(2)]
        pvp = kb.ps(st, [128, 512], F32, "pvp")
        for b in range(NB):
            MSET(kb, hp_[b][0:1, :], 0.0, w=[hp_[b]])
        for tt in range(S // 128):
            b = tt % NB
            t0 = tt * 128
            H_, HP = hc[b], hp_[b]
            kb.dma("sp", H_[:], hT_d[t0:t0 + 128, 0:2560], w=[H_])
            if tt == 0:
                kb.dma("pool", HP[1:128, :], hT_d[0:127, 0:2560], w=[HP])
            else:
                kb.dma("pool", HP[:], hT_d[t0 - 1:t0 + 127, 0:2560], w=[HP])
            TT(kb, HP[:], HP[:], H_[:], ALU.subtract, r=[HP, H_], w=[HP])
            TT(kb, HP[:], HP[:], mub[:], ALU.mult, r=[HP, mub], w=[HP])
            TT(kb, H_[:], H_[:], HP[:], ALU.add, r=[H_, HP], w=[H_])
            r_ = H_[:, 0:768]
            k_ = H_[:, 832:1600]
            v_ = H_[:, 1600:2368]
            TR(kb, ptr[0:64, 0:128], H_[:, 768:832], C.ident_f[:], r=[H_, C.ident_f], w=[ptr])
            TR(kb, ptr[0:64, 128:256], H_[:, 2368:2432], C.ident_f[:], r=[H_, C.ident_f], w=[ptr])
            TR(kb, ptr[:, 256:384], H_[:, 2432:2560], C.ident_f[:], r=[H_, C.ident_f], w=[ptr])
            ACT(kb, twT[b][:], ptr[0:64, 0:128], AF.Tanh, r=[ptr], w=[twT[b]])
            CP(kb, alT[b][:], ptr[0:64, 128:256], r=[ptr], w=[alT[b]])
            ACT(kb, sgT[b][:], ptr[:, 256:384], AF.Sigmoid, r=[ptr], w=[sgT[b]])
            for (lh, rh, pp) in ((twT[b], w2b, pw), (alT[b], a2b, pa), (sgT[b], g2b, pgp)):
                MM(kb, pp[0][:, 0:512], lh[:], rh[:, 0:512], True, True, r=[lh, rh], w=[pp[0]])
                MM(kb, pp[1][:, 0:256], lh[:], rh[:, 512:768], True, True, r=[lh, rh], w=[pp[1]])
            W_, A_, B_, K_, G_, a_, q_ = Wt[b], At[b], Bt[b], Kt[b], Gt[b], aa[b], sq[b]
            for (c0, c1, hh) in ((0, 512, 0), (512, 768, 1)):
                n = c1 - c0
                TT(kb, W_[:, c0:c1], pw[hh][:, 0:n], w0b[:, c0:c1], ALU.add, r=[pw[hh], w0b], w=[W_])
                TT(kb, a_[:, c0:c1], pa[hh][:, 0:n], a0b[:, c0:c1], ALU.add, r=[pa[hh], a0b], w=[a_])
                CP(kb, G_[:, c0:c1], pgp[hh][:, 0:n], r=[pgp[hh]], w=[G_], eng="act")
            ACT(kb, W_[:], W_[:], AF.Sigmoid, r=[W_], w=[W_])
            ACT(kb, W_[:], W_[:], AF.Exp, r=[W_], w=[W_], scale=-float(np.exp(-0.5)))
            ACT(kb, a_[:], a_[:], AF.Sigmoid, r=[a_], w=[a_])
            TT(kb, A_[:], k_, kkb[:], ALU.mult, r=[H_, kkb], w=[A_])
            TT(kb, q_[:], A_[:], A_[:], ALU.mult, r=[A_], w=[q_])
            RED(kb, ss[b][:], q_[:].rearrange("p (h k) -> p h k", k=64), ALU.add, r=[q_], w=[ss[b]])
            ACT(kb, ss[b][:], ss[b][:], AF.Sqrt, r=[ss[b]], w=[ss[b]])
            TS(kb, ss[b][:], ss[b][:], 1e-12, None, ALU.max, None, r=[ss[b]], w=[ss[b]])
            kb.op("dve", lambda E, s_=ss[b]: E.reciprocal(out=s_[:], in_=s_[:]), r=[ss[b]], w=[ss[b]])
            TT(kb, A_[:].rearrange("p (h k) -> p h k", k=64), A_[:].rearrange("p (h k) -> p h k", k=64), ss[b][:, :].unsqueeze(2).broadcast_to([128, RW_H, 64]), ALU.mult, r=[A_, ss[b]], w=[A_])
            TT(kb, B_[:], A_[:], a_[:], ALU.mult, r=[A_, a_], w=[B_])
            TS(kb, A_[:], A_[:], -1.0, None, ALU.mult, None, r=[A_], w=[A_])
            STT(kb, q_[:], a_[:], -1.0, kab[:], ALU.add, ALU.mult, r=[a_, kab], w=[q_])
            STT(kb, K_[:], q_[:], 1.0, k_, ALU.add, ALU.mult, r=[q_, H_], w=[K_])
            for j in range(6):
                TR(kb, pvp[:, 0:128] if j % 2 == 0 else pvp[:, 128:256], H_[:, 1600 + j * 128:1600 + (j + 1) * 128], C.ident_f[:], r=[H_, C.ident_f], w=[pvp])
                CP(kb, vps[b][:, j, :], pvp[:, 0:128] if j % 2 == 0 else pvp[:, 128:256], r=[pvp], w=[vps[b]], eng="act" if j % 2 else "dve")
            rows = slice(t0, t0 + 128)
            store_perm(kb, "sp", Wd[rows, :], W_[:], r=[W_])
            store_perm(kb, "pool", Kd[rows, :], K_[:], r=[K_])
            store_perm(kb, "sp", Ad[rows, :], A_[:], r=[A_])
            store_perm(kb, "pool", Bd[rows, :], B_[:], r=[B_])
            store_perm(kb, "sp", Rd[rows, :], H_[:, 0:768], r=[H_])
            kb.dma("pool", Vd[rows, :], H_[:, 1600:2368], r=[H_])
            kb.dma("sp", Gd[rows, :], G_[:], r=[G_])
            kb.dma("pool", VPd[:, :, t0:t0 + 128], vps[b][:], r=[vps[b]])
        kb.barrier()

    with contextlib.ExitStack() as st:
        T = 8
        Sst = kb.sb(st, [128, 6, 64], F32, "Sst")
        SK = [("S", j) for j in range(6)]
        MSET(kb, Sst[:], 0.0, w=SK)
        BC = [kb.sb(st, [128, T, 5, 384], F32, "BC") for _ in range(2)]
        vP = [kb.sb(st, [128, 6, 512], F32, "vP") for _ in range(2)]
        oP = [kb.sb(st, [128, 6, 512], F32, "oP") for _ in range(2)]
        tmp = [kb.sb(st, [128, 6, 64], F32, "tmp") for _ in range(2)]
        sa = [kb.sb(st, [128, 6], F32, "sa") for _ in range(2)]
        otile = [kb.sb(st, [128, TOKW], F32, "otile") for _ in range(2)]
        pto = [kb.ps(st, [128, 512], F32, "pto") for _ in range(2)]
        srcs = (Ad, Wd, Bd, Kd, Rd)
        qs = ("sp", "act", "pool")
        for blk in range(S // T):
            bb = blk % 2
            t0 = blk * T
            g = (t0 // 512) % 2
            if t0 % 512 == 0:
                kb.dma("sp", vP[g][:], VPd[:, :, t0:t0 + 512], w=[vP[g]])
            for vi in range(5):
                for hp in range(2):
                    kb.dma(qs[(vi * 2 + hp) % 3], BC[bb][hp * 64:(hp + 1) * 64, :, vi, :], srcs[vi][t0:t0 + T, hp * 384:(hp + 1) * 384].partition_broadcast(64), w=[("BC", bb, vi, hp)])
            for tl in range(T):
                tg = (t0 + tl) % 512
                ti = tl % 2

                def bc(vi, bb=bb, tl=tl):
                    return BC[bb][:, tl, vi, :].rearrange("p (j k) -> p j k", k=64)

                def bk(vi, bb=bb):
                    return [("BC", bb, vi, 0), ("BC", bb, vi, 1)]

                TT(kb, tmp[ti][:], Sst[:], bc(0), ALU.mult, r=SK + bk(0), w=[tmp[ti]])
                RED(kb, sa[ti][:], tmp[ti][:], ALU.add, r=[tmp[ti]], w=[sa[ti]])
                TT(kb, Sst[:], Sst[:], bc(1), ALU.mult, r=SK + bk(1), w=SK)
                Bv, Kv = bc(2), bc(3)
                for j in range(6):
                    STT(kb, Sst[:, j, :], Bv[:, j, :], sa[ti][:, j:j + 1], Sst[:, j, :], ALU.mult, ALU.add, r=[sa[ti], SK[j]] + bk(2), w=[SK[j]])
                for j in range(6):
                    STT(kb, Sst[:, j, :], Kv[:, j, :], vP[g][:, j, tg:tg + 1], Sst[:, j, :], ALU.mult, ALU.add, r=[vP[g], SK[j]] + bk(3), w=[SK[j]])
                TT(kb, tmp[ti][:], Sst[:], bc(4), ALU.mult, r=SK + bk(4), w=[tmp[ti]])
                RED(kb, oP[g][:, :, tg], tmp[ti][:], ALU.add, r=[tmp[ti]], w=[oP[g]])
            if (t0 + T) % 512 == 0:
                G0 = t0 + T - 512
                for sub in range(4):
                    ot = otile[sub % 2]
                    for j in range(6):
                        p = pto[j % 2]
                        TR(kb, p[:, 0:128], oP[g][:, j, sub * 128:(sub + 1) * 128], C.ident_f[:], r=[oP[g], C.ident_f], w=[p])
                        CP(kb, ot[:, j * 128:(j + 1) * 128], p[:, 0:128], r=[p], w=[ot], eng="act")
                    kb.dma("sp", Od[G0 + sub * 128:G0 + (sub + 1) * 128, :], ot[:], r=[ot])
        kb.barrier()
    with contextlib.ExitStack() as st:
        lnw = bcast_row(kb, st, P["ln_w"], TOKW, "lnw")
        lnb = bcast_row(kb, st, P["ln_b"], TOKW, "lnb")
        rkb = bcast_row(kb, st, P["r_k"], TOKW, "rkb")
        NB = 2
        o_ = [kb.sb(st, [128, TOKW], F32, "o") for _ in range(NB)]
        r_ = [kb.sb(st, [128, TOKW], F32, "r") for _ in range(NB)]
        k_ = [kb.sb(st, [128, TOKW], F32, "k") for _ in range(NB)]
        v_ = [kb.sb(st, [128, TOKW], F32, "v") for _ in range(NB)]
        g_ = [kb.sb(st, [128, TOKW], F32, "g") for _ in range(NB)]
        q_ = [kb.sb(st, [128, TOKW], F32, "q") for _ in range(NB)]
        s1 = [kb.sb(st, [128, RW_H], F32, "s1") for _ in range(NB)]
        s2 = [kb.sb(st, [128, RW_H], F32, "s2") for _ in range(NB)]
        s3 = [kb.sb(st, [128, RW_H], F32, "s3") for _ in range(NB)]

        def h3(ap):
            return ap.rearrange("p (h k) -> p h k", k=64)

        def b3(ap):
            return ap.unsqueeze(2).broadcast_to([128, RW_H, 64])

        for tt in range(S // 128):
            b = tt % NB
            rows = slice(tt * 128, (tt + 1) * 128)
            O, R_, K_, V_, G_, Q_ = o_[b], r_[b], k_[b], v_[b], g_[b], q_[b]
            kb.dma("sp", O[:], Od[rows, :], w=[O])
            load_perm(kb, "pool", R_, Rd[rows, :])
            load_perm(kb, "pool", K_, Kd[rows, :])
            kb.dma("sp", V_[:], Vd[rows, :], w=[V_])
            kb.dma("sp", G_[:], Gd[rows, :], w=[G_])
            RED(kb, s1[b][:], h3(O[:]), ALU.add, r=[O], w=[s1[b]])
            TT(kb, Q_[:], O[:], O[:], ALU.mult, r=[O], w=[Q_])
            RED(kb, s2[b][:], h3(Q_[:]), ALU.add, r=[Q_], w=[s2[b]])
            TS(kb, s1[b][:], s1[b][:], 1.0 / 64, None, ALU.mult, None, r=[s1[b]], w=[s1[b]])
            STT(kb, s3[b][:], s1[b][:], -1.0, s1[b][:], ALU.mult, ALU.mult, r=[s1[b]], w=[s3[b]])
            STT(kb, s2[b][:], s2[b][:], 1.0 / 64, s3[b][:], ALU.mult, ALU.add, r=[s2[b], s3[b]], w=[s2[b]])
            TS(kb, s2[b][:], s2[b][:], 64e-5, None, ALU.add, None, r=[s2[b]], w=[s2[b]])
            ACT(kb, s2[b][:], s2[b][:], AF.Sqrt, r=[s2[b]], w=[s2[b]])
            kb.op("dve", lambda E, x=s2[b]: E.reciprocal(out=x[:], in_=x[:]), r=[s2[b]], w=[s2[b]])
            TT(kb, h3(O[:]), h3(O[:]), b3(s1[b][:, :]), ALU.subtract, r=[O, s1[b]], w=[O])
            TT(kb, h3(O[:]), h3(O[:]), b3(s2[b][:, :]), ALU.mult, r=[O, s2[b]], w=[O])
            TT(kb, O[:], O[:], lnw[:], ALU.mult, r=[O, lnw], w=[O])
            TT(kb, O[:], O[:], lnb[:], ALU.add, r=[O, lnb], w=[O])
            TT(kb, Q_[:], R_[:], K_[:], ALU.mult, r=[(R_, 0), (R_, 1), (K_, 0), (K_, 1), Q_], w=[Q_])
            TT(kb, Q_[:], Q_[:], rkb[:], ALU.mult, r=[Q_, rkb], w=[Q_])
            RED(kb, s3[b][:], h3(Q_[:]), ALU.add, r=[Q_], w=[s3[b]])
            TT(kb, h3(Q_[:]), h3(V_[:]), b3(s3[b][:, :]), ALU.mult, r=[V_, s3[b]], w=[Q_])
            TT(kb, O[:], O[:], Q_[:], ALU.add, r=[O, Q_], w=[O])
            TT(kb, O[:], O[:], G_[:], ALU.mult, r=[O, G_], w=[O])
            kb.dma("sp", mix_d[rows, 0:TOKW], O[:], r=[O])
        kb.barrier()


NSA_BIG = 1.0e4
NEG = -1.0e30


def _t5_bucket_np(dist):
    d = np.maximum(dist, 0)
    scaled = (np.log(np.maximum(d, 1).astype(np.float32) / np.float32(16)) / np.float32(np.log(128 / 16))).astype(np.float32)
    large = np.minimum(16 + (scaled * np.float32(16)).astype(np.int32), 31)
    return np.where(d < 16, d, large)


def nsa_tables_np(rel_table):
    kk = np.arange(128)[:, None]
    qq = np.arange(512)[None, :]
    out = np.empty((12, 18, 128, 512), np.float32)
    for o in range(5):
        dist = qq - kk - 128 * (o - 1)
        bk = _t5_bucket_np(dist)
        for h in range(12):
            out[h, o] = np.where(dist >= 0, rel_table[bk, h], NEG)
    for o in range(8):
        dist = qq - kk - 128 * (o - 4)
        bk = _t5_bucket_np(dist)
        for h in range(12):
            out[h, 5 + o] = np.where((dist >= 0) & (dist < 512), rel_table[bk, h], NEG)
    for o in range(5):
        dist = 512 * o + qq - 16 * kk - 31
        bk = _t5_bucket_np(dist)
        for h in range(12):
            out[h, 13 + o] = np.where(dist >= 0, rel_table[bk, h], NEG)
    return out.reshape(216, 128 * 512)


def nsa_static_np():
    q = np.arange(S)[:, None]
    m = np.arange(64)[None, :]
    cur = q // 64
    causal = m <= cur
    forced = (m == 0) | (m == cur) | (m == cur - 1)
    A = np.where(causal & ~forced, 1.0, 0.0).astype(np.float32)
    B = np.where(causal & forced, NSA_BIG, np.where(causal, 0.0, -1.0)).astype(np.float32)
    AB = np.stack([A, B]).reshape(2, 32, 128, 64).transpose(2, 0, 1, 3).copy()
    n = np.arange(256)[:, None]
    cs, ce = n * 16, n * 16 + 31
    c2s = ((cs < m * 64 + 64) & (ce >= m * 64) & (n < 255)).astype(np.float32)
    c2s = c2s.reshape(2, 128, 64).transpose(1, 0, 2).copy()
    E = np.zeros((64, 32, 128), np.float32)
    for j in range(32):
        for k in range(128):
            E[2 * j + k // 64, j, k] = NSA_BIG
    return AB, c2s, E


def stage_nsa(kb, C, hF_d, hT_d, P, tab_d, mix_d):
    QC, KCC, VCC, KSC, VSC, KWC, VWC, GLC = 0, 768, 1024, 1280, 1536, 1792, 2048, 2304
    with contextlib.ExitStack() as st:
        KC = [kb.sb(st, [64, 256], BF16, "KC") for _ in range(4)]
        VCx = [kb.sb(st, [128, 2, 129], BF16, "VCx") for _ in range(4)]
        c2s = kb.sb(st, [128, 2, 64], F32, "c2s")
        kb.dma("sp", c2s[:], P["c2s"], w=[c2s])
        CH = bcast_row(kb, st, P["rel31"], 12, "CH")
        gbb = bcast_row(kb, st, P["gate_b"], 36, "gbb")
        GATE = kb.sb(st, [128, 32, 36], F32, "GATE")
        kb.dma("sp", GATE[:], hT_d[:, GLC:GLC + 36].rearrange("(t p) c -> p t c", p=128), w=[GATE])
        TT(kb, GATE[:], GATE[:], gbb[:, :].unsqueeze(1).broadcast_to([128, 32, 36]), ALU.add, r=[GATE, gbb], w=[GATE])
        ACT(kb, GATE[:], GATE[:], AF.Sigmoid, r=[GATE], w=[GATE])
        import os as _os
        _stop = int(_os.environ.get("NSA_STOP", "99"))
        if _stop == -1:
            kb.barrier()
            return
        with contextlib.ExitStack() as s0:
            w1b = [kb.sb(s0, [64, 32, 256], BF16, "w1b") for _ in range(2)]
            w2b = [kb.sb(s0, [128, 2, 64], BF16, "w2b") for _ in range(2)]
            peT = [kb.sb(s0, [64, 32, 2], BF16, "peT") for _ in range(2)]
            cv = [kb.sb(s0, [128, 2], F32, "cv") for _ in range(2)]
            praw = kb.sb(s0, [32, 2, 64], F32, "praw")
            kb.dma("sp", praw[:], P["pe"].rearrange("a l d -> l a d"), w=[praw])
            pp = kb.ps(s0, [128, 512], F32, "pp")
            ph = [kb.ps(s0, [128, 512], F32, "ph") for _ in range(2)]
            pk = kb.ps(s0, [128, 512], F32, "pk")
            pv = [kb.ps(s0, [128, 512], F32, "pv") for _ in range(2)]
            pcv = [pp, ph[0], ph[1], pk]
            for a in range(2):
                kb.dma("pool", w1b[a][:], P["w1"][a].rearrange("(l d) j -> d l j", d=64), w=[w1b[a]])
                kb.dma("pool", w2b[a][:], P["w2"][a].rearrange("(c j) d -> j c d", j=128), w=[w2b[a]])
                TR(kb, pp[0:64, a * 32:(a + 1) * 32], praw[:, a, :], C.ident_f[0:32, 0:32], r=[praw, C.ident_f], w=[pp])
                CP(kb, peT[a][:, :, 0], pp[0:64, a * 32:(a + 1) * 32], r=[pp], w=[peT[a]])
                CP(kb, peT[a][:, :, 1], pp[0:64, a * 32:(a + 1) * 32], r=[pp], w=[peT[a]])
            _c0 = int(_os.environ.get("NSA_C0", "99"))
            if _c0 == 1:
                kb.barrier()
                return
            for a in range(2):
                for jc in range(2):
                    for l in range(32):
                        MM(kb, pcv[a * 2 + jc][:, 0:2], w1b[a][:, l, jc * 128:(jc + 1) * 128], peT[a][:, l, :], l == 0, l == 31, r=[w1b[a], peT[a]], w=[pcv[a * 2 + jc]])
                for jc in range(2):
                    CP(kb, cv[a][:, jc:jc + 1], pcv[a * 2 + jc][:, 0:1], r=[pcv[a * 2 + jc]], w=[cv[a]])
            if _c0 == 2:
                kb.barrier()
                return
            kvT = [kb.sb(s0, [64, S], BF16, "kvT") for _ in range(2)]
            xg = [kb.sb(s0, [128, 256], F32, "xg") for _ in range(2)]
            x2 = [kb.sb(s0, [128, 256], F32, "x2") for _ in range(2)]
            gT = [kb.sb(s0, [128, 2, 256], BF16, "gT") for _ in range(2)]
            for a in range(2):
                MSET(kb, VCx[0][:], 0.0, w=[VCx[0]]) if a == 0 else None
            for g in range(1, 4):
                MSET(kb, VCx[g][:], 0.0, w=[VCx[g]])
            i = 0
            for g in range(4):
                for a in range(2):
                    src = kvT[i % 2]
                    G_ = gT[i % 2]
                    i += 1
                    c0 = (KCC if a == 0 else VCC) + g * 64
                    kb.dma("pool", src[:], hF_d[c0:c0 + 64, :], w=[src])
                    if a == 0:
                        MSET(kb, G_[:, :, 255:256], 0.0, w=[("gpad", id(G_))])
                    for jc in range(2):
                        p = ph[jc]
                        for l in range(32):
                            MM(kb, p[:, 0:255], w1b[a][:, l, jc * 128:(jc + 1) * 128], src[:, l:l + 16 * 254 + 1:16], l == 0, l == 31, r=[w1b[a], src], w=[p])
                        if _c0 == 3:
                            continue
                        X, X2 = xg[jc], x2[jc]
                        ACT(kb, X[:, 0:255], p[:, 0:255], AF.Identity, r=[p, cv[a]], w=[X], bias=cv[a][:, jc:jc + 1], scale=1.0)
                        TT(kb, X2[:, 0:255], X[:, 0:255], X[:, 0:255], ALU.mult, r=[X], w=[X2])
                        TS(kb, X2[:, 0:255], X2[:, 0:255], 0.044715, 1.0, ALU.mult, ALU.add, r=[X2], w=[X2])
                        TT(kb, X2[:, 0:255], X2[:, 0:255], X[:, 0:255], ALU.mult, r=[X, X2], w=[X2])
                        ACT(kb, X2[:, 0:255], X2[:, 0:255], AF.Tanh, r=[X2], w=[X2], scale=0.7978845608028654)
                        STT(kb, X2[:, 0:255], X2[:, 0:255], 1.0, X[:, 0:255], ALU.add, ALU.mult, r=[X, X2], w=[X2])
                        TS(kb, G_[:, jc, 0:255], X2[:, 0:255], 0.5, None, ALU.mult, None, r=[X2], w=[G_])
                    if _c0 in (3, 4):
                        continue
                    if a == 0:
                        for jc in range(2):
                            MM(kb, pk[0:64, 0:256], w2b[0][:, jc, :], G_[:, jc, :], jc == 0, jc == 1, r=[w2b[0], G_, ("gpad", id(G_))], w=[pk])
                        CP(kb, KC[g][:], pk[0:64, 0:256], r=[pk], w=[KC[g]])
                    else:
                        for nc_ in range(2):
                            nn = 128 if nc_ == 0 else 127
                            for jc in range(2):
                                MM(kb, pv[nc_][0:nn, 0:64], G_[:, jc, nc_ * 128:nc_ * 128 + nn], w2b[1][:, jc, :], jc == 0, jc == 1, r=[w2b[1], G_], w=[pv[nc_]])
                            CP(kb, VCx[g][0:nn, nc_, 0:64], pv[nc_][0:nn, 0:64], r=[pv[nc_]], w=[VCx[g]])
                            MSET(kb, VCx[g][0:nn, nc_, 64:65], 1.0, w=[VCx[g]])
                            CP(kb, VCx[g][0:nn, nc_, 65:129], c2s[0:nn, nc_, :], r=[c2s], w=[VCx[g]], eng="act")
            kb.barrier()
        import os as _os
        _stop = int(_os.environ.get("NSA_STOP", "99"))
        if _stop == 0:
            return
        Y = kb.sb(st, [128, 32, 192], F32, "Y")
        IMP = kb.sb(st, [128, 32, 64], F32, "IMP")
        SELT = kb.sb(st, [64, S], BF16, "SELT")
        qT = [kb.sb(st, [64, S], BF16, "qT") for _ in range(2)]
        sc_f = [kb.sb(st, [128, 512], F32, "scf") for _ in range(3)]
        pT = [kb.sb(st, [128, 512], BF16, "pT") for _ in range(3)]
        rd = [kb.sb(st, [128, 1], F32, "rd") for _ in range(4)]
        psc = [kb.ps(st, [128, 512], F32, "psc") for _ in range(3)]
        pacc = [kb.ps(st, [128, 512], F32, "pacc") for _ in range(4)]
        qi = 0
        sci = 0

        def load_q(h):
            nonlocal qi
            t = qT[qi % 2]
            qi += 1
            kb.dma("pool", t[:], hF_d[QC + h * 64:QC + (h + 1) * 64, :], w=[t])
            TS(kb, t[:], t[:], 0.125, None, ALU.mult, None, r=[t], w=[t])
            return t

        def finish(acc, ncol, h, br, I, qs, init):
            r_ = rd[qs]
            tile = I * 4 + qs
            TS(kb, r_[:], acc[:, 64:65], 1e-30, None, ALU.max, None, r=[acc], w=[r_])
            kb.op("dve", lambda E: E.reciprocal(out=r_[:], in_=r_[:]), r=[r_], w=[r_])
            hl = h % 3
            if ncol > 65:
                if hl == 0:
                    TS(kb, IMP[:, tile, :], acc[:, 65:129], r_[:, 0:1], None, ALU.mult, None, r=[acc, r_], w=[("IMP", tile)])
                else:
                    STT(kb, IMP[:, tile, :], acc[:, 65:129], r_[:, 0:1], IMP[:, tile, :], ALU.mult, ALU.add, r=[acc, r_, ("IMP", tile)], w=[("IMP", tile)])
            TT(kb, r_[:], r_[:], GATE[:, tile, h * 3 + br:h * 3 + br + 1], ALU.mult, r=[r_, GATE], w=[r_])
            ydst = Y[:, tile, hl * 64:(hl + 1) * 64]
            if init:
                TS(kb, ydst, acc[:, 0:64], r_[:, 0:1], None, ALU.mult, None, r=[acc, r_], w=[("Y", tile, hl)])
            else:
                STT(kb, ydst, acc[:, 0:64], r_[:, 0:1], ydst, ALU.mult, ALU.add, r=[acc, r_, ("Y", tile, hl)], w=[("Y", tile, hl)])

        for g in range(4):
            with contextlib.ExitStack() as s1:
                bC = kb.sb(s1, [128, 5, 512], F32, "bC")
                for r in range(3):
                    h = g * 3 + r
                    q_ = load_q(h)
                    kb.dma("sp", bC[:], tab_d[h * 18 + 13:h * 18 + 18, :].rearrange("o (k q) -> k o q", q=512), w=[bC])
                    for I in range(8):
                        ets = []
                        for nc_ in range(2):
                            off = I - 4 * nc_
                            if off < 0:
                                continue
                            p = psc[sci % 3]
                            e_ = pT[sci % 3]
                            f_ = sc_f[sci % 3]
                            sci += 1
                            MM(kb, p[:], KC[g][:, nc_ * 128:(nc_ + 1) * 128], q_[:, I * 512:(I + 1) * 512], True, True, r=[KC[g], q_], w=[p])
                            if off < 5:
                                TT(kb, f_[:], p[:], bC[:, off, :], ALU.add, r=[p, bC], w=[f_])
                                ACT(kb, e_[:], f_[:], AF.Exp, r=[f_], w=[e_])
                            else:
                                ACT(kb, e_[:], p[:], AF.Exp, r=[p, CH], w=[e_], bias=CH[:, h:h + 1], scale=1.0)
                            ets.append((nc_, e_))
                        for qs in range(4):
                            acc = pacc[qs]
                            for ii, (nc_, e_) in enumerate(ets):
                                MM(kb, acc[:, 0:129], e_[:, qs * 128:(qs + 1) * 128], VCx[g][:, nc_, :], ii == 0, ii == len(ets) - 1, r=[e_, VCx[g]], w=[acc])
                            finish(acc, 129, h, 0, I, qs, True)
                kb.barrier()
            if _stop == 1:
                return
            with contextlib.ExitStack() as s2:
                AB = kb.sb(s2, [128, 2, 32, 64], F32, "AB")
                kb.dma("sp", AB[:], P["AB"], w=[AB])
                scs = [kb.sb(s2, [128, 64], F32, "scs") for _ in range(2)]
                sc2 = [kb.sb(s2, [128, 64], F32, "sc2") for _ in range(2)]
                m1 = [kb.sb(s2, [128, 8], F32, "m1") for _ in range(2)]
                m2 = [kb.sb(s2, [128, 8], F32, "m2") for _ in range(2)]
                selb = [kb.sb(s2, [128, 64], BF16, "selb") for _ in range(2)]
                ptb = kb.ps(s2, [128, 1024], BF16, "ptb")
                for tile in range(32):
                    i = tile % 2
                    TT(kb, scs[i][:], IMP[:, tile, :], AB[:, 0, tile, :], ALU.mult, r=[("IMP", tile), AB], w=[scs[i]])
                    TT(kb, scs[i][:], scs[i][:], AB[:, 1, tile, :], ALU.add, r=[scs[i], AB], w=[scs[i]])
                    kb.op("dve", lambda E, i=i: E.max(out=m1[i][:], in_=scs[i][:]), r=[scs[i]], w=[m1[i]])
                    kb.op("dve", lambda E, i=i: E.match_replace(out=sc2[i][:], in_to_replace=m1[i][:], in_values=scs[i][:], imm_value=-2.0), r=[scs[i], m1[i]], w=[sc2[i]])
                    kb.op("dve", lambda E, i=i: E.max(out=m2[i][:], in_=sc2[i][:]), r=[sc2[i]], w=[m2[i]])
                    TS(kb, m2[i][:, 7:8], m2[i][:, 7:8], 0.0, None, ALU.max, None, r=[m2[i]], w=[m2[i]])
                    TS(kb, selb[i][:], scs[i][:], m2[i][:, 7:8], -1.0, ALU.is_ge, ALU.add, r=[scs[i], m2[i]], w=[selb[i]])
                    TR(kb, ptb[0:64, (tile % 4) * 128:(tile % 4 + 1) * 128], selb[i][:], C.ident_b[:], r=[selb[i], C.ident_b], w=[ptb])
                    if tile % 4 == 3:
                        CP(kb, SELT[:, (tile - 3) * 128:(tile + 1) * 128], ptb[0:64, 0:512], r=[ptb], w=[SELT], eng="act")
                kb.barrier()
            if _stop == 2:
                return
            with contextlib.ExitStack() as s3:
                bS = kb.sb(s3, [128, 13, 512], F32, "bS")
                Eall = kb.sb(s3, [64, 32, 128], BF16, "Eall")
                kb.dma("pool", Eall[:], P["E"], w=[Eall])
                ksT = kb.sb(s3, [64, S], BF16, "ksT")
                kwT = kb.sb(s3, [64, S], BF16, "kwT")
                kb.dma("pool", ksT[:], hF_d[KSC + g * 64:KSC + (g + 1) * 64, :], w=[ksT])
                kb.dma("pool", kwT[:], hF_d[KWC + g * 64:KWC + (g + 1) * 64, :], w=[kwT])
                VSx = kb.sb(s3, [128, 32, 65], BF16, "VSx")
                VWx = kb.sb(s3, [128, 32, 65], BF16, "VWx")
                MSET(kb, VSx[:, :, 64:65], 1.0, w=[("vs1",)])
                MSET(kb, VWx[:, :, 64:65], 1.0, w=[("vw1",)])
                kb.dma("pool", VSx[:, :, 0:64], hT_d[:, VSC + g * 64:VSC + (g + 1) * 64].rearrange("(t p) c -> p t c", p=128), w=[VSx])
                kb.dma("pool", VWx[:, :, 0:64], hT_d[:, VWC + g * 64:VWC + (g + 1) * 64].rearrange("(t p) c -> p t c", p=128), w=[VWx])
                for r in range(3):
                    h = g * 3 + r
                    q_ = load_q(h)
                    kb.dma("sp", bS[:], tab_d[h * 18:h * 18 + 13, :].rearrange("o (k q) -> k o q", q=512), w=[bS])
                    for I in range(8):
                        for j in range(4 * I + 4):
                            off = j - 4 * I
                            p = psc[sci % 3]
                            e_ = pT[sci % 3]
                            f_ = sc_f[sci % 3]
                            sci += 1
                            MM(kb, p[:], ksT[:, j * 128:(j + 1) * 128], q_[:, I * 512:(I + 1) * 512], True, False, r=[ksT, q_], w=[p])
                            MM(kb, p[:], Eall[:, j, :], SELT[:, I * 512:(I + 1) * 512], False, True, r=[Eall, SELT], w=[p])
                            if off >= -1:
                                TT(kb, f_[:], p[:], bS[:, off + 1, :], ALU.add, r=[p, bS], w=[f_])
                                ACT(kb, e_[:], f_[:], AF.Exp, r=[f_], w=[e_])
                            else:
                                ACT(kb, e_[:], p[:], AF.Exp, r=[p, CH], w=[e_], bias=CH[:, h:h + 1], scale=1.0)
                            for qs in range(4):
                                if j > 4 * I + qs:
                                    continue
                                MM(kb, pacc[qs][:, 0:65], e_[:, qs * 128:(qs + 1) * 128], VSx[:, j, :], j == 0, j == 4 * I + qs, r=[e_, VSx, ("vs1",)], w=[pacc[qs]])
                        for qs in range(4):
                            finish(pacc[qs], 65, h, 1, I, qs, False)
                        for j in range(max(0, 4 * I - 4), 4 * I + 4):
                            off = j - 4 * I
                            p = psc[sci % 3]
                            e_ = pT[sci % 3]
                            f_ = sc_f[sci % 3]
                            sci += 1
                            MM(kb, p[:], kwT[:, j * 128:(j + 1) * 128], q_[:, I * 512:(I + 1) * 512], True, True, r=[kwT, q_], w=[p])
                            TT(kb, f_[:], p[:], bS[:, 5 + off + 4, :], ALU.add, r=[p, bS], w=[f_])
                            ACT(kb, e_[:], f_[:], AF.Exp, r=[f_], w=[e_])
                            for qs in range(4):
                                lo = max(0, 4 * I + qs - 4)
                                hi = 4 * I + qs
                                if j < lo or j > hi:
                                    continue
                                MM(kb, pacc[qs][:, 0:65], e_[:, qs * 128:(qs + 1) * 128], VWx[:, j, :], j == lo, j == hi, r=[e_, VWx, ("vw1",)], w=[pacc[qs]])
                        for qs in range(4):
                            finish(pacc[qs], 65, h, 2, I, qs, False)
                kb.dma("sp", mix_d[:, g * 192:(g + 1) * 192].rearrange("(t p) c -> p t c", p=128), Y[:], r=[("Y", t_, hl_) for t_ in range(32) for hl_ in range(3)])
                kb.barrier()


LAYER_KIND = ["nsa", "gla", "rwkv", "nsa"]
LAYER_COLS = [NSA_COLS, GLA_COLS, RWKV_COLS, NSA_COLS]


def gathered_specs():
    sp = [("nsa_w_in0", D, NSA_COLS, F32), ("nsa_w_in1", D, NSA_COLS, F32), ("gla_w_in", D, GLA_COLS, F32), ("rwkv_w_in", D, RWKV_COLS, F32),
          ("cmp_w1_0", 4096, 256, F32), ("cmp_w1_1", 4096, 256, F32), ("tab", 216 * 128, 512, F32)]
    for l in range(DEPTH):
        sp += [("mem_w_kv%d" % l, D, 512, F32), ("w_out%d" % l, D, D, F32)]
    for l in range(DEPTH):
        sp += [("wg%d" % l, NE * D, 2 * D, BF16), ("wd%d" % l, NE * D, D, BF16)]
    return sp


SMALL_SPECS = dict(
    ident=[128, 128], glac=[3, 128, 128], nsa_AB=[128, 2, 32, 64], nsa_c2s=[128, 2, 64], nsa_E=[64, 32, 128], rel31=[12],
    nsa_gate_b=[2, 36], nsa_pe=[2, 2, 32, 64], nsa_w2=[2, 2, 256, 64],
    gla_w_a2=[16, 384], gla_b_a=[384], gla_b_og=[768], gla_norm_w=[192],
    rwkv_mu=[2560], rwkv_w0=[768], rwkv_w2=[64, 768], rwkv_a0=[768], rwkv_a2=[64, 768], rwkv_g2=[128, 768], rwkv_k_k=[768], rwkv_k_a=[768],
    rwkv_r_k=[768], rwkv_ln_w=[768], rwkv_ln_b=[768],
    ln1_g=[4, D], ln1_b=[4, D], ln2_g=[4, D], ln2_b=[4, D], router_w=[4, D, NE], router_b=[4, NE], exp_b_gu=[4, NE, 2 * D], exp_b_dn=[4, NE, D],
)


MODE = "replicate"
NUSE = 8


def build_program(depth=DEPTH, mode=MODE, nseq=None):
    nc = bass.Bass("TRN2", target_bir_lowering=False)
    if nseq is None:
        nseq = 1 if mode == "allgather" else NCORES // NUSE

    def ein(n, shape, dt=F32):
        return nc.dram_tensor(n, list(shape), dt, kind="ExternalInput").ap()

    x_all = ein("x", [nseq, S, D])
    mem_all = ein("mem", [nseq, 256, D])
    sm = {k: ein(k, v) for k, v in SMALL_SPECS.items()}
    y_all = nc.dram_tensor("y", [nseq, S, D], F32, kind="ExternalOutput").ap()
    kb = KB(nc)
    C = Consts(kb, sm["ident"])
    G = {}
    for (name, rows, cols, dt) in gathered_specs():
        step = 1024
        if mode == "allgather":
            rs = rows // NCORES
            src = ein("sh_" + name, [rs, cols])
            bounce = kb.dram([rs, cols], dt, "bn_" + name).ap()
            full = kb.dram([rows, cols], dt, "g_" + name).ap()
            for r0 in range(0, rs, step):
                r1 = min(rs, r0 + step)
                kb.dma("pool", bounce[r0:r1, :], src[r0:r1, :], w=[("bn", name, r0)])
            kb.coll(name, "AllGather", [bounce], [full], r=[("bn", name, r0) for r0 in range(0, rs, step)], w=[("g", name)])
            G[name] = full
        else:
            src = ein("sh_" + name, [rows, cols])
            if dt == F32:
                G[name] = src
            else:
                full = kb.dram([rows, cols], dt, "g_" + name).ap()
                for r0 in range(0, rows, step):
                    kb.dma("pool", full[r0:r0 + step, :], src[r0:r0 + step, :], w=[("g", name, r0)])
                G[name] = full

    def need(nm):
        if mode == "allgather":
            kb.need(nm)

    hF = kb.dram([RWKV_COLS, S], F32, "hF").ap()
    hT = kb.dram([S, RWKV_COLS], F32, "hT").ap()
    mix = kb.dram([S, D], F32, "mix").ap()
    x1 = kb.dram([S, D], F32, "x1").ap()
    xs = [kb.dram([S, D], F32, "xs%d" % i).ap() for i in range(2)]
    rscr = {k: kb.dram([S, TOKW], F32, "rw" + k).ap() for k in "WKABRVGO"}
    rscr["VP"] = kb.dram([128, 6, S], F32, "rwVP").ap()
    if mode != "allgather":
        kb.barrier()
    for sq in range(nseq):
        x_in = x_all[sq]
        mem_d = mem_all[sq]
        y_d = y_all[sq]
        nsa_i = 0
        for l in range(depth):
            kind = LAYER_KIND[l]
            ncols = LAYER_COLS[l]
            hFv, hTv = hF[0:ncols, :], hT[:, 0:ncols]
            if kind == "nsa":
                wname = "nsa_w_in%d" % nsa_i
            elif kind == "gla":
                wname = "gla_w_in"
            else:
                wname = "rwkv_w_in"
            need(wname)
            stage_proj(kb, C, x_in, G[wname], ncols, hFv, hTv)
            if kind == "nsa":
                j = nsa_i
                nsa_i += 1
                need("tab")
                need("cmp_w1_%d" % j)
                P = dict(gate_b=sm["nsa_gate_b"][j], pe=sm["nsa_pe"][j], w1=G["cmp_w1_%d" % j].rearrange("(a r) c -> a r c", a=2), w2=sm["nsa_w2"][j],
                         rel31=sm["rel31"], AB=sm["nsa_AB"], c2s=sm["nsa_c2s"], E=sm["nsa_E"])
                stage_nsa(kb, C, hFv, hTv, P, G["tab"].rearrange("(o k) q -> o (k q)", k=128), mix)
            elif kind == "gla":
                stage_gla(kb, C, hFv, hTv, sm["gla_w_a2"], sm["gla_b_a"], sm["gla_b_og"], sm["gla_norm_w"], sm["glac"], mix)
            else:
                P = {k: sm["rwkv_" + k] for k in ("mu", "w0", "w2", "a0", "a2", "g2", "k_k", "k_a", "r_k", "ln_w", "ln_b")}
                stage_rwkv(kb, C, hTv, P, mix, rscr)
            need("mem_w_kv%d" % l)
            stage_memattn(kb, C, mem_d, G["mem_w_kv%d" % l], hFv, ncols - MEMW, mix)
            need("w_out%d" % l)
            stage_outproj_ln(kb, C, mix, G["w_out%d" % l], x_in, sm["ln1_g"][l], sm["ln1_b"][l], x1)
            need("wg%d" % l)
            need("wd%d" % l)
            xo = y_d if l == depth - 1 else xs[l % 2]
            stage_moe(kb, C, x1, sm["router_w"][l], sm["router_b"][l], G["wg%d" % l].rearrange("(e r) n -> e r n", e=NE), G["wd%d" % l].rearrange("(e r) n -> e r n", e=NE),
                      sm["exp_b_gu"][l], sm["exp_b_dn"][l], sm["ln2_g"][l], sm["ln2_b"][l], xo)
            x_in = xo
    kb.emit()
    return nc


def host_inputs(inp, mode=MODE):
    f = lambda a: np.ascontiguousarray(np.asarray(a, dtype=np.float32))
    AB, c2s, E = nsa_static_np()
    rel = f(inp["rel_table"])
    small = dict(
        ident=np.eye(128, dtype=np.float32), glac=gla_consts_np(), nsa_AB=AB, nsa_c2s=c2s, nsa_E=E, rel31=f(rel[31]),
        nsa_gate_b=f(inp["nsa_gate_b"]), nsa_pe=f(inp["nsa_cmp_pe"]), nsa_w2=f(inp["nsa_cmp_w2"]),
        gla_w_a2=f(inp["gla_w_a2"][0]), gla_b_a=f(inp["gla_b_a"][0]), gla_b_og=f(inp["gla_b_og"][0]), gla_norm_w=f(inp["gla_norm_w"][0]),
        ln1_g=f(inp["ln1_g"]), ln1_b=f(inp["ln1_b"]), ln2_g=f(inp["ln2_g"]), ln2_b=f(inp["ln2_b"]),
        router_w=f(inp["router_w"]), router_b=f(inp["router_b"]), exp_b_gu=f(inp["exp_b_gu"]), exp_b_dn=f(inp["exp_b_dn"]),
    )
    for k in ("mu", "w0", "w2", "a0", "a2", "g2", "k_k", "k_a", "r_k", "ln_w", "ln_b"):
        small["rwkv_" + k] = f(inp["rwkv_" + k][0]).reshape(SMALL_SPECS["rwkv_" + k])
    full = {
        "nsa_w_in0": f(inp["nsa_w_in"][0]), "nsa_w_in1": f(inp["nsa_w_in"][1]), "gla_w_in": f(inp["gla_w_in"][0]), "rwkv_w_in": f(inp["rwkv_w_in"][0]),
        "cmp_w1_0": f(inp["nsa_cmp_w1"][0]).reshape(4096, 256), "cmp_w1_1": f(inp["nsa_cmp_w1"][1]).reshape(4096, 256),
        "tab": nsa_tables_np(rel).reshape(216 * 128, 512),
    }
    for l in range(DEPTH):
        full["mem_w_kv%d" % l] = f(inp["mem_w_kv"][l])
        full["w_out%d" % l] = f(inp["w_out"][l])
    maps = []
    x = np.asarray(inp["x"], dtype=np.float32)
    mem = np.asarray(inp["mem"], dtype=np.float32)
    wgu = np.asarray(inp["exp_w_gu"], dtype=np.float32)
    wdn = np.asarray(inp["exp_w_dn"], dtype=np.float32)
    if mode == "allgather":
        for c in range(NCORES):
            m = {"x": f(x[c:c + 1]), "mem": f(mem[c:c + 1])}
            m.update(small)
            for k, v in full.items():
                rs = v.shape[0] // NCORES
                m["sh_" + k] = np.ascontiguousarray(v[c * rs:(c + 1) * rs])
            for l in range(DEPTH):
                m["sh_wg%d" % l] = np.ascontiguousarray(wgu[l, 4 * c:4 * c + 4]).reshape(4 * D, 2 * D)
                m["sh_wd%d" % l] = np.ascontiguousarray(wdn[l, 4 * c:4 * c + 4]).reshape(4 * D, D)
            maps.append(m)
    else:
        nseq = NCORES // NUSE
        shared = dict(small)
        for k, v in full.items():
            shared["sh_" + k] = v
        for l in range(DEPTH):
            shared["sh_wg%d" % l] = np.ascontiguousarray(wgu[l]).reshape(NE * D, 2 * D)
            shared["sh_wd%d" % l] = np.ascontiguousarray(wdn[l]).reshape(NE * D, D)
        for c in range(NUSE):
            m = {"x": f(x[c * nseq:(c + 1) * nseq]), "mem": f(mem[c * nseq:(c + 1) * nseq])}
            m.update(shared)
            maps.append(m)
    return maps


_NC_CACHE = {}


def kernel(**inputs):
    if "nc" not in _NC_CACHE:
        _NC_CACHE["nc"] = build_program()
    nc = _NC_CACHE["nc"]
    maps = host_inputs(inputs)
    res = run_bass_kernel_spmd(nc, maps, core_ids=list(range(len(maps))))
    return np.concatenate([np.asarray(r["y"], dtype=np.float32) for r in res.results], axis=0)
```

```python
import contextlib
import numpy as np
import concourse.bass as bass
import concourse.mybir as mybir
from concourse.bass_utils import run_bass_kernel_spmd

F32 = mybir.dt.float32
BF16 = mybir.dt.bfloat16
ALU = mybir.AluOpType
AF = mybir.ActivationFunctionType
AX = mybir.AxisListType

D = 1024
S = 4096
NCORES = 8
DEPTH = 4
MEMW = 256
TOKW = 768
ALPHA = (2 * DEPTH) ** 0.25
LN_EPS = 1e-5
NSA_COLS = 2596
GLA_COLS = 2576
RWKV_COLS = 2816
NE = 32


class Op:
    __slots__ = ("eng", "fn", "deps", "need_inc", "val", "is_dma", "dsem", "dval", "coll")

    def __init__(self, eng, fn, is_dma):
        self.eng, self.fn, self.is_dma = eng, fn, is_dma
        self.deps, self.need_inc, self.val = [], False, 0
        self.dsem, self.dval = None, 0
        self.coll = False


class Tl:
    def __init__(self, t):
        self.t = t

    def __getitem__(self, idx):
        return self.t[idx]


class KB:
    ENGS = ("pe", "dve", "act", "pool", "sp")
    KD = 8

    def __init__(self, nc):
        self.nc = nc
        self.es = contextlib.ExitStack()
        self.e = {"pe": nc.tensor, "dve": nc.vector, "act": nc.scalar, "pool": nc.gpsimd, "sp": nc.sync}
        self.ops = []
        self.last = {e: None for e in self.ENGS}
        self.res = {}
        self.pending = {e: [] for e in self.ENGS}
        self.nd = {e: 0 for e in self.ENGS}
        self.slot_last = {}
        self.csem = {e: nc.alloc_semaphore(name="c_" + e) for e in self.ENGS}
        self.dsems = {}
        for q in ("sp", "pool", "act"):
            for s in range(self.KD):
                self.dsems[(q, s)] = nc.alloc_semaphore(name="d_%s%d" % (q, s))
        self.uid = 0
        self.colls = {}
        self._clear_sems()
        nc.all_engine_barrier()

    def _clear_sems(self):
        for h in list(self.csem.values()) + list(self.dsems.values()):
            self.nc.gpsimd.sem_clear(h)

    def name(self, p):
        self.uid += 1
        return "%s_%d" % (p, self.uid)

    def sb(self, st, shape, dt, name="sb"):
        return Tl(st.enter_context(self.nc.sbuf_tensor(self.name(name), list(shape), dt)))

    def ps(self, st, shape, dt, name="ps"):
        return Tl(st.enter_context(self.nc.psum_tensor(self.name(name), list(shape), dt)))

    def dram(self, shape, dt, name="scr"):
        return self.nc.dram_tensor(self.name(name), list(shape), dt)

    def coll(self, name, kind, ins, outs, r=(), w=()):
        sem = self.nc.alloc_semaphore(name="cc_" + name)
        self.nc.gpsimd.sem_clear(sem)
        self.dsems[("cc", name)] = sem
        o = self.op("pool", lambda E: E.collective_compute(kind, ALU.bypass, replica_groups=[list(range(NCORES))], ins=ins, outs=outs), r=r, w=w, dma=True, coll=("cc", name))
        self.colls[name] = o
        return o

    def need(self, name):
        o = self.colls[name]
        for e in self.ENGS:
            self.pending[e] = list(self.pending[e]) + [o]

    def op(self, eng, fn, r=(), w=(), dma=False, coll=None):
        o = Op(eng, fn, dma)
        deps = list(self.pending[eng])
        self.pending[eng] = []
        for k in r:
            st = self.res.get(k)
            if st:
                deps += st[0]
        for k in w:
            st = self.res.get(k)
            if st:
                deps += st[0]
                deps += st[1]
        seen = set()
        for d in deps:
            if d is o or id(d) in seen:
                continue
            if (not d.is_dma) and d.eng == eng and eng == "pe":
                continue
            seen.add(id(d))
            o.deps.append(d)
            if not d.is_dma:
                d.need_inc = True
        if coll is not None:
            o.coll = True
            o.dsem = coll
            o.dval = 1
        elif dma:
            slot = self.nd[eng] % self.KD
            o.dsem = (eng, slot)
            o.dval = 16 * (self.nd[eng] // self.KD + 1)
            prev = self.slot_last.get((eng, slot))
            if prev is not None and id(prev) not in seen:
                o.deps.append(prev)
            self.slot_last[(eng, slot)] = o
            self.nd[eng] += 1
        for k in r:
            if k in w:
                continue
            st = self.res.setdefault(k, ([], []))
            if not dma:
                st[1][:] = [x for x in st[1] if x.is_dma or x.eng != eng]
            st[1].append(o)
        for k in w:
            self.res[k] = ([o], [])
        self.ops.append(o)
        if not dma:
            self.last[eng] = o
        return o

    def dma(self, q, out, in_, r=(), w=(), **kw):
        return self.op(q, lambda E: E.dma_start(out=out, in_=in_, **kw), r=r, w=w, dma=True)

    def barrier(self):
        targets = [o for o in self.last.values() if o is not None and not o.is_dma]
        targets += list(self.slot_last.values())
        for e in self.ENGS:
            self.pending[e] = list(self.pending[e]) + targets
        self.res = {}

    def emit(self):
        self.barrier()
        final = self.pending["sp"]
        for d in final:
            if not d.is_dma:
                d.need_inc = True
        cnt = {e: 0 for e in self.ENGS}
        for o in self.ops:
            if (not o.is_dma) and o.need_inc:
                cnt[o.eng] += 1
                o.val = cnt[o.eng]
        waited = {}

        def do_wait(engname, E, d):
            if d.is_dma:
                key, val, sem = ("d",) + d.dsem, d.dval, self.dsems[d.dsem]
            else:
                key, val, sem = ("c", d.eng), d.val, self.csem[d.eng]
            if waited.get((engname, key), 0) >= val:
                return
            waited[(engname, key)] = val
            E.wait_ge(sem, val)

        for o in self.ops:
            E = self.e[o.eng]
            for d in o.deps:
                do_wait(o.eng, E, d)
            ins = o.fn(E)
            if o.coll:
                ins.then_inc(self.dsems[o.dsem])
            elif o.is_dma:
                ins.then_inc(self.dsems[o.dsem], 16)
            elif o.need_inc:
                ins.then_inc(self.csem[o.eng], 1)
        for d in final:
            do_wait("sp", self.e["sp"], d)
        self.nc.all_engine_barrier()
        self._clear_sems()
        self.nc.all_engine_barrier()
        self.es.close()


def evac(kb, i, out, in_, r, w):
    if i % 2 == 0:
        kb.op("dve", lambda E: E.tensor_copy(out=out, in_=in_), r=r, w=w)
    else:
        kb.op("act", lambda E: E.copy(out=out, in_=in_), r=r, w=w)


class Consts:
    def __init__(self, kb, ident_d):
        es = kb.es
        self.ident_f = kb.sb(es, [128, 128], F32, "identf")
        self.ident_b = kb.sb(es, [128, 128], BF16, "identb")
        kb.dma("sp", self.ident_f[:], ident_d, w=[self.ident_f])
        kb.op("dve", lambda E: E.tensor_copy(out=self.ident_b[:], in_=self.ident_f[:]), r=[self.ident_f], w=[self.ident_b])


def load_xT(kb, C, x_d, t0, nsub, xin, xb, xT, pst, ncolchunks=8, q="sp", off=0):
    cols = ncolchunks * 128
    kb.dma(q, xin[:, 0:nsub, 0:cols], x_d[t0:t0 + nsub * 128, 0:cols].rearrange("(s p) c -> p s c", p=128), w=[xin])
    kb.op("act", lambda E: E.copy(out=xb[:, 0:nsub, 0:cols], in_=xin[:, 0:nsub, 0:cols]), r=[xin], w=[xb])
    for kc in range(ncolchunks):
        p = pst[kc % len(pst)]
        for s in range(nsub):
            kb.op("pe", lambda E, s=s, kc=kc, p=p: E.transpose(out=p[:, s * 128:(s + 1) * 128], in_=xb[:, s, kc * 128:(kc + 1) * 128], identity=C.ident_b[:]),
                  r=[xb, C.ident_b], w=[p])
        evac(kb, kc, xT[:, kc, off:off + nsub * 128], p[:, 0:nsub * 128], r=[p], w=[xT])


def stage_proj(kb, C, x_d, w_d, ncols, hF_d, hT_d):
    with contextlib.ExitStack() as st:
        W = kb.sb(st, [128, 8, ncols], BF16, "W")
        for kc in range(8):
            kb.dma("pool", W[:, kc, :], w_d[kc * 128:(kc + 1) * 128, :], w=[W])
        xin = [kb.sb(st, [128, 4, D], F32, "xin") for _ in range(2)]
        xb = [kb.sb(st, [128, 4, D], BF16, "xb") for _ in range(2)]
        xT = [kb.sb(st, [128, 8, 512], BF16, "xT") for _ in range(2)]
        pst = [kb.ps(st, [128, 1024], BF16, "pst") for _ in range(2)]
        pm = [kb.ps(st, [128, 512], F32, "pm") for _ in range(4)]
        oF = [kb.sb(st, [128, 512], F32, "oF") for _ in range(3)]
        oT = [kb.sb(st, [128, ncols], F32, "oT") for _ in range(2)]
        nfc = (ncols + 127) // 128
        ncb = (ncols + 511) // 512
        cnt = 0
        for tt in range(S // 512):
            b = tt % 2
            load_xT(kb, C, x_d, tt * 512, 4, xin[b], xb[b], xT[b], pst)
            for c in range(nfc):
                mc = min(128, ncols - c * 128)
                p = pm[cnt % 4]
                o = oF[cnt % 3]
                for kc in range(8):
                    kb.op("pe", lambda E, p=p, c=c, mc=mc, kc=kc, b=b: E.matmul(out=p[0:mc, :], lhsT=W[:, kc, c * 128:c * 128 + mc], rhs=xT[b][:, kc, :], start=(kc == 0), stop=(kc == 7)),
                          r=[W, xT[b]], w=[p])
                evac(kb, cnt, o[0:mc, :], p[0:mc, :], r=[p], w=[o])
                kb.dma("sp", hF_d[c * 128:c * 128 + mc, tt * 512:(tt + 1) * 512], o[0:mc, :], r=[o])
                cnt += 1
            for s in range(4):
                ot = oT[s % 2]
                for cb in range(ncb):
                    nn = min(512, ncols - cb * 512)
                    p = pm[cnt % 4]
                    for kc in range(8):
                        kb.op("pe", lambda E, p=p, cb=cb, nn=nn, kc=kc, b=b, s=s: E.matmul(out=p[:, 0:nn], lhsT=xT[b][:, kc, s * 128:(s + 1) * 128], rhs=W[:, kc, cb * 512:cb * 512 + nn], start=(kc == 0), stop=(kc == 7)),
                              r=[W, xT[b]], w=[p])
                    evac(kb, cnt, ot[:, cb * 512:cb * 512 + nn], p[:, 0:nn], r=[p], w=[ot])
                    cnt += 1
                t0 = tt * 512 + s * 128
                kb.dma("pool", hT_d[t0:t0 + 128, :], ot[:], r=[ot])
        kb.barrier()


class LNBufs:
    def __init__(self, kb, st, g_d, b_d):
        self.g = kb.sb(st, [128, D], F32, "lng")
        self.b = kb.sb(st, [128, D], F32, "lnb")
        kb.dma("sp", self.g[:], g_d.partition_broadcast(128), w=[self.g])
        kb.dma("sp", self.b[:], b_d.partition_broadcast(128), w=[self.b])
        self.stats = kb.sb(st, [128, 2, 6], F32, "lnst")
        self.mv = kb.sb(st, [128, 2], F32, "lnmv")
        self.acc = kb.sb(st, [128, 2], F32, "lnacc")
        self.rstd = kb.sb(st, [128, 1], F32, "lnrs")


def ln_tile(kb, L, z, zap, out, outap):
    kb.op("act", lambda E: E.activation(out=outap, in_=zap, func=AF.Identity, accum_out=L.acc[:, 0:1]), r=[z], w=[out, L.acc])
    kb.op("act", lambda E: E.activation(out=outap, in_=zap, func=AF.Square, accum_out=L.acc[:, 1:2]), r=[z], w=[out, L.acc])
    kb.op("act", lambda E: E.mul(out=L.mv[:], in_=L.acc[:], mul=1.0 / D), r=[L.acc], w=[L.mv])
    kb.op("dve", lambda E: E.scalar_tensor_tensor(out=L.rstd[:], in0=L.mv[:, 0:1], scalar=-1.0, in1=L.mv[:, 0:1], op0=ALU.mult, op1=ALU.mult), r=[L.mv], w=[L.rstd])
    kb.op("dve", lambda E: E.tensor_tensor(out=L.rstd[:], in0=L.rstd[:], in1=L.mv[:, 1:2], op=ALU.add), r=[L.mv, L.rstd], w=[L.rstd])
    kb.op("act", lambda E: E.activation(out=L.rstd[:], in_=L.rstd[:], func=AF.Sqrt, bias=L.epsb[:], scale=1.0), r=[L.rstd, L.epsb], w=[L.rstd])
    kb.op("dve", lambda E: E.reciprocal(out=L.rstd[:], in_=L.rstd[:]), r=[L.rstd], w=[L.rstd])
    kb.op("dve", lambda E: E.tensor_scalar(out=zap, in0=zap, scalar1=L.mv[:, 0:1], scalar2=L.rstd[:, 0:1], op0=ALU.subtract, op1=ALU.mult), r=[z, L.mv, L.rstd], w=[z])
    kb.op("dve", lambda E: E.tensor_tensor(out=zap, in0=zap, in1=L.g[:], op=ALU.mult), r=[z, L.g], w=[z])
    kb.op("dve", lambda E: E.tensor_tensor(out=outap, in0=zap, in1=L.b[:], op=ALU.add), r=[z, L.b], w=[out])


def make_eps(kb, st, L):
    L.epsb = kb.sb(st, [128, 1], F32, "eps")
    kb.op("dve", lambda E: E.memset(L.epsb[:], LN_EPS), w=[L.epsb])


def stage_memattn(kb, C, mem_d, wkv_d, hF_d, qc0, mix_d):
    with contextlib.ExitStack() as st:
        W = kb.sb(st, [128, 8, 512], BF16, "Wkv")
        for kc in range(8):
            kb.dma("pool", W[:, kc, :], wkv_d[kc * 128:(kc + 1) * 128, :], w=[W])
        xin = kb.sb(st, [128, 2, D], F32, "min")
        xb = kb.sb(st, [128, 2, D], BF16, "mb")
        memT = kb.sb(st, [128, 8, 256], BF16, "memT")
        pst = [kb.ps(st, [128, 1024], BF16, "pst") for _ in range(2)]
        load_xT(kb, C, mem_d, 0, 2, xin, xb, memT, pst)
        kT = kb.sb(st, [64, 4, 256], BF16, "kT")
        Vx = kb.sb(st, [128, 2, 4, 65], BF16, "Vx")
        kb.op("dve", lambda E: E.memset(Vx[:], 1.0), w=[Vx])
        pm = [kb.ps(st, [128, 512], F32, "pm") for _ in range(2)]
        for h in range(4):
            p = pm[h % 2]
            for kc in range(8):
                kb.op("pe", lambda E, p=p, h=h, kc=kc: E.matmul(out=p[0:64, 0:256], lhsT=W[:, kc, h * 64:(h + 1) * 64], rhs=memT[:, kc, :], start=(kc == 0), stop=(kc == 7)), r=[W, memT], w=[p])
            evac(kb, h, kT[:, h, :], p[0:64, 0:256], r=[p], w=[kT])
        for mc in range(2):
            p = pm[mc % 2]
            for kc in range(8):
                kb.op("pe", lambda E, p=p, mc=mc, kc=kc: E.matmul(out=p[:, 0:256], lhsT=memT[:, kc, mc * 128:(mc + 1) * 128], rhs=W[:, kc, 256:512], start=(kc == 0), stop=(kc == 7)), r=[W, memT], w=[p])
            kb.op("dve", lambda E, p=p, mc=mc: E.tensor_copy(out=Vx[:, mc, :, 0:64], in_=p[:, 0:256].rearrange("p (h d) -> p h d", d=64)), r=[p], w=[Vx])
        qm = [kb.sb(st, [64, 4, 512], BF16, "qm") for _ in range(2)]
        pT = [kb.sb(st, [128, 512], BF16, "pT") for _ in range(4)]
        po = [kb.ps(st, [128, 4, 65], F32, "po") for _ in range(2)]
        ym = [kb.sb(st, [128, 4, 256], F32, "ym") for _ in range(2)]
        rc = [kb.sb(st, [128, 4], F32, "rc") for _ in range(2)]
        cnt = 0
        for tt in range(S // 512):
            q = qm[tt % 2]
            y = ym[tt % 2]
            kb.dma("pool", q[:], hF_d[qc0:qc0 + 256, tt * 512:(tt + 1) * 512].rearrange("(h d) t -> d h t", d=64), w=[q])
            for h in range(4):
                pts = []
                for mc in range(2):
                    p = pm[cnt % 2]
                    t = pT[cnt % 4]
                    cnt += 1
                    kb.op("pe", lambda E, p=p, h=h, mc=mc, q=q: E.matmul(out=p[:], lhsT=kT[:, h, mc * 128:(mc + 1) * 128], rhs=q[:, h, :], start=True, stop=True), r=[kT, q], w=[p])
                    kb.op("act", lambda E, p=p, t=t: E.activation(out=t[:], in_=p[:], func=AF.Exp, scale=0.125), r=[p], w=[t])
                    pts.append(t)
                o = po[h % 2]
                for qs in range(4):
                    for mc in range(2):
                        kb.op("pe", lambda E, o=o, qs=qs, mc=mc, h=h, t=pts[mc]: E.matmul(out=o[:, qs, :], lhsT=t[:, qs * 128:(qs + 1) * 128], rhs=Vx[:, mc, h, :], start=(mc == 0), stop=(mc == 1)), r=[pts[mc], Vx], w=[o])
                r_ = rc[h % 2]
                kb.op("dve", lambda E, o=o, r_=r_: E.reciprocal(out=r_[:], in_=o[:, :, 64]), r=[o], w=[r_])
                for qs in range(4):
                    kb.op("dve", lambda E, o=o, r_=r_, qs=qs, h=h, y=y: E.tensor_scalar(out=y[:, qs, h * 64:(h + 1) * 64], in0=o[:, qs, 0:64], scalar1=r_[:, qs:qs + 1], scalar2=None, op0=ALU.mult), r=[o, r_], w=[y])
            kb.dma("sp", mix_d[tt * 512:(tt + 1) * 512, TOKW:D].rearrange("(s p) c -> p s c", p=128), y[:], r=[y])
        kb.barrier()


def stage_outproj_ln(kb, C, mix_d, wout_d, xres_d, g_d, b_d, xout_d):
    with contextlib.ExitStack() as st:
        W = kb.sb(st, [128, 8, D], BF16, "Wo")
        for kc in range(8):
            kb.dma("pool", W[:, kc, :], wout_d[kc * 128:(kc + 1) * 128, :], w=[W])
        L = LNBufs(kb, st, g_d, b_d)
        make_eps(kb, st, L)
        xin = [kb.sb(st, [128, 4, D], F32, "xin") for _ in range(2)]
        xb = [kb.sb(st, [128, 4, D], BF16, "xb") for _ in range(2)]
        xT = [kb.sb(st, [128, 8, 512], BF16, "xT") for _ in range(2)]
        xr = [kb.sb(st, [128, 4, D], F32, "xr") for _ in range(2)]
        z = [kb.sb(st, [128, D], F32, "z") for _ in range(2)]
        zo = [kb.sb(st, [128, D], F32, "zo") for _ in range(2)]
        pst = [kb.ps(st, [128, 1024], BF16, "pst") for _ in range(2)]
        pm = [kb.ps(st, [128, 512], F32, "pm") for _ in range(4)]
        cnt = 0
        for tt in range(S // 512):
            b = tt % 2
            load_xT(kb, C, mix_d, tt * 512, 4, xin[b], xb[b], xT[b], pst)
            kb.dma("pool", xr[b][:], xres_d[tt * 512:(tt + 1) * 512, :].rearrange("(s p) c -> p s c", p=128), w=[xr[b]])
            for s in range(4):
                zz = z[s % 2]
                oo = zo[s % 2]
                for hh in range(2):
                    p = pm[cnt % 4]
                    cnt += 1
                    for kc in range(8):
                        kb.op("pe", lambda E, p=p, kc=kc, b=b, s=s, hh=hh: E.matmul(out=p[:], lhsT=xT[b][:, kc, s * 128:(s + 1) * 128], rhs=W[:, kc, hh * 512:(hh + 1) * 512], start=(kc == 0), stop=(kc == 7)), r=[xT[b], W], w=[p])
                    kb.op("dve", lambda E, p=p, zz=zz, b=b, s=s, hh=hh: E.scalar_tensor_tensor(out=zz[:, hh * 512:(hh + 1) * 512], in0=xr[b][:, s, hh * 512:(hh + 1) * 512], scalar=ALPHA, in1=p[:], op0=ALU.mult, op1=ALU.add), r=[xr[b], p], w=[zz])
                ln_tile(kb, L, zz, zz[:], oo, oo[:])
                t0 = tt * 512 + s * 128
                kb.dma("sp", xout_d[t0:t0 + 128, :], oo[:], r=[oo])
        kb.barrier()


def stage_moe(kb, C, x1_d, wr_d, br_d, wg_d, wd_d, bgu_d, bdn_d, g_d, b_d, xout_d):
    with contextlib.ExitStack() as st:
        Wr = kb.sb(st, [128, 8, NE], F32, "Wr")
        kb.dma("sp", Wr[:], wr_d.rearrange("(kc p) e -> p kc e", p=128), w=[Wr])
        brb = kb.sb(st, [128, NE], F32, "brb")
        kb.dma("sp", brb[:], br_d.partition_broadcast(128), w=[brb])
        bdn = kb.sb(st, [NE, D], F32, "bdn")
        kb.dma("sp", bdn[:], bdn_d, w=[bdn])
        bguT = kb.sb(st, [128, 16, NE], F32, "bguT")
        with contextlib.ExitStack() as st0:
            braw = kb.sb(st0, [NE, 2 * D], F32, "braw")
            kb.dma("sp", braw[:], bgu_d, w=[braw])
            pb = kb.ps(st0, [128, 16, NE], F32, "pb")
            for c in range(16):
                kb.op("pe", lambda E, c=c: E.transpose(out=pb[:, c, :], in_=braw[:, c * 128:(c + 1) * 128], identity=C.ident_f[0:NE, 0:NE]), r=[braw, C.ident_f], w=[pb])
            kb.op("dve", lambda E: E.tensor_copy(out=bguT[:], in_=pb[:]), r=[pb], w=[bguT])
            kb.barrier()
        L = LNBufs(kb, st, g_d, b_d)
        make_eps(kb, st, L)
        Wg = [kb.sb(st, [128, 8, 2 * D], BF16, "Wg") for _ in range(2)]
        Wd = [kb.sb(st, [128, 8, D], BF16, "Wd") for _ in range(2)]
        xT = kb.sb(st, [128, 8, 1024], BF16, "xT")
        yacc = [kb.sb(st, [128, D], F32, "yacc") for _ in range(8)]
        gates = kb.sb(st, [128, 8, NE], F32, "gates")
        gT = kb.sb(st, [NE, 8, 128], F32, "gT")
        ecnt = 0
        for sup in range(S // 1024):
            T0 = sup * 1024
            with contextlib.ExitStack() as s1:
                xin = kb.sb(s1, [128, 4, D], F32, "xin")
                xb = kb.sb(s1, [128, 4, D], BF16, "xb")
                xTf = [kb.sb(s1, [128, 8, 128], F32, "xTf") for _ in range(2)]
                pst = [kb.ps(s1, [128, 1024], BF16, "pst") for _ in range(2)]
                ptf = [kb.ps(s1, [128, 4, 128], F32, "ptf") for _ in range(2)]
                plg = [kb.ps(s1, [128, 512], F32, "plg") for _ in range(2)]
                sm = [dict(lg=kb.sb(s1, [128, NE], F32, "lg"), m8=kb.sb(s1, [128, 8], F32, "m8"), nm=kb.sb(s1, [128, 1], F32, "nm"),
                           mk=kb.sb(s1, [128, NE], F32, "mk"), ex=kb.sb(s1, [128, NE], F32, "ex"), ss=kb.sb(s1, [128, 1], F32, "ss")) for _ in range(2)]
                for half in range(2):
                    load_xT(kb, C, x1_d, T0 + half * 512, 4, xin, xb, xT, pst, off=half * 512)
                    for s_ in range(4):
                        tile = half * 4 + s_
                        xf = xTf[tile % 2]
                        for kc in range(8):
                            p = ptf[kc // 4]
                            kb.op("pe", lambda E, p=p, kc=kc, s_=s_: E.transpose(out=p[:, kc % 4, :], in_=xin[:, s_, kc * 128:(kc + 1) * 128], identity=C.ident_f[:]), r=[xin, C.ident_f], w=[p])
                            if kc % 4 == 3:
                                evac(kb, kc // 4, xf[:, kc - 3:kc + 1, :], p[:], r=[p], w=[xf])
                        pl_ = plg[tile % 2]
                        for kc in range(8):
                            kb.op("pe", lambda E, pl_=pl_, kc=kc, xf=xf: E.matmul(out=pl_[:, 0:NE], lhsT=xf[:, kc, :], rhs=Wr[:, kc, :], start=(kc == 0), stop=(kc == 7)), r=[xf, Wr], w=[pl_])
                        m = sm[tile % 2]
                        kb.op("dve", lambda E, m=m, pl_=pl_: E.tensor_tensor(out=m["lg"][:], in0=pl_[:, 0:NE], in1=brb[:], op=ALU.add), r=[pl_, brb], w=[m["lg"]])
                        kb.op("dve", lambda E, m=m: E.max(out=m["m8"][:], in_=m["lg"][:]), r=[m["lg"]], w=[m["m8"]])
                        kb.op("dve", lambda E, m=m: E.tensor_scalar(out=m["mk"][:], in0=m["lg"][:], scalar1=m["m8"][:, 3:4], scalar2=None, op0=ALU.is_ge), r=[m["lg"], m["m8"]], w=[m["mk"]])
                        kb.op("dve", lambda E, m=m: E.tensor_scalar(out=m["nm"][:], in0=m["m8"][:, 0:1], scalar1=-1.0, scalar2=None, op0=ALU.mult), r=[m["m8"]], w=[m["nm"]])
                        kb.op("act", lambda E, m=m: E.activation(out=m["ex"][:], in_=m["lg"][:], func=AF.Exp, bias=m["nm"][:], scale=1.0), r=[m["lg"], m["nm"]], w=[m["ex"]])
                        kb.op("dve", lambda E, m=m: E.tensor_tensor(out=m["ex"][:], in0=m["ex"][:], in1=m["mk"][:], op=ALU.mult), r=[m["ex"], m["mk"]], w=[m["ex"]])
                        kb.op("dve", lambda E, m=m: E.tensor_reduce(out=m["ss"][:], in_=m["ex"][:], axis=AX.X, op=ALU.add), r=[m["ex"]], w=[m["ss"]])
                        kb.op("dve", lambda E, m=m: E.reciprocal(out=m["ss"][:], in_=m["ss"][:]), r=[m["ss"]], w=[m["ss"]])
                        kb.op("dve", lambda E, m=m, tile=tile: E.tensor_scalar(out=gates[:, tile, :], in0=m["ex"][:], scalar1=m["ss"][:, 0:1], scalar2=None, op0=ALU.mult), r=[m["ex"], m["ss"]], w=[gates])
                        pg_ = plg[tile % 2]
                        kb.op("pe", lambda E, pg_=pg_, tile=tile: E.transpose(out=pg_[0:NE, 0:128], in_=gates[:, tile, :], identity=C.ident_f[:]), r=[gates, C.ident_f], w=[pg_])
                        kb.op("act", lambda E, pg_=pg_, tile=tile: E.copy(out=gT[:, tile, :], in_=pg_[0:NE, 0:128]), r=[pg_], w=[gT])
                for tile in range(8):
                    for hh in range(2):
                        p = plg[(tile * 2 + hh) % 2]
                        kb.op("pe", lambda E, p=p, tile=tile, hh=hh: E.matmul(out=p[:], lhsT=gT[:, tile, :], rhs=bdn[:, hh * 512:(hh + 1) * 512], start=True, stop=True), r=[gT, bdn], w=[p])
                        evac(kb, hh, yacc[tile][:, hh * 512:(hh + 1) * 512], p[:], r=[p], w=[yacc[tile]])
                kb.barrier()
            with contextlib.ExitStack() as s2:
                actT = [kb.sb(s2, [128, 8, 512], BF16, "actT") for _ in range(2)]
                glu = [kb.sb(s2, [128, 512], F32, "glu") for _ in range(2)]
                sig = [kb.sb(s2, [128, 512], F32, "sig") for _ in range(2)]
                lin = [kb.sb(s2, [128, 512], F32, "lin") for _ in range(2)]
                pg = [kb.ps(s2, [128, 512], F32, "pg") for _ in range(2)]
                pl = [kb.ps(s2, [128, 512], F32, "pl") for _ in range(2)]
                py = [kb.ps(s2, [128, 512], F32, "py") for _ in range(2)]
                cnt = 0
                ycnt = 0
                pend_dn = []
                for e in range(NE):
                    b = ecnt % 2
                    ecnt += 1
                    for hk in range(2):
                        kb.dma("sp", Wg[b][:, hk * 4:(hk + 1) * 4, :], wg_d[e, hk * 512:(hk + 1) * 512, :].rearrange("(kc p) n -> p kc n", p=128), w=[Wg[b]])
                    kb.dma("sp", Wd[b][:], wd_d[e].rearrange("(kc p) n -> p kc n", p=128), w=[Wd[b]])
                    for t5 in range(2):
                        aT = actT[(e * 2 + t5) % 2]
                        for fc in range(8):
                            i = cnt % 2
                            cnt += 1
                            if fc == 2 and pend_dn:
                                pend_dn.pop(0)()
                            for kc in range(8):
                                kb.op("pe", lambda E, i=i, kc=kc, fc=fc, b=b, t5=t5: E.matmul(out=pg[i][:], lhsT=Wg[b][:, kc, fc * 128:(fc + 1) * 128], rhs=xT[:, kc, t5 * 512:(t5 + 1) * 512], start=(kc == 0), stop=(kc == 7)), r=[Wg[b], xT], w=[pg[i]])
                            for kc in range(8):
                                kb.op("pe", lambda E, i=i, kc=kc, fc=fc, b=b, t5=t5: E.matmul(out=pl[i][:], lhsT=Wg[b][:, kc, D + fc * 128:D + (fc + 1) * 128], rhs=xT[:, kc, t5 * 512:(t5 + 1) * 512], start=(kc == 0), stop=(kc == 7)), r=[Wg[b], xT], w=[pl[i]])
                            kb.op("dve", lambda E, i=i, fc=fc, e=e: E.tensor_scalar(out=glu[i][:], in0=pg[i][:], scalar1=bguT[:, fc, e:e + 1], scalar2=7.0, op0=ALU.add, op1=ALU.min), r=[pg[i], bguT], w=[glu[i]])
                            kb.op("act", lambda E, i=i: E.activation(out=sig[i][:], in_=glu[i][:], func=AF.Sigmoid, scale=1.702), r=[glu[i]], w=[sig[i]])
                            kb.op("dve", lambda E, i=i, fc=fc, e=e: E.tensor_scalar(out=lin[i][:], in0=pl[i][:], scalar1=bguT[:, 8 + fc, e:e + 1], scalar2=7.0, op0=ALU.add, op1=ALU.min), r=[pl[i], bguT], w=[lin[i]])
                            kb.op("dve", lambda E, i=i: E.tensor_scalar(out=lin[i][:], in0=lin[i][:], scalar1=-7.0, scalar2=1.0, op0=ALU.max, op1=ALU.add), r=[lin[i]], w=[lin[i]])
                            kb.op("dve", lambda E, i=i: E.tensor_tensor(out=lin[i][:], in0=lin[i][:], in1=glu[i][:], op=ALU.mult), r=[lin[i], glu[i]], w=[lin[i]])
                            kb.op("dve", lambda E, i=i, fc=fc, aT=aT: E.tensor_tensor(out=aT[:, fc, :], in0=lin[i][:], in1=sig[i][:], op=ALU.mult), r=[lin[i], sig[i]], w=[aT])
                        def dn_block(t5=t5, b=b, aT=aT, e=e):
                            nonlocal ycnt
                            for sub in range(4):
                                tile = t5 * 4 + sub
                                for hh in range(2):
                                    p = py[ycnt % 2]
                                    ycnt += 1
                                    for fc in range(8):
                                        kb.op("pe", lambda E, p=p, fc=fc, sub=sub, hh=hh, b=b, aT=aT: E.matmul(out=p[:], lhsT=aT[:, fc, sub * 128:(sub + 1) * 128], rhs=Wd[b][:, fc, hh * 512:(hh + 1) * 512], start=(fc == 0), stop=(fc == 7)), r=[aT, Wd[b]], w=[p])
                                    kb.op("dve", lambda E, p=p, tile=tile, hh=hh, e=e: E.scalar_tensor_tensor(out=yacc[tile][:, hh * 512:(hh + 1) * 512], in0=p[:], scalar=gates[:, tile, e:e + 1], in1=yacc[tile][:, hh * 512:(hh + 1) * 512], op0=ALU.mult, op1=ALU.add), r=[p, gates, yacc[tile]], w=[yacc[tile]])
                        pend_dn.append(dn_block)
                while pend_dn:
                    pend_dn.pop(0)()
                kb.barrier()
            with contextlib.ExitStack() as s3:
                xr = [kb.sb(s3, [128, D], F32, "xr") for _ in range(2)]
                zo = [kb.sb(s3, [128, D], F32, "zo") for _ in range(2)]
                for tile in range(8):
                    t0 = T0 + tile * 128
                    x_ = xr[tile % 2]
                    o_ = zo[tile % 2]
                    kb.dma("pool", x_[:], x1_d[t0:t0 + 128, :], w=[x_])
                    kb.op("dve", lambda E, x_=x_, tile=tile: E.scalar_tensor_tensor(out=yacc[tile][:], in0=x_[:], scalar=ALPHA, in1=yacc[tile][:], op0=ALU.mult, op1=ALU.add), r=[x_, yacc[tile]], w=[yacc[tile]])
                    ln_tile(kb, L, yacc[tile], yacc[tile][:], o_, o_[:])
                    kb.dma("sp", xout_d[t0:t0 + 128, :], o_[:], r=[o_])
                kb.barrier()


GLA_H, GLA_DK, GLA_DV = 4, 96, 192


def gla_consts_np():
    s = np.arange(128)[:, None]
    c = np.arange(128)[None, :]
    same = (s // 64) == (c // 64)
    tri_incl = np.where(same & (s <= c), -1.0 / 16.0, 0.0)
    tri_up = np.where(same & (s > c), -1.0 / 16.0, 0.0)
    m01 = np.where(same & (s <= c), 1.0, 0.0)
    return np.stack([tri_incl, tri_up, m01]).astype(np.float32)


def stage_gla(kb, C, hF_d, hT_d, wa2_d, ba_d, bog_d, nw_d, gc_d, mix_d):
    H, DK, DV = GLA_H, GLA_DK, GLA_DV
    with contextlib.ExitStack() as st:
        gc = kb.sb(st, [128, 3, 128], F32, "gc")
        kb.dma("sp", gc[:], gc_d.rearrange("a s c -> s a c"), w=[gc])
        wa2 = kb.sb(st, [16, 384], F32, "wa2")
        kb.dma("sp", wa2[:], wa2_d, w=[wa2])
        bab = kb.sb(st, [128, 384], F32, "bab")
        kb.dma("sp", bab[:], ba_d.partition_broadcast(128), w=[bab])
        bogb = kb.sb(st, [128, TOKW], F32, "bogb")
        kb.dma("sp", bogb[:], bog_d.partition_broadcast(128), w=[bogb])
        nwb = kb.sb(st, [128, DV], F32, "nwb")
        kb.dma("sp", nwb[:], nw_d.partition_broadcast(128), w=[nwb])
        epsb = kb.sb(st, [128, 1], F32, "eps")
        kb.op("dve", lambda E: E.memset(epsb[:], LN_EPS), w=[epsb])
        Sf = [kb.sb(st, [DK, DV], F32, "Sf") for _ in range(H)]
        Sb = [[kb.sb(st, [DK, DV], BF16, "Sb") for _ in range(2)] for _ in range(H)]
        for h in range(H):
            kb.op("dve", lambda E, h=h: E.memset(Sf[h][:], 0.0), w=[Sf[h]])
            kb.op("dve", lambda E, h=h: E.memset(Sb[h][1][:], 0.0), w=[Sb[h][1]])
        NB = 2
        tm = [kb.sb(st, [128, 1936], F32, "tm") for _ in range(NB)]
        qkT = [kb.sb(st, [DK, 2, H, 128], F32, "qkT") for _ in range(NB)]
        alrT = [kb.sb(st, [16, 128], F32, "alrT") for _ in range(NB)]
        G = [kb.sb(st, [128, 384], F32, "G") for _ in range(NB)]
        vb = [kb.sb(st, [128, TOKW], BF16, "vb") for _ in range(NB)]
        sg = [kb.sb(st, [128, TOKW], F32, "sg") for _ in range(NB)]
        kdec = [kb.sb(st, [128, 384], BF16, "kdec") for _ in range(NB)]
        edec = [kb.sb(st, [128, 384], F32, "edec") for _ in range(NB)]
        yt = [kb.sb(st, [128, TOKW], F32, "yt") for _ in range(NB)]
        eb = [kb.sb(st, [DK, 128], F32, "eb") for _ in range(4)]
        enb = [kb.sb(st, [DK, 128], F32, "enb") for _ in range(4)]
        qd = [kb.sb(st, [DK, 128], BF16, "qd") for _ in range(4)]
        kd = [kb.sb(st, [DK, 128], BF16, "kd") for _ in range(4)]
        qd0 = [kb.sb(st, [DK, 128], BF16, "qd0") for _ in range(4)]
        qd1 = [kb.sb(st, [DK, 128], BF16, "qd1") for _ in range(4)]
        for i in range(4):
            kb.op("dve", lambda E, i=i: E.memset(qd0[i][:], 0.0), w=[qd0[i]])
            kb.op("dve", lambda E, i=i: E.memset(qd1[i][:], 0.0), w=[qd1[i]])
        AT = [kb.sb(st, [128, 128], BF16, "AT") for _ in range(2)]
        ebl = [kb.sb(st, [DK, 2], F32, "ebl") for _ in range(4)]
        ssq = [kb.sb(st, [128, 1], F32, "ssq") for _ in range(2)]
        rstd = [kb.sb(st, [128, 1], F32, "rstd") for _ in range(2)]
        junk = kb.sb(st, [128, DV], F32, "junk")
        otmp = [kb.sb(st, [128, DV], F32, "otmp") for _ in range(2)]
        pz = kb.ps(st, [128, 512], F32, "pz")
        pdec = kb.ps(st, [128, 512], F32, "pdec")
        pbc = [kb.ps(st, [128, 512], F32, "pbc") for _ in range(2)]
        pA = kb.ps(st, [128, 512], F32, "pA")
        po = [kb.ps(st, [128, 512], F32, "po") for _ in range(2)]
        pds = kb.ps(st, [128, 512], F32, "pds")
        hc = 0
        for tt in range(S // 128):
            b = tt % NB
            t0 = tt * 128
            kb.dma("sp", tm[b][:], hT_d[t0:t0 + 128, 384:2320], w=[tm[b]])
            kb.dma("pool", qkT[b][:], hF_d[0:768, t0:t0 + 128].rearrange("(a h d) t -> d a h t", a=2, h=H), w=[qkT[b]])
            kb.dma("pool", alrT[b][:], hF_d[1536:1552, t0:t0 + 128], w=[alrT[b]])
            k_tm = tm[b][:, 0:384]
            v_tm = tm[b][:, 384:1152]
            og_tm = tm[b][:, 1168:1936]
            kb.op("pe", lambda E, b=b: E.matmul(out=pz[:, 0:384], lhsT=alrT[b][:], rhs=wa2[:], start=True, stop=True), r=[alrT[b], wa2], w=[pz])
            kb.op("dve", lambda E, b=b: E.tensor_tensor(out=G[b][:], in0=pz[:, 0:384], in1=bab[:], op=ALU.add), r=[pz, bab], w=[G[b]])
            kb.op("act", lambda E, b=b: E.activation(out=G[b][:], in_=G[b][:], func=AF.Exp, scale=-1.0), r=[G[b]], w=[G[b]])
            kb.op("act", lambda E, b=b: E.activation(out=G[b][:], in_=G[b][:], func=AF.Ln, bias=1.0, scale=1.0), r=[G[b]], w=[G[b]])
            kb.op("act", lambda E, b=b: E.copy(out=vb[b][:], in_=tm[b][:, 384:1152]), r=[tm[b]], w=[vb[b]])
            kb.op("dve", lambda E, b=b: E.tensor_tensor(out=sg[b][:], in0=tm[b][:, 1168:1936], in1=bogb[:], op=ALU.add), r=[tm[b], bogb], w=[sg[b]])
            kb.op("act", lambda E, b=b: E.activation(out=sg[b][:], in_=sg[b][:], func=AF.Silu), r=[sg[b]], w=[sg[b]])
            kb.op("pe", lambda E, b=b: E.matmul(out=pdec[:, 0:384], lhsT=gc[:, 1, :], rhs=G[b][:], start=True, stop=True), r=[gc, G[b]], w=[pdec])
            kb.op("act", lambda E, b=b: E.activation(out=edec[b][:], in_=pdec[:, 0:384], func=AF.Exp), r=[pdec], w=[edec[b]])
            kb.op("dve", lambda E, b=b: E.tensor_tensor(out=kdec[b][:], in0=tm[b][:, 0:384], in1=edec[b][:], op=ALU.mult), r=[tm[b], edec[b]], w=[kdec[b]])
            for h in range(H):
                i = hc % 4
                i2 = hc % 2
                hc += 1
                pb_ = pbc[i2]
                kb.op("pe", lambda E, b=b, h=h, pb_=pb_: E.matmul(out=pb_[0:DK, 0:128], lhsT=G[b][:, h * DK:(h + 1) * DK], rhs=gc[:, 0, :], start=True, stop=True), r=[G[b], gc], w=[pb_])
                kb.op("act", lambda E, i=i, pb_=pb_: E.activation(out=eb[i][:], in_=pb_[0:DK, 0:128], func=AF.Exp), r=[pb_], w=[eb[i]])
                kb.op("act", lambda E, i=i, pb_=pb_: E.activation(out=enb[i][:], in_=pb_[0:DK, 0:128], func=AF.Exp, scale=-1.0), r=[pb_], w=[enb[i]])
                kb.op("dve", lambda E, i=i, b=b, h=h: E.scalar_tensor_tensor(out=qd[i][:], in0=qkT[b][:, 0, h, :], scalar=float(DK) ** -0.5, in1=eb[i][:], op0=ALU.mult, op1=ALU.mult), r=[qkT[b], eb[i]], w=[qd[i]])
                kb.op("dve", lambda E, i=i, b=b, h=h: E.tensor_tensor(out=kd[i][:], in0=qkT[b][:, 1, h, :], in1=enb[i][:], op=ALU.mult), r=[qkT[b], enb[i]], w=[kd[i]])
                kb.op("act", lambda E, i=i: E.copy(out=qd0[i][:, 0:64], in_=qd[i][:, 0:64]), r=[qd[i]], w=[qd0[i]])
                kb.op("act", lambda E, i=i: E.copy(out=qd1[i][:, 64:128], in_=qd[i][:, 64:128]), r=[qd[i]], w=[qd1[i]])
                kb.op("act", lambda E, i=i: E.copy(out=ebl[i][:, 0:1], in_=eb[i][:, 63:64]), r=[eb[i]], w=[ebl[i]])
                kb.op("act", lambda E, i=i: E.copy(out=ebl[i][:, 1:2], in_=eb[i][:, 127:128]), r=[eb[i]], w=[ebl[i]])
                kb.op("pe", lambda E, i=i: E.matmul(out=pA[:, 0:128], lhsT=kd[i][:], rhs=qd[i][:], start=True, stop=True), r=[kd[i], qd[i]], w=[pA])
                at = AT[i2]
                kb.op("dve", lambda E, at=at: E.tensor_tensor(out=at[:], in0=pA[:, 0:128], in1=gc[:, 2, :], op=ALU.mult), r=[pA, gc], w=[at])
                o = po[i2]
                sb_in = Sb[h][1]
                sb_mid = Sb[h][0]
                kb.op("pe", lambda E, o=o, at=at, b=b, h=h: E.matmul(out=o[:, 0:DV], lhsT=at[:], rhs=vb[b][:, h * DV:(h + 1) * DV], start=True, stop=False), r=[at, vb[b]], w=[o])
                kb.op("pe", lambda E, o=o, i=i, sb_in=sb_in: E.matmul(out=o[:, 0:DV], lhsT=qd0[i][:], rhs=sb_in[:], start=False, stop=False), r=[qd0[i], sb_in], w=[o])
                for j in range(2):
                    kb.op("pe", lambda E, b=b, h=h, j=j: E.matmul(out=pds[0:DK, 0:DV], lhsT=kdec[b][j * 64:(j + 1) * 64, h * DK:(h + 1) * DK], rhs=vb[b][j * 64:(j + 1) * 64, h * DV:(h + 1) * DV], start=True, stop=True), r=[kdec[b], vb[b]], w=[pds])
                    kb.op("dve", lambda E, h=h, i=i, j=j: E.scalar_tensor_tensor(out=Sf[h][:], in0=Sf[h][:], scalar=ebl[i][:, j:j + 1], in1=pds[0:DK, 0:DV], op0=ALU.mult, op1=ALU.add), r=[Sf[h], ebl[i], pds], w=[Sf[h]])
                    dst = sb_mid if j == 0 else sb_in
                    kb.op("act", lambda E, h=h, dst=dst: E.copy(out=dst[:], in_=Sf[h][:]), r=[Sf[h]], w=[dst])
                    if j == 0:
                        kb.op("pe", lambda E, o=o, i=i, sb_mid=sb_mid: E.matmul(out=o[:, 0:DV], lhsT=qd1[i][:], rhs=sb_mid[:], start=False, stop=True), r=[qd1[i], sb_mid], w=[o])
                sq = ssq[i2]
                rs = rstd[i2]
                ot = otmp[i2]
                kb.op("act", lambda E, o=o, sq=sq: E.activation(out=junk[:], in_=o[:, 0:DV], func=AF.Square, accum_out=sq[:]), r=[o], w=[junk, sq])
                kb.op("act", lambda E, sq=sq, rs=rs: E.activation(out=rs[:], in_=sq[:], func=AF.Sqrt, bias=epsb[:], scale=1.0 / DV), r=[sq, epsb], w=[rs])
                kb.op("dve", lambda E, rs=rs: E.reciprocal(out=rs[:], in_=rs[:]), r=[rs], w=[rs])
                kb.op("dve", lambda E, o=o, rs=rs, ot=ot: E.scalar_tensor_tensor(out=ot[:], in0=o[:, 0:DV], scalar=rs[:, 0:1], in1=nwb[:], op0=ALU.mult, op1=ALU.mult), r=[o, rs, nwb], w=[ot])
                kb.op("dve", lambda E, ot=ot, b=b, h=h: E.tensor_tensor(out=yt[b][:, h * DV:(h + 1) * DV], in0=ot[:], in1=sg[b][:, h * DV:(h + 1) * DV], op=ALU.mult), r=[ot, sg[b]], w=[yt[b]])
            kb.dma("sp", mix_d[t0:t0 + 128, 0:TOKW], yt[b][:], r=[yt[b]])
        kb.barrier()


def TT(kb, out, in0, in1, op, r, w, eng="dve"):
    return kb.op(eng, lambda E: E.tensor_tensor(out=out, in0=in0, in1=in1, op=op), r=r, w=w)


def TS(kb, out, in0, s1, s2, op0, op1, r, w, eng="dve"):
    if op1 is None:
        return kb.op(eng, lambda E: E.tensor_scalar(out=out, in0=in0, scalar1=s1, scalar2=None, op0=op0), r=r, w=w)
    return kb.op(eng, lambda E: E.tensor_scalar(out=out, in0=in0, scalar1=s1, scalar2=s2, op0=op0, op1=op1), r=r, w=w)


def STT(kb, out, in0, scalar, in1, op0, op1, r, w):
    return kb.op("dve", lambda E: E.scalar_tensor_tensor(out=out, in0=in0, scalar=scalar, in1=in1, op0=op0, op1=op1), r=r, w=w)


def ACT(kb, out, in_, func, r, w, bias=None, scale=None, accum_out=None):
    kw = {}
    if bias is not None:
        kw["bias"] = bias
    if scale is not None:
        kw["scale"] = scale
    if accum_out is not None:
        kw["accum_out"] = accum_out
    return kb.op("act", lambda E: E.activation(out=out, in_=in_, func=func, **kw), r=r, w=w)


def MM(kb, out, lhsT, rhs, start, stop, r, w):
    return kb.op("pe", lambda E: E.matmul(out=out, lhsT=lhsT, rhs=rhs, start=start, stop=stop), r=r, w=w)


def TR(kb, out, in_, ident, r, w):
    return kb.op("pe", lambda E: E.transpose(out=out, in_=in_, identity=ident), r=r, w=w)


def CP(kb, out, in_, r, w, eng="dve"):
    if eng == "act":
        return kb.op("act", lambda E: E.copy(out=out, in_=in_), r=r, w=w)
    return kb.op("dve", lambda E: E.tensor_copy(out=out, in_=in_), r=r, w=w)


def RED(kb, out, in_, op, r, w):
    return kb.op("dve", lambda E: E.tensor_reduce(out=out, in_=in_, axis=AX.X, op=op), r=r, w=w)


def MSET(kb, ap, val, w):
    return kb.op("dve", lambda E: E.memset(ap, val), w=w)


def bcast_row(kb, st, d_ap, n, name):
    t = kb.sb(st, [128, n], F32, name)
    kb.dma("sp", t[:], d_ap.partition_broadcast(128), w=[t])
    return t


RW_H = 12


def perm_view(ap):
    return ap.rearrange("t (hp j k) -> t hp j k", hp=2, j=6)


def nat_view(ap):
    return ap.rearrange("t (j hp k) -> t hp j k", hp=2, j=6)


def store_perm(kb, q, dram_rows, tile_ap, r):
    for hp in range(2):
        kb.dma(q, dram_rows[:, hp * 384:(hp + 1) * 384].rearrange("t (j k) -> t j k", k=64), nat_view(tile_ap)[:, hp], r=r)


def load_perm(kb, q, tile, dram_rows):
    for hp in range(2):
        kb.dma(q, nat_view(tile[:])[:, hp], dram_rows[:, hp * 384:(hp + 1) * 384].rearrange("t (j k) -> t j k", k=64), w=[(tile, hp)])


def stage_rwkv(kb, C, hT_d, P, mix_d, scr):
    Wd, Kd, Ad, Bd, Rd = scr["W"], scr["K"], scr["A"], scr["B"], scr["R"]
    Vd, Gd, VPd, Od = scr["V"], scr["G"], scr["VP"], scr["O"]
    with contextlib.ExitStack() as st:
        mub = bcast_row(kb, st, P["mu"], 2560, "mub")
        w0b = bcast_row(kb, st, P["w0"], TOKW, "w0b")
        a0b = bcast_row(kb, st, P["a0"], TOKW, "a0b")
        kkb = bcast_row(kb, st, P["k_k"], TOKW, "kkb")
        kab = bcast_row(kb, st, P["k_a"], TOKW, "kab")
        w2b = kb.sb(st, [64, TOKW], BF16, "w2b")
        a2b = kb.sb(st, [64, TOKW], BF16, "a2b")
        g2b = kb.sb(st, [128, TOKW], BF16, "g2b")
        kb.dma("pool", w2b[:], P["w2"], w=[w2b])
        kb.dma("pool", a2b[:], P["a2"], w=[a2b])
        kb.dma("pool", g2b[:], P["g2"], w=[g2b])
        NB = 2
        hc = [kb.sb(st, [128, 2560], F32, "hc") for _ in range(NB)]
        hp_ = [kb.sb(st, [128, 2560], F32, "hp") for _ in range(NB)]
        twT = [kb.sb(st, [64, 128], BF16, "twT") for _ in range(NB)]
        alT = [kb.sb(st, [64, 128], BF16, "alT") for _ in range(NB)]
        sgT = [kb.sb(st, [128, 128], BF16, "sgT") for _ in range(NB)]
        Wt = [kb.sb(st, [128, TOKW], F32, "Wt") for _ in range(NB)]
        At = [kb.sb(st, [128, TOKW], F32, "At") for _ in range(NB)]
        Bt = [kb.sb(st, [128, TOKW], F32, "Bt") for _ in range(NB)]
        Kt = [kb.sb(st, [128, TOKW], F32, "Kt") for _ in range(NB)]
        Gt = [kb.sb(st, [128, TOKW], F32, "Gt") for _ in range(NB)]
        aa = [kb.sb(st, [128, TOKW], F32, "aa") for _ in range(NB)]
        sq = [kb.sb(st, [128, TOKW], F32, "sq") for _ in range(NB)]
        ss = [kb.sb(st, [128, RW_H], F32, "ss") for _ in range(NB)]
        vps = [kb.sb(st, [128, 6, 128], F32, "vps") for _ in range(NB)]
        ptr = kb.ps(st, [128, 512], F32, "ptr")
        pw = [kb.ps(st, [128, 512], F32, "pw") for _ in range(2)]
        pa = [kb.ps(st, [128, 512], F32, "pa") for _ in range(2)]
        pgp = [kb.ps(st, [128, 512], F32, "pgp") for _ in range(2)]
        pvp = kb.ps(st, [128, 512], F32, "pvp")
        for b in range(NB):
            MSET(kb, hp_[b][0:1, :], 0.0, w=[hp_[b]])
        for tt in range(S // 128):
            b = tt % NB
            t0 = tt * 128
            H_, HP = hc[b], hp_[b]
            kb.dma("sp", H_[:], hT_d[t0:t0 + 128, 0:2560], w=[H_])
            if tt == 0:
                kb.dma("pool", HP[1:128, :], hT_d[0:127, 0:2560], w=[HP])
            else:
                kb.dma("pool", HP[:], hT_d[t0 - 1:t0 + 127, 0:2560], w=[HP])
            TT(kb, HP[:], HP[:], H_[:], ALU.subtract, r=[HP, H_], w=[HP])
            TT(kb, HP[:], HP[:], mub[:], ALU.mult, r=[HP, mub], w=[HP])
            TT(kb, H_[:], H_[:], HP[:], ALU.add, r=[H_, HP], w=[H_])
            r_ = H_[:, 0:768]
            k_ = H_[:, 832:1600]
            v_ = H_[:, 1600:2368]
            TR(kb, ptr[0:64, 0:128], H_[:, 768:832], C.ident_f[:], r=[H_, C.ident_f], w=[ptr])
            TR(kb, ptr[0:64, 128:256], H_[:, 2368:2432], C.ident_f[:], r=[H_, C.ident_f], w=[ptr])
            TR(kb, ptr[:, 256:384], H_[:, 2432:2560], C.ident_f[:], r=[H_, C.ident_f], w=[ptr])
            ACT(kb, twT[b][:], ptr[0:64, 0:128], AF.Tanh, r=[ptr], w=[twT[b]])
            CP(kb, alT[b][:], ptr[0:64, 128:256], r=[ptr], w=[alT[b]])
            ACT(kb, sgT[b][:], ptr[:, 256:384], AF.Sigmoid, r=[ptr], w=[sgT[b]])
            for (lh, rh, pp) in ((twT[b], w2b, pw), (alT[b], a2b, pa), (sgT[b], g2b, pgp)):
                MM(kb, pp[0][:, 0:512], lh[:], rh[:, 0:512], True, True, r=[lh, rh], w=[pp[0]])
                MM(kb, pp[1][:, 0:256], lh[:], rh[:, 512:768], True, True, r=[lh, rh], w=[pp[1]])
            W_, A_, B_, K_, G_, a_, q_ = Wt[b], At[b], Bt[b], Kt[b], Gt[b], aa[b], sq[b]
            for (c0, c1, hh) in ((0, 512, 0), (512, 768, 1)):
                n = c1 - c0
                TT(kb, W_[:, c0:c1], pw[hh][:, 0:n], w0b[:, c0:c1], ALU.add, r=[pw[hh], w0b], w=[W_])
                TT(kb, a_[:, c0:c1], pa[hh][:, 0:n], a0b[:, c0:c1], ALU.add, r=[pa[hh], a0b], w=[a_])
                CP(kb, G_[:, c0:c1], pgp[hh][:, 0:n], r=[pgp[hh]], w=[G_], eng="act")
            ACT(kb, W_[:], W_[:], AF.Sigmoid, r=[W_], w=[W_])
            ACT(kb, W_[:], W_[:], AF.Exp, r=[W_], w=[W_], scale=-float(np.exp(-0.5)))
            ACT(kb, a_[:], a_[:], AF.Sigmoid, r=[a_], w=[a_])
            TT(kb, A_[:], k_, kkb[:], ALU.mult, r=[H_, kkb], w=[A_])
            TT(kb, q_[:], A_[:], A_[:], ALU.mult, r=[A_], w=[q_])
            RED(kb, ss[b][:], q_[:].rearrange("p (h k) -> p h k", k=64), ALU.add, r=[q_], w=[ss[b]])
            ACT(kb, ss[b][:], ss[b][:], AF.Sqrt, r=[ss[b]], w=[ss[b]])
            TS(kb, ss[b][:], ss[b][:], 1e-12, None, ALU.max, None, r=[ss[b]], w=[ss[b]])
            kb.op("dve", lambda E, s_=ss[b]: E.reciprocal(out=s_[:], in_=s_[:]), r=[ss[b]], w=[ss[b]])
            TT(kb, A_[:].rearrange("p (h k) -> p h k", k=64), A_[:].rearrange("p (h k) -> p h k", k=64), ss[b][:, :].unsqueeze(2).broadcast_to([128, RW_H, 64]), ALU.mult, r=[A_, ss[b]], w=[A_])
            TT(kb, B_[:], A_[:], a_[:], ALU.mult, r=[A_, a_], w=[B_])
            TS(kb, A_[:], A_[:], -1.0, None, ALU.mult, None, r=[A_], w=[A_])
            STT(kb, q_[:], a_[:], -1.0, kab[:], ALU.add, ALU.mult, r=[a_, kab], w=[q_])
            STT(kb, K_[:], q_[:], 1.0, k_, ALU.add, ALU.mult, r=[q_, H_], w=[K_])
            for j in range(6):
                TR(kb, pvp[:, 0:128] if j % 2 == 0 else pvp[:, 128:256], H_[:, 1600 + j * 128:1600 + (j + 1) * 128], C.ident_f[:], r=[H_, C.ident_f], w=[pvp])
                CP(kb, vps[b][:, j, :], pvp[:, 0:128] if j % 2 == 0 else pvp[:, 128:256], r=[pvp], w=[vps[b]], eng="act" if j % 2 else "dve")
            rows = slice(t0, t0 + 128)
            store_perm(kb, "sp", Wd[rows, :], W_[:], r=[W_])
            store_perm(kb, "pool", Kd[rows, :], K_[:], r=[K_])
            store_perm(kb, "sp", Ad[rows, :], A_[:], r=[A_])
            store_perm(kb, "pool", Bd[rows, :], B_[:], r=[B_])
            store_perm(kb, "sp", Rd[rows, :], H_[:, 0:768], r=[H_])
            kb.dma("pool", Vd[rows, :], H_[:, 1600:2368], r=[H_])
            kb.dma("sp", Gd[rows, :], G_[:], r=[G_])
            kb.dma("pool", VPd[:, :, t0:t0 + 128], vps[b][:], r=[vps[b]])
        kb.barrier()

    with contextlib.ExitStack() as st:
        T = 8
        Sst = kb.sb(st, [128, 6, 64], F32, "Sst")
        SK = [("S", j) for j in range(6)]
        MSET(kb, Sst[:], 0.0, w=SK)
        BC = [kb.sb(st, [128, T, 5, 384], F32, "BC") for _ in range(2)]
        vP = [kb.sb(st, [128, 6, 512], F32, "vP") for _ in range(2)]
        oP = [kb.sb(st, [128, 6, 512], F32, "oP") for _ in range(2)]
        tmp = [kb.sb(st, [128, 6, 64], F32, "tmp") for _ in range(2)]
        sa = [kb.sb(st, [128, 6], F32, "sa") for _ in range(2)]
        otile = [kb.sb(st, [128, TOKW], F32, "otile") for _ in range(2)]
        pto = [kb.ps(st, [128, 512], F32, "pto") for _ in range(2)]
        srcs = (Ad, Wd, Bd, Kd, Rd)
        qs = ("sp", "act", "pool")
        for blk in range(S // T):
            bb = blk % 2
            t0 = blk * T
            g = (t0 // 512) % 2
            if t0 % 512 == 0:
                kb.dma("sp", vP[g][:], VPd[:, :, t0:t0 + 512], w=[vP[g]])
            for vi in range(5):
                for hp in range(2):
                    kb.dma(qs[(vi * 2 + hp) % 3], BC[bb][hp * 64:(hp + 1) * 64, :, vi, :], srcs[vi][t0:t0 + T, hp * 384:(hp + 1) * 384].partition_broadcast(64), w=[("BC", bb, vi, hp)])
            for tl in range(T):
                tg = (t0 + tl) % 512
                ti = tl % 2

                def bc(vi, bb=bb, tl=tl):
                    return BC[bb][:, tl, vi, :].rearrange("p (j k) -> p j k", k=64)

                def bk(vi, bb=bb):
                    return [("BC", bb, vi, 0), ("BC", bb, vi, 1)]

                TT(kb, tmp[ti][:], Sst[:], bc(0), ALU.mult, r=SK + bk(0), w=[tmp[ti]])
                RED(kb, sa[ti][:], tmp[ti][:], ALU.add, r=[tmp[ti]], w=[sa[ti]])
                TT(kb, Sst[:], Sst[:], bc(1), ALU.mult, r=SK + bk(1), w=SK)
                Bv, Kv = bc(2), bc(3)
                for j in range(6):
                    STT(kb, Sst[:, j, :], Bv[:, j, :], sa[ti][:, j:j + 1], Sst[:, j, :], ALU.mult, ALU.add, r=[sa[ti], SK[j]] + bk(2), w=[SK[j]])
                for j in range(6):
                    STT(kb, Sst[:, j, :], Kv[:, j, :], vP[g][:, j, tg:tg + 1], Sst[:, j, :], ALU.mult, ALU.add, r=[vP[g], SK[j]] + bk(3), w=[SK[j]])
                TT(kb, tmp[ti][:], Sst[:], bc(4), ALU.mult, r=SK + bk(4), w=[tmp[ti]])
                RED(kb, oP[g][:, :, tg], tmp[ti][:], ALU.add, r=[tmp[ti]], w=[oP[g]])
            if (t0 + T) % 512 == 0:
                G0 = t0 + T - 512
                for sub in range(4):
                    ot = otile[sub % 2]
                    for j in range(6):
                        p = pto[j % 2]
                        TR(kb, p[:, 0:128], oP[g][:, j, sub * 128:(sub + 1) * 128], C.ident_f[:], r=[oP[g], C.ident_f], w=[p])
                        CP(kb, ot[:, j * 128:(j + 1) * 128], p[:, 0:128], r=[p], w=[ot], eng="act")
                    kb.dma("sp", Od[G0 + sub * 128:G0 + (sub + 1) * 128, :], ot[:], r=[ot])
        kb.barrier()
    with contextlib.ExitStack() as st:
        lnw = bcast_row(kb, st, P["ln_w"], TOKW, "lnw")
        lnb = bcast_row(kb, st, P["ln_b"], TOKW, "lnb")
        rkb = bcast_row(kb, st, P["r_k"], TOKW, "rkb")
        NB = 2
        o_ = [kb.sb(st, [128, TOKW], F32, "o") for _ in range(NB)]
        r_ = [kb.sb(st, [128, TOKW], F32, "r") for _ in range(NB)]
        k_ = [kb.sb(st, [128, TOKW], F32, "k") for _ in range(NB)]
        v_ = [kb.sb(st, [128, TOKW], F32, "v") for _ in range(NB)]
        g_ = [kb.sb(st, [128, TOKW], F32, "g") for _ in range(NB)]
        q_ = [kb.sb(st, [128, TOKW], F32, "q") for _ in range(NB)]
        s1 = [kb.sb(st, [128, RW_H], F32, "s1") for _ in range(NB)]
        s2 = [kb.sb(st, [128, RW_H], F32, "s2") for _ in range(NB)]
        s3 = [kb.sb(st, [128, RW_H], F32, "s3") for _ in range(NB)]

        def h3(ap):
            return ap.rearrange("p (h k) -> p h k", k=64)

        def b3(ap):
            return ap.unsqueeze(2).broadcast_to([128, RW_H, 64])

        for tt in range(S // 128):
            b = tt % NB
            rows = slice(tt * 128, (tt + 1) * 128)
            O, R_, K_, V_, G_, Q_ = o_[b], r_[b], k_[b], v_[b], g_[b], q_[b]
            kb.dma("sp", O[:], Od[rows, :], w=[O])
            load_perm(kb, "pool", R_, Rd[rows, :])
            load_perm(kb, "pool", K_, Kd[rows, :])
            kb.dma("sp", V_[:], Vd[rows, :], w=[V_])
            kb.dma("sp", G_[:], Gd[rows, :], w=[G_])
            RED(kb, s1[b][:], h3(O[:]), ALU.add, r=[O], w=[s1[b]])
            TT(kb, Q_[:], O[:], O[:], ALU.mult, r=[O], w=[Q_])
            RED(kb, s2[b][:], h3(Q_[:]), ALU.add, r=[Q_], w=[s2[b]])
            TS(kb, s1[b][:], s1[b][:], 1.0 / 64, None, ALU.mult, None, r=[s1[b]], w=[s1[b]])
            STT(kb, s3[b][:], s1[b][:], -1.0, s1[b][:], ALU.mult, ALU.mult, r=[s1[b]], w=[s3[b]])
            STT(kb, s2[b][:], s2[b][:], 1.0 / 64, s3[b][:], ALU.mult, ALU.add, r=[s2[b], s3[b]], w=[s2[b]])
            TS(kb, s2[b][:], s2[b][:], 64e-5, None, ALU.add, None, r=[s2[b]], w=[s2[b]])
            ACT(kb, s2[b][:], s2[b][:], AF.Sqrt, r=[s2[b]], w=[s2[b]])
            kb.op("dve", lambda E, x=s2[b]: E.reciprocal(out=x[:], in_=x[:]), r=[s2[b]], w=[s2[b]])
            TT(kb, h3(O[:]), h3(O[:]), b3(s1[b][:, :]), ALU.subtract, r=[O, s1[b]], w=[O])
            TT(kb, h3(O[:]), h3(O[:]), b3(s2[b][:, :]), ALU.mult, r=[O, s2[b]], w=[O])
            TT(kb, O[:], O[:], lnw[:], ALU.mult, r=[O, lnw], w=[O])
            TT(kb, O[:], O[:], lnb[:], ALU.add, r=[O, lnb], w=[O])
            TT(kb, Q_[:], R_[:], K_[:], ALU.mult, r=[(R_, 0), (R_, 1), (K_, 0), (K_, 1), Q_], w=[Q_])
            TT(kb, Q_[:], Q_[:], rkb[:], ALU.mult, r=[Q_, rkb], w=[Q_])
            RED(kb, s3[b][:], h3(Q_[:]), ALU.add, r=[Q_], w=[s3[b]])
            TT(kb, h3(Q_[:]), h3(V_[:]), b3(s3[b][:, :]), ALU.mult, r=[V_, s3[b]], w=[Q_])
            TT(kb, O[:], O[:], Q_[:], ALU.add, r=[O, Q_], w=[O])
            TT(kb, O[:], O[:], G_[:], ALU.mult, r=[O, G_], w=[O])
            kb.dma("sp", mix_d[rows, 0:TOKW], O[:], r=[O])
        kb.barrier()


NSA_BIG = 1.0e4
NEG = -1.0e30


def _t5_bucket_np(dist):
    d = np.maximum(dist, 0)
    scaled = (np.log(np.maximum(d, 1).astype(np.float32) / np.float32(16)) / np.float32(np.log(128 / 16))).astype(np.float32)
    large = np.minimum(16 + (scaled * np.float32(16)).astype(np.int32), 31)
    return np.where(d < 16, d, large)


def nsa_tables_np(rel_table):
    kk = np.arange(128)[:, None]
    qq = np.arange(512)[None, :]
    out = np.empty((12, 18, 128, 512), np.float32)
    for o in range(5):
        dist = qq - kk - 128 * (o - 1)
        bk = _t5_bucket_np(dist)
        for h in range(12):
            out[h, o] = np.where(dist >= 0, rel_table[bk, h], NEG)
    for o in range(8):
        dist = qq - kk - 128 * (o - 4)
        bk = _t5_bucket_np(dist)
        for h in range(12):
            out[h, 5 + o] = np.where((dist >= 0) & (dist < 512), rel_table[bk, h], NEG)
    for o in range(5):
        dist = 512 * o + qq - 16 * kk - 31
        bk = _t5_bucket_np(dist)
        for h in range(12):
            out[h, 13 + o] = np.where(dist >= 0, rel_table[bk, h], NEG)
    return out.reshape(216, 128 * 512)


def nsa_static_np():
    q = np.arange(S)[:, None]
    m = np.arange(64)[None, :]
    cur = q // 64
    causal = m <= cur
    forced = (m == 0) | (m == cur) | (m == cur - 1)
    A = np.where(causal & ~forced, 1.0, 0.0).astype(np.float32)
    B = np.where(causal & forced, NSA_BIG, np.where(causal, 0.0, -1.0)).astype(np.float32)
    AB = np.stack([A, B]).reshape(2, 32, 128, 64).transpose(2, 0, 1, 3).copy()
    n = np.arange(256)[:, None]
    cs, ce = n * 16, n * 16 + 31
    c2s = ((cs < m * 64 + 64) & (ce >= m * 64) & (n < 255)).astype(np.float32)
    c2s = c2s.reshape(2, 128, 64).transpose(1, 0, 2).copy()
    E = np.zeros((64, 32, 128), np.float32)
    for j in range(32):
        for k in range(128):
            E[2 * j + k // 64, j, k] = NSA_BIG
    return AB, c2s, E


def stage_nsa(kb, C, hF_d, hT_d, P, tab_d, mix_d):
    QC, KCC, VCC, KSC, VSC, KWC, VWC, GLC = 0, 768, 1024, 1280, 1536, 1792, 2048, 2304
    with contextlib.ExitStack() as st:
        KC = [kb.sb(st, [64, 256], BF16, "KC") for _ in range(4)]
        VCx = [kb.sb(st, [128, 2, 129], BF16, "VCx") for _ in range(4)]
        c2s = kb.sb(st, [128, 2, 64], F32, "c2s")
        kb.dma("sp", c2s[:], P["c2s"], w=[c2s])
        CH = bcast_row(kb, st, P["rel31"], 12, "CH")
        gbb = bcast_row(kb, st, P["gate_b"], 36, "gbb")
        GATE = kb.sb(st, [128, 32, 36], F32, "GATE")
        kb.dma("sp", GATE[:], hT_d[:, GLC:GLC + 36].rearrange("(t p) c -> p t c", p=128), w=[GATE])
        TT(kb, GATE[:], GATE[:], gbb[:, :].unsqueeze(1).broadcast_to([128, 32, 36]), ALU.add, r=[GATE, gbb], w=[GATE])
        ACT(kb, GATE[:], GATE[:], AF.Sigmoid, r=[GATE], w=[GATE])
        import os as _os
        _stop = int(_os.environ.get("NSA_STOP", "99"))
        if _stop == -1:
            kb.barrier()
            return
        with contextlib.ExitStack() as s0:
            w1b = [kb.sb(s0, [64, 32, 256], BF16, "w1b") for _ in range(2)]
            w2b = [kb.sb(s0, [128, 2, 64], BF16, "w2b") for _ in range(2)]
            peT = [kb.sb(s0, [64, 32, 2], BF16, "peT") for _ in range(2)]
            cv = [kb.sb(s0, [128, 2], F32, "cv") for _ in range(2)]
            praw = kb.sb(s0, [32, 2, 64], F32, "praw")
            kb.dma("sp", praw[:], P["pe"].rearrange("a l d -> l a d"), w=[praw])
            pp = kb.ps(s0, [128, 512], F32, "pp")
            ph = [kb.ps(s0, [128, 512], F32, "ph") for _ in range(2)]
            pk = kb.ps(s0, [128, 512], F32, "pk")
            pv = [kb.ps(s0, [128, 512], F32, "pv") for _ in range(2)]
            pcv = [pp, ph[0], ph[1], pk]
            for a in range(2):
                kb.dma("pool", w1b[a][:], P["w1"][a].rearrange("(l d) j -> d l j", d=64), w=[w1b[a]])
                kb.dma("pool", w2b[a][:], P["w2"][a].rearrange("(c j) d -> j c d", j=128), w=[w2b[a]])
                TR(kb, pp[0:64, a * 32:(a + 1) * 32], praw[:, a, :], C.ident_f[0:32, 0:32], r=[praw, C.ident_f], w=[pp])
                CP(kb, peT[a][:, :, 0], pp[0:64, a * 32:(a + 1) * 32], r=[pp], w=[peT[a]])
                CP(kb, peT[a][:, :, 1], pp[0:64, a * 32:(a + 1) * 32], r=[pp], w=[peT[a]])
            _c0 = int(_os.environ.get("NSA_C0", "99"))
            if _c0 == 1:
                kb.barrier()
                return
            for a in range(2):
                for jc in range(2):
                    for l in range(32):
                        MM(kb, pcv[a * 2 + jc][:, 0:2], w1b[a][:, l, jc * 128:(jc + 1) * 128], peT[a][:, l, :], l == 0, l == 31, r=[w1b[a], peT[a]], w=[pcv[a * 2 + jc]])
                for jc in range(2):
                    CP(kb, cv[a][:, jc:jc + 1], pcv[a * 2 + jc][:, 0:1], r=[pcv[a * 2 + jc]], w=[cv[a]])
            if _c0 == 2:
                kb.barrier()
                return
            kvT = [kb.sb(s0, [64, S], BF16, "kvT") for _ in range(2)]
            xg = [kb.sb(s0, [128, 256], F32, "xg") for _ in range(2)]
            x2 = [kb.sb(s0, [128, 256], F32, "x2") for _ in range(2)]
            gT = [kb.sb(s0, [128, 2, 256], BF16, "gT") for _ in range(2)]
            for a in range(2):
                MSET(kb, VCx[0][:], 0.0, w=[VCx[0]]) if a == 0 else None
            for g in range(1, 4):
                MSET(kb, VCx[g][:], 0.0, w=[VCx[g]])
            i = 0
            for g in range(4):
                for a in range(2):
                    src = kvT[i % 2]
                    G_ = gT[i % 2]
                    i += 1
                    c0 = (KCC if a == 0 else VCC) + g * 64
                    kb.dma("pool", src[:], hF_d[c0:c0 + 64, :], w=[src])
                    if a == 0:
                        MSET(kb, G_[:, :, 255:256], 0.0, w=[("gpad", id(G_))])
                    for jc in range(2):
                        p = ph[jc]
                        for l in range(32):
                            MM(kb, p[:, 0:255], w1b[a][:, l, jc * 128:(jc + 1) * 128], src[:, l:l + 16 * 254 + 1:16], l == 0, l == 31, r=[w1b[a], src], w=[p])
                        if _c0 == 3:
                            continue
                        X, X2 = xg[jc], x2[jc]
                        ACT(kb, X[:, 0:255], p[:, 0:255], AF.Identity, r=[p, cv[a]], w=[X], bias=cv[a][:, jc:jc + 1], scale=1.0)
                        TT(kb, X2[:, 0:255], X[:, 0:255], X[:, 0:255], ALU.mult, r=[X], w=[X2])
                        TS(kb, X2[:, 0:255], X2[:, 0:255], 0.044715, 1.0, ALU.mult, ALU.add, r=[X2], w=[X2])
                        TT(kb, X2[:, 0:255], X2[:, 0:255], X[:, 0:255], ALU.mult, r=[X, X2], w=[X2])
                        ACT(kb, X2[:, 0:255], X2[:, 0:255], AF.Tanh, r=[X2], w=[X2], scale=0.7978845608028654)
                        STT(kb, X2[:, 0:255], X2[:, 0:255], 1.0, X[:, 0:255], ALU.add, ALU.mult, r=[X, X2], w=[X2])
                        TS(kb, G_[:, jc, 0:255], X2[:, 0:255], 0.5, None, ALU.mult, None, r=[X2], w=[G_])
                    if _c0 in (3, 4):
                        continue
                    if a == 0:
                        for jc in range(2):
                            MM(kb, pk[0:64, 0:256], w2b[0][:, jc, :], G_[:, jc, :], jc == 0, jc == 1, r=[w2b[0], G_, ("gpad", id(G_))], w=[pk])
                        CP(kb, KC[g][:], pk[0:64, 0:256], r=[pk], w=[KC[g]])
                    else:
                        for nc_ in range(2):
                            nn = 128 if nc_ == 0 else 127
                            for jc in range(2):
                                MM(kb, pv[nc_][0:nn, 0:64], G_[:, jc, nc_ * 128:nc_ * 128 + nn], w2b[1][:, jc, :], jc == 0, jc == 1, r=[w2b[1], G_], w=[pv[nc_]])
                            CP(kb, VCx[g][0:nn, nc_, 0:64], pv[nc_][0:nn, 0:64], r=[pv[nc_]], w=[VCx[g]])
                            MSET(kb, VCx[g][0:nn, nc_, 64:65], 1.0, w=[VCx[g]])
                            CP(kb, VCx[g][0:nn, nc_, 65:129], c2s[0:nn, nc_, :], r=[c2s], w=[VCx[g]], eng="act")
            kb.barrier()
        import os as _os
        _stop = int(_os.environ.get("NSA_STOP", "99"))
        if _stop == 0:
            return
        Y = kb.sb(st, [128, 32, 192], F32, "Y")
        IMP = kb.sb(st, [128, 32, 64], F32, "IMP")
        SELT = kb.sb(st, [64, S], BF16, "SELT")
        qT = [kb.sb(st, [64, S], BF16, "qT") for _ in range(2)]
        sc_f = [kb.sb(st, [128, 512], F32, "scf") for _ in range(3)]
        pT = [kb.sb(st, [128, 512], BF16, "pT") for _ in range(3)]
        rd = [kb.sb(st, [128, 1], F32, "rd") for _ in range(4)]
        psc = [kb.ps(st, [128, 512], F32, "psc") for _ in range(3)]
        pacc = [kb.ps(st, [128, 512], F32, "pacc") for _ in range(4)]
        qi = 0
        sci = 0

        pend = []

        def defer(fn):
            pend.append(fn)
            if len(pend) > 2:
                pend.pop(0)()

        def flush():
            while pend:
                pend.pop(0)()

        def load_q(h):
            nonlocal qi
            t = qT[qi % 2]
            qi += 1
            kb.dma("pool", t[:], hF_d[QC + h * 64:QC + (h + 1) * 64, :], w=[t])
            TS(kb, t[:], t[:], 0.125, None, ALU.mult, None, r=[t], w=[t])
            return t

        def finish(acc, ncol, h, br, I, qs, init):
            r_ = rd[qs]
            tile = I * 4 + qs
            TS(kb, r_[:], acc[:, 64:65], 1e-30, None, ALU.max, None, r=[acc], w=[r_])
            kb.op("dve", lambda E: E.reciprocal(out=r_[:], in_=r_[:]), r=[r_], w=[r_])
            hl = h % 3
            if ncol > 65:
                if hl == 0:
                    TS(kb, IMP[:, tile, :], acc[:, 65:129], r_[:, 0:1], None, ALU.mult, None, r=[acc, r_], w=[("IMP", tile)])
                else:
                    STT(kb, IMP[:, tile, :], acc[:, 65:129], r_[:, 0:1], IMP[:, tile, :], ALU.mult, ALU.add, r=[acc, r_, ("IMP", tile)], w=[("IMP", tile)])
            TT(kb, r_[:], r_[:], GATE[:, tile, h * 3 + br:h * 3 + br + 1], ALU.mult, r=[r_, GATE], w=[r_])
            ydst = Y[:, tile, hl * 64:(hl + 1) * 64]
            if init:
                TS(kb, ydst, acc[:, 0:64], r_[:, 0:1], None, ALU.mult, None, r=[acc, r_], w=[("Y", tile, hl)])
            else:
                STT(kb, ydst, acc[:, 0:64], r_[:, 0:1], ydst, ALU.mult, ALU.add, r=[acc, r_, ("Y", tile, hl)], w=[("Y", tile, hl)])

        for g in range(4):
            with contextlib.ExitStack() as s1:
                bCs = [kb.sb(s1, [128, 5, 512], F32, "bC") for _ in range(2)]
                for r in range(3):
                    h = g * 3 + r
                    q_ = load_q(h)
                    bC = bCs[r % 2]
                    kb.dma("sp", bC[:], tab_d[h * 18 + 13:h * 18 + 18, :].rearrange("o (k q) -> k o q", q=512), w=[bC])
                    for I in range(8):
                        ets = []
                        for nc_ in range(2):
                            off = I - 4 * nc_
                            if off < 0:
                                continue
                            p = psc[sci % 3]
                            e_ = pT[sci % 3]
                            f_ = sc_f[sci % 3]
                            sci += 1
                            MM(kb, p[:], KC[g][:, nc_ * 128:(nc_ + 1) * 128], q_[:, I * 512:(I + 1) * 512], True, True, r=[KC[g], q_], w=[p])
                            if off < 5:
                                TT(kb, f_[:], p[:], bC[:, off, :], ALU.add, r=[p, bC], w=[f_])
                                ACT(kb, e_[:], f_[:], AF.Exp, r=[f_], w=[e_])
                            else:
                                ACT(kb, e_[:], p[:], AF.Exp, r=[p, CH], w=[e_], bias=CH[:, h:h + 1], scale=1.0)
                            ets.append((nc_, e_))
                        for qs in range(4):
                            acc = pacc[qs]
                            for ii, (nc_, e_) in enumerate(ets):
                                MM(kb, acc[:, 0:129], e_[:, qs * 128:(qs + 1) * 128], VCx[g][:, nc_, :], ii == 0, ii == len(ets) - 1, r=[e_, VCx[g]], w=[acc])
                            finish(acc, 129, h, 0, I, qs, True)
                kb.barrier()
            if _stop == 1:
                return
            with contextlib.ExitStack() as s2:
                AB = kb.sb(s2, [128, 2, 32, 64], F32, "AB")
                kb.dma("sp", AB[:], P["AB"], w=[AB])
                scs = [kb.sb(s2, [128, 64], F32, "scs") for _ in range(2)]
                sc2 = [kb.sb(s2, [128, 64], F32, "sc2") for _ in range(2)]
                m1 = [kb.sb(s2, [128, 8], F32, "m1") for _ in range(2)]
                m2 = [kb.sb(s2, [128, 8], F32, "m2") for _ in range(2)]
                selb = [kb.sb(s2, [128, 64], BF16, "selb") for _ in range(2)]
                ptb = kb.ps(s2, [128, 1024], BF16, "ptb")
                for tile in range(32):
                    i = tile % 2
                    TT(kb, scs[i][:], IMP[:, tile, :], AB[:, 0, tile, :], ALU.mult, r=[("IMP", tile), AB], w=[scs[i]])
                    TT(kb, scs[i][:], scs[i][:], AB[:, 1, tile, :], ALU.add, r=[scs[i], AB], w=[scs[i]])
                    kb.op("dve", lambda E, i=i: E.max(out=m1[i][:], in_=scs[i][:]), r=[scs[i]], w=[m1[i]])
                    kb.op("dve", lambda E, i=i: E.match_replace(out=sc2[i][:], in_to_replace=m1[i][:], in_values=scs[i][:], imm_value=-2.0), r=[scs[i], m1[i]], w=[sc2[i]])
                    kb.op("dve", lambda E, i=i: E.max(out=m2[i][:], in_=sc2[i][:]), r=[sc2[i]], w=[m2[i]])
                    TS(kb, m2[i][:, 7:8], m2[i][:, 7:8], 0.0, None, ALU.max, None, r=[m2[i]], w=[m2[i]])
                    TS(kb, selb[i][:], scs[i][:], m2[i][:, 7:8], -1.0, ALU.is_ge, ALU.add, r=[scs[i], m2[i]], w=[selb[i]])
                    TR(kb, ptb[0:64, (tile % 4) * 128:(tile % 4 + 1) * 128], selb[i][:], C.ident_b[:], r=[selb[i], C.ident_b], w=[ptb])
                    if tile % 4 == 3:
                        CP(kb, SELT[:, (tile - 3) * 128:(tile + 1) * 128], ptb[0:64, 0:512], r=[ptb], w=[SELT], eng="act")
                kb.barrier()
            if _stop == 2:
                return
            with contextlib.ExitStack() as s3:
                bSs = [kb.sb(s3, [128, 13, 512], F32, "bS") for _ in range(2)]
                Eall = kb.sb(s3, [64, 32, 128], BF16, "Eall")
                kb.dma("pool", Eall[:], P["E"], w=[Eall])
                ksT = kb.sb(s3, [64, S], BF16, "ksT")
                kwT = kb.sb(s3, [64, S], BF16, "kwT")
                kb.dma("pool", ksT[:], hF_d[KSC + g * 64:KSC + (g + 1) * 64, :], w=[ksT])
                kb.dma("pool", kwT[:], hF_d[KWC + g * 64:KWC + (g + 1) * 64, :], w=[kwT])
                VSx = kb.sb(s3, [128, 32, 65], BF16, "VSx")
                VWx = kb.sb(s3, [128, 32, 65], BF16, "VWx")
                MSET(kb, VSx[:, :, 64:65], 1.0, w=[("vs1",)])
                MSET(kb, VWx[:, :, 64:65], 1.0, w=[("vw1",)])
                kb.dma("pool", VSx[:, :, 0:64], hT_d[:, VSC + g * 64:VSC + (g + 1) * 64].rearrange("(t p) c -> p t c", p=128), w=[VSx])
                kb.dma("pool", VWx[:, :, 0:64], hT_d[:, VWC + g * 64:VWC + (g + 1) * 64].rearrange("(t p) c -> p t c", p=128), w=[VWx])
                for r in range(3):
                    h = g * 3 + r
                    q_ = load_q(h)
                    bS = bSs[r % 2]
                    kb.dma("sp", bS[:], tab_d[h * 18:h * 18 + 13, :].rearrange("o (k q) -> k o q", q=512), w=[bS])
                    for I in range(8):
                        for j in range(4 * I + 4):
                            off = j - 4 * I
                            p = psc[sci % 3]
                            e_ = pT[sci % 3]
                            f_ = sc_f[sci % 3]
                            sci += 1
                            MM(kb, p[:], ksT[:, j * 128:(j + 1) * 128], q_[:, I * 512:(I + 1) * 512], True, False, r=[ksT, q_], w=[p])
                            MM(kb, p[:], Eall[:, j, :], SELT[:, I * 512:(I + 1) * 512], False, True, r=[Eall, SELT], w=[p])
                            if off >= -1:
                                TT(kb, f_[:], p[:], bS[:, off + 1, :], ALU.add, r=[p, bS], w=[f_])
                                ACT(kb, e_[:], f_[:], AF.Exp, r=[f_], w=[e_])
                            else:
                                ACT(kb, e_[:], p[:], AF.Exp, r=[p, CH], w=[e_], bias=CH[:, h:h + 1], scale=1.0)
                            def pv_s(e_=e_, j=j, I=I):
                                for qs in range(4):
                                    if j > 4 * I + qs:
                                        continue
                                    MM(kb, pacc[qs][:, 0:65], e_[:, qs * 128:(qs + 1) * 128], VSx[:, j, :], j == 0, j == 4 * I + qs, r=[e_, VSx, ("vs1",)], w=[pacc[qs]])
                            defer(pv_s)
                        flush()
                        for qs in range(4):
                            finish(pacc[qs], 65, h, 1, I, qs, False)
                        for j in range(max(0, 4 * I - 4), 4 * I + 4):
                            off = j - 4 * I
                            p = psc[sci % 3]
                            e_ = pT[sci % 3]
                            f_ = sc_f[sci % 3]
                            sci += 1
                            MM(kb, p[:], kwT[:, j * 128:(j + 1) * 128], q_[:, I * 512:(I + 1) * 512], True, True, r=[kwT, q_], w=[p])
                            TT(kb, f_[:], p[:], bS[:, 5 + off + 4, :], ALU.add, r=[p, bS], w=[f_])
                            ACT(kb, e_[:], f_[:], AF.Exp, r=[f_], w=[e_])
                            def pv_w(e_=e_, j=j, I=I):
                                for qs in range(4):
                                    lo = max(0, 4 * I + qs - 4)
                                    hi = 4 * I + qs
                                    if j < lo or j > hi:
                                        continue
                                    MM(kb, pacc[qs][:, 0:65], e_[:, qs * 128:(qs + 1) * 128], VWx[:, j, :], j == lo, j == hi, r=[e_, VWx, ("vw1",)], w=[pacc[qs]])
                            defer(pv_w)
                        flush()
                        for qs in range(4):
                            finish(pacc[qs], 65, h, 2, I, qs, False)
                kb.dma("sp", mix_d[:, g * 192:(g + 1) * 192].rearrange("(t p) c -> p t c", p=128), Y[:], r=[("Y", t_, hl_) for t_ in range(32) for hl_ in range(3)])
                kb.barrier()


LAYER_KIND = ["nsa", "gla", "rwkv", "nsa"]
LAYER_COLS = [NSA_COLS, GLA_COLS, RWKV_COLS, NSA_COLS]


def gathered_specs():
    sp = [("nsa_w_in0", D, NSA_COLS, F32), ("nsa_w_in1", D, NSA_COLS, F32), ("gla_w_in", D, GLA_COLS, F32), ("rwkv_w_in", D, RWKV_COLS, F32),
          ("cmp_w1_0", 4096, 256, F32), ("cmp_w1_1", 4096, 256, F32), ("tab", 216 * 128, 512, F32)]
    for l in range(DEPTH):
        sp += [("mem_w_kv%d" % l, D, 512, F32), ("w_out%d" % l, D, D, F32)]
    for l in range(DEPTH):
        sp += [("wg%d" % l, NE * D, 2 * D, BF16), ("wd%d" % l, NE * D, D, BF16)]
    return sp


SMALL_SPECS = dict(
    ident=[128, 128], glac=[3, 128, 128], nsa_AB=[128, 2, 32, 64], nsa_c2s=[128, 2, 64], nsa_E=[64, 32, 128], rel31=[12],
    nsa_gate_b=[2, 36], nsa_pe=[2, 2, 32, 64], nsa_w2=[2, 2, 256, 64],
    gla_w_a2=[16, 384], gla_b_a=[384], gla_b_og=[768], gla_norm_w=[192],
    rwkv_mu=[2560], rwkv_w0=[768], rwkv_w2=[64, 768], rwkv_a0=[768], rwkv_a2=[64, 768], rwkv_g2=[128, 768], rwkv_k_k=[768], rwkv_k_a=[768],
    rwkv_r_k=[768], rwkv_ln_w=[768], rwkv_ln_b=[768],
    ln1_g=[4, D], ln1_b=[4, D], ln2_g=[4, D], ln2_b=[4, D], router_w=[4, D, NE], router_b=[4, NE], exp_b_gu=[4, NE, 2 * D], exp_b_dn=[4, NE, D],
)


MODE = "replicate"
NUSE = 8


def build_program(depth=DEPTH, mode=MODE, nseq=None):
    nc = bass.Bass("TRN2", target_bir_lowering=False)
    if nseq is None:
        nseq = 1 if mode == "allgather" else NCORES // NUSE

    def ein(n, shape, dt=F32):
        return nc.dram_tensor(n, list(shape), dt, kind="ExternalInput").ap()

    x_all = ein("x", [nseq, S, D])
    mem_all = ein("mem", [nseq, 256, D])
    sm = {k: ein(k, v) for k, v in SMALL_SPECS.items()}
    y_all = nc.dram_tensor("y", [nseq, S, D], F32, kind="ExternalOutput").ap()
    kb = KB(nc)
    C = Consts(kb, sm["ident"])
    G = {}
    for (name, rows, cols, dt) in gathered_specs():
        step = 1024
        if mode == "allgather":
            rs = rows // NCORES
            src = ein("sh_" + name, [rs, cols])
            bounce = kb.dram([rs, cols], dt, "bn_" + name).ap()
            full = kb.dram([rows, cols], dt, "g_" + name).ap()
            for r0 in range(0, rs, step):
                r1 = min(rs, r0 + step)
                kb.dma("pool", bounce[r0:r1, :], src[r0:r1, :], w=[("bn", name, r0)])
            kb.coll(name, "AllGather", [bounce], [full], r=[("bn", name, r0) for r0 in range(0, rs, step)], w=[("g", name)])
            G[name] = full
        else:
            src = ein("sh_" + name, [rows, cols])
            if dt == F32:
                G[name] = src
            else:
                full = kb.dram([rows, cols], dt, "g_" + name).ap()
                for r0 in range(0, rows, step):
                    kb.dma("pool", full[r0:r0 + step, :], src[r0:r0 + step, :], w=[("g", name, r0)])
                G[name] = full

    def need(nm):
        if mode == "allgather":
            kb.need(nm)

    hF = kb.dram([RWKV_COLS, S], F32, "hF").ap()
    hT = kb.dram([S, RWKV_COLS], F32, "hT").ap()
    mix = kb.dram([S, D], F32, "mix").ap()
    x1 = kb.dram([S, D], F32, "x1").ap()
    xs = [kb.dram([S, D], F32, "xs%d" % i).ap() for i in range(2)]
    rscr = {k: kb.dram([S, TOKW], F32, "rw" + k).ap() for k in "WKABRVGO"}
    rscr["VP"] = kb.dram([128, 6, S], F32, "rwVP").ap()
    if mode != "allgather":
        kb.barrier()
    for sq in range(nseq):
        x_in = x_all[sq]
        mem_d = mem_all[sq]
        y_d = y_all[sq]
        nsa_i = 0
        for l in range(depth):
            kind = LAYER_KIND[l]
            ncols = LAYER_COLS[l]
            hFv, hTv = hF[0:ncols, :], hT[:, 0:ncols]
            if kind == "nsa":
                wname = "nsa_w_in%d" % nsa_i
            elif kind == "gla":
                wname = "gla_w_in"
            else:
                wname = "rwkv_w_in"
            need(wname)
            stage_proj(kb, C, x_in, G[wname], ncols, hFv, hTv)
            if kind == "nsa":
                j = nsa_i
                nsa_i += 1
                need("tab")
                need("cmp_w1_%d" % j)
                P = dict(gate_b=sm["nsa_gate_b"][j], pe=sm["nsa_pe"][j], w1=G["cmp_w1_%d" % j].rearrange("(a r) c -> a r c", a=2), w2=sm["nsa_w2"][j],
                         rel31=sm["rel31"], AB=sm["nsa_AB"], c2s=sm["nsa_c2s"], E=sm["nsa_E"])
                stage_nsa(kb, C, hFv, hTv, P, G["tab"].rearrange("(o k) q -> o (k q)", k=128), mix)
            elif kind == "gla":
                stage_gla(kb, C, hFv, hTv, sm["gla_w_a2"], sm["gla_b_a"], sm["gla_b_og"], sm["gla_norm_w"], sm["glac"], mix)
            else:
                P = {k: sm["rwkv_" + k] for k in ("mu", "w0", "w2", "a0", "a2", "g2", "k_k", "k_a", "r_k", "ln_w", "ln_b")}
                stage_rwkv(kb, C, hTv, P, mix, rscr)
            need("mem_w_kv%d" % l)
            stage_memattn(kb, C, mem_d, G["mem_w_kv%d" % l], hFv, ncols - MEMW, mix)
            need("w_out%d" % l)
            stage_outproj_ln(kb, C, mix, G["w_out%d" % l], x_in, sm["ln1_g"][l], sm["ln1_b"][l], x1)
            need("wg%d" % l)
            need("wd%d" % l)
            xo = y_d if l == depth - 1 else xs[l % 2]
            stage_moe(kb, C, x1, sm["router_w"][l], sm["router_b"][l], G["wg%d" % l].rearrange("(e r) n -> e r n", e=NE), G["wd%d" % l].rearrange("(e r) n -> e r n", e=NE),
                      sm["exp_b_gu"][l], sm["exp_b_dn"][l], sm["ln2_g"][l], sm["ln2_b"][l], xo)
            x_in = xo
    kb.emit()
    return nc


def host_inputs(inp, mode=MODE):
    f = lambda a: np.ascontiguousarray(np.asarray(a, dtype=np.float32))
    AB, c2s, E = nsa_static_np()
    rel = f(inp["rel_table"])
    small = dict(
        ident=np.eye(128, dtype=np.float32), glac=gla_consts_np(), nsa_AB=AB, nsa_c2s=c2s, nsa_E=E, rel31=f(rel[31]),
        nsa_gate_b=f(inp["nsa_gate_b"]), nsa_pe=f(inp["nsa_cmp_pe"]), nsa_w2=f(inp["nsa_cmp_w2"]),
        gla_w_a2=f(inp["gla_w_a2"][0]), gla_b_a=f(inp["gla_b_a"][0]), gla_b_og=f(inp["gla_b_og"][0]), gla_norm_w=f(inp["gla_norm_w"][0]),
        ln1_g=f(inp["ln1_g"]), ln1_b=f(inp["ln1_b"]), ln2_g=f(inp["ln2_g"]), ln2_b=f(inp["ln2_b"]),
        router_w=f(inp["router_w"]), router_b=f(inp["router_b"]), exp_b_gu=f(inp["exp_b_gu"]), exp_b_dn=f(inp["exp_b_dn"]),
    )
    for k in ("mu", "w0", "w2", "a0", "a2", "g2", "k_k", "k_a", "r_k", "ln_w", "ln_b"):
        small["rwkv_" + k] = f(inp["rwkv_" + k][0]).reshape(SMALL_SPECS["rwkv_" + k])
    full = {
        "nsa_w_in0": f(inp["nsa_w_in"][0]), "nsa_w_in1": f(inp["nsa_w_in"][1]), "gla_w_in": f(inp["gla_w_in"][0]), "rwkv_w_in": f(inp["rwkv_w_in"][0]),
        "cmp_w1_0": f(inp["nsa_cmp_w1"][0]).reshape(4096, 256), "cmp_w1_1": f(inp["nsa_cmp_w1"][1]).reshape(4096, 256),
        "tab": nsa_tables_np(rel).reshape(216 * 128, 512),
    }
    for l in range(DEPTH):
        full["mem_w_kv%d" % l] = f(inp["mem_w_kv"][l])
        full["w_out%d" % l] = f(inp["w_out"][l])
    maps = []
    x = np.asarray(inp["x"], dtype=np.float32)
    mem = np.asarray(inp["mem"], dtype=np.float32)
    wgu = np.asarray(inp["exp_w_gu"], dtype=np.float32)
    wdn = np.asarray(inp["exp_w_dn"], dtype=np.float32)
    if mode == "allgather":
        for c in range(NCORES):
            m = {"x": f(x[c:c + 1]), "mem": f(mem[c:c + 1])}
            m.update(small)
            for k, v in full.items():
                rs = v.shape[0] // NCORES
                m["sh_" + k] = np.ascontiguousarray(v[c * rs:(c + 1) * rs])
            for l in range(DEPTH):
                m["sh_wg%d" % l] = np.ascontiguousarray(wgu[l, 4 * c:4 * c + 4]).reshape(4 * D, 2 * D)
                m["sh_wd%d" % l] = np.ascontiguousarray(wdn[l, 4 * c:4 * c + 4]).reshape(4 * D, D)
            maps.append(m)
    else:
        nseq = NCORES // NUSE
        shared = dict(small)
        for k, v in full.items():
            shared["sh_" + k] = v
        for l in range(DEPTH):
            shared["sh_wg%d" % l] = np.ascontiguousarray(wgu[l]).reshape(NE * D, 2 * D)
            shared["sh_wd%d" % l] = np.ascontiguousarray(wdn[l]).reshape(NE * D, D)
        for c in range(NUSE):
            m = {"x": f(x[c * nseq:(c + 1) * nseq]), "mem": f(mem[c * nseq:(c + 1) * nseq])}
            m.update(shared)
            maps.append(m)
    return maps


_NC_CACHE = {}


def kernel(**inputs):
    if "nc" not in _NC_CACHE:
        _NC_CACHE["nc"] = build_program()
    nc = _NC_CACHE["nc"]
    maps = host_inputs(inputs)
    res = run_bass_kernel_spmd(nc, maps, core_ids=list(range(len(maps))))
    return np.concatenate([np.asarray(r["y"], dtype=np.float32) for r in res.results], axis=0)
```
